# Optimizing a Trainium2 kernel written in Bass

```python
import math
import numpy as np
import jax
import jax.numpy as jnp
from jax import lax


D_MODEL = 2048
BATCH = 8
SEQ = 4096
DEPTH = 4

CHUNK = 64
N_EVEN = (DEPTH + 1) // 2
N_ODD = DEPTH // 2
ROPE_THETA = 500000.0
NORM_EPS = 1e-5

A_HEADS = 8
A_HEAD_DIM = 128
A_ROPE_DIM = A_HEAD_DIM // 4
A_NOPE_DIM = A_HEAD_DIM - A_ROPE_DIM
A_V_DIM = 128
A_WIDTH = A_HEADS * A_V_DIM
A_Q_RANK = 512
A_KV_RANK = 256
IDX_HEADS = 16
IDX_DIM = 64
IDX_ROPE_DIM = IDX_DIM // 4
TOPK_MAX = 256
Q_BLOCK = 128

B_HEAD = 64
B_WIDTH = D_MODEL // 2
B_HEADS = B_WIDTH // B_HEAD
B_DECAY_LORA = 64
B_A_LORA = 64
B_GATE_LORA = 160
B_LN_EPS = 64e-5

C_WIDTH = D_MODEL
C_GROUP = 16
C_GROUPS = C_WIDTH // C_GROUP
C_STATE = 64

MEM_LEN = 256
X_HEADS = 4
X_HEAD_DIM = 128

FFN_DIM = 4 * D_MODEL

A_COLS = A_Q_RANK + A_KV_RANK + A_ROPE_DIM + IDX_DIM + IDX_HEADS
B_COLS = 3 * B_WIDTH + B_DECAY_LORA + B_A_LORA + B_GATE_LORA
EVEN_COLS = A_COLS + B_COLS

kernel_name = 'hybrid_dsa_rwkv7_s5_stream_encoder'


def rms_norm(x, g):
    xf = x.astype(jnp.float32)
    y = xf * lax.rsqrt(jnp.mean(xf * xf, axis=-1, keepdims=True) + NORM_EPS)
    return (y * g.astype(jnp.float32)).astype(x.dtype)


def split_cols(t, sizes):
    offs = np.cumsum(sizes)[:-1].tolist()
    return jnp.split(t, offs, axis=-1)


def rope_tables(pos, rot_dim):
    inv_freq = ROPE_THETA ** (-jnp.arange(0, rot_dim, 2, dtype=jnp.float32) / rot_dim)
    ang = pos[..., None] * inv_freq
    return jnp.cos(ang), jnp.sin(ang)


def apply_partial_rope(x, cos, sin, rot_dim):
    xf = x.astype(jnp.float32)
    half = rot_dim // 2
    x1, x2, rest = xf[..., :half], xf[..., half:rot_dim], xf[..., rot_dim:]
    out = jnp.concatenate([x1 * cos - x2 * sin, x2 * cos + x1 * sin, rest], axis=-1)
    return out.astype(x.dtype)


def token_shift(t):
    return jnp.pad(t[:, :-1], ((0, 0), (1, 0), (0, 0)))


def dsa_mixer(h, cos_a, sin_a, cos_i, sin_i, cq_norm, ckv_norm, kidx_norm, w_uq, w_uk, w_uv, w_qidx):
    f32 = jnp.float32
    bsz, seq, _ = h.shape
    c_q, c_kv, k_rope, k_idx, w_idx = split_cols(h, [A_Q_RANK, A_KV_RANK, A_ROPE_DIM, IDX_DIM, IDX_HEADS])
    c_q = rms_norm(c_q, cq_norm)
    c_kv = rms_norm(c_kv, ckv_norm)
    k_rope = apply_partial_rope(k_rope, cos_a, sin_a, A_ROPE_DIM)
    q = jnp.einsum('bsr,rhd->bshd', c_q, w_uq)
    q_rope = apply_partial_rope(q[..., :A_ROPE_DIM], cos_a[:, :, None], sin_a[:, :, None], A_ROPE_DIM)
    q_lat = jnp.einsum('bshd,rhd->bshr', q[..., A_ROPE_DIM:], w_uk)
    q_idx = apply_partial_rope(jnp.einsum('bsr,rhd->bshd', c_q, w_qidx), cos_i[:, :, None], sin_i[:, :, None], IDX_ROPE_DIM)
    k_idx = apply_partial_rope(rms_norm(k_idx, kidx_norm), cos_i, sin_i, IDX_ROPE_DIM)
    w_idx = w_idx.astype(f32) * (IDX_HEADS ** -0.5) * (IDX_DIM ** -0.5)

    topk = min(TOPK_MAX, seq // 4)
    n_blk = seq // Q_BLOCK
    scale = A_HEAD_DIM ** -0.5
    key_pos = jnp.arange(seq, dtype=jnp.int32)
    gather = jax.vmap(lambda table, idx: table[idx])

    def to_blocks(t):
        return t.reshape((bsz, n_blk, Q_BLOCK) + t.shape[2:]).swapaxes(0, 1)

    def attend_block(args):
        ql, qr, qi, wi, t0 = args
        q_pos = t0 + jnp.arange(Q_BLOCK, dtype=jnp.int32)
        limit = (q_pos // CHUNK + 1) * CHUNK
        allowed = key_pos[None, :] < limit[:, None]
        logits = jnp.einsum('bqhd,bsd->bqhs', qi.astype(f32), k_idx.astype(f32))
        score = jnp.einsum('bqh,bqhs->bqs', wi, jax.nn.relu(logits))
        score = jnp.where(allowed[None], score, -jnp.inf)
        _, sel = lax.top_k(score, topk)
        valid = sel < limit[None, :, None]
        ckv_sel = gather(c_kv, sel)
        kr_sel = gather(k_rope, sel)
        s = (jnp.einsum('bqhr,bqkr->bqhk', ql, ckv_sel).astype(f32)
             + jnp.einsum('bqhd,bqkd->bqhk', qr, kr_sel).astype(f32)) * scale
        s = jnp.where(valid[:, :, None, :], s, -jnp.inf)
        p = jax.nn.softmax(s, axis=-1).astype(ckv_sel.dtype)
        return jnp.einsum('bqhk,bqkr->bqhr', p, ckv_sel)

    blk_start = jnp.arange(n_blk, dtype=jnp.int32) * Q_BLOCK
    o_lat = lax.map(attend_block, (to_blocks(q_lat), to_blocks(q_rope), to_blocks(q_idx), to_blocks(w_idx), blk_start))
    o_lat = o_lat.swapaxes(0, 1).reshape(bsz, seq, A_HEADS, A_KV_RANK)
    return jnp.einsum('bshr,rhd->bshd', o_lat, w_uv).reshape(bsz, seq, A_WIDTH)


def rwkv7_mixer(h, mu, w0, w2, a0, a2, g2, k_k, k_a, r_k, ln_w, ln_b):
    f32 = jnp.float32
    bsz, seq, _ = h.shape
    hm = h + (token_shift(h) - h) * mu
    r, k, v, wl, al, gl = split_cols(hm, [B_WIDTH, B_WIDTH, B_WIDTH, B_DECAY_LORA, B_A_LORA, B_GATE_LORA])
    log_w = -jax.nn.softplus(-(w0 + jnp.tanh(wl) @ w2).astype(f32)) - 0.5
    decay = jnp.exp(-jnp.exp(log_w))
    a = jax.nn.sigmoid((a0 + al @ a2).astype(f32))
    g = (jax.nn.sigmoid(gl) @ g2).astype(f32)

    def heads(t):
        return t.reshape(bsz, seq, B_HEADS, B_HEAD)

    r, k, v = heads(r.astype(f32)), heads(k.astype(f32)), heads(v.astype(f32))
    decay, a = heads(decay), heads(a)
    kk = k * k_k.astype(f32).reshape(B_HEADS, B_HEAD)
    kk = kk * lax.rsqrt(jnp.maximum(jnp.sum(kk * kk, axis=-1, keepdims=True), 1e-24))
    k = k * (1.0 + (a - 1.0) * k_a.astype(f32).reshape(B_HEADS, B_HEAD))

    def step(state, inp):
        r_t, w_t, k_t, v_t, kk_t, a_t = inp
        sa = jnp.einsum('bhvk,bhk->bhv', state, -kk_t)
        state = (state * w_t[:, :, None, :] + sa[..., None] * (kk_t * a_t)[:, :, None, :]
                 + v_t[..., None] * k_t[:, :, None, :])
        return state, jnp.einsum('bhvk,bhk->bhv', state, r_t)

    tm = lambda t: jnp.moveaxis(t, 1, 0)
    state0 = jnp.zeros((bsz, B_HEADS, B_HEAD, B_HEAD), f32)
    _, out = lax.scan(step, state0, (tm(r), tm(decay), tm(k), tm(v), tm(kk), tm(a)))
    out = jnp.moveaxis(out, 0, 1)
    mean = jnp.mean(out, axis=-1, keepdims=True)
    var = jnp.mean(jnp.square(out - mean), axis=-1, keepdims=True)
    out = ((out - mean) * lax.rsqrt(var + B_LN_EPS)).reshape(bsz, seq, B_WIDTH)
    out = out * ln_w.astype(f32) + ln_b.astype(f32)
    bonus = jnp.sum(r * k * r_k.astype(f32), axis=-1, keepdims=True) * v
    out = (out + bonus.reshape(bsz, seq, B_WIDTH)) * g
    return out.astype(h.dtype)


def s5_mixer(u, lam_re, lam_im, log_dt, b_re, b_im, c_re, c_im, d_skip, w_glu, b_glu):
    f32 = jnp.float32
    bsz, seq, _ = u.shape
    lam = lax.complex(jnp.minimum(lam_re.astype(f32), -1e-4), lam_im.astype(f32))
    dt = jnp.exp(log_dt.astype(f32))[:, None]
    lam_bar = jnp.exp(lam * dt)
    b_bar = ((lam_bar - 1.0) / lam)[..., None] * lax.complex(b_re.astype(f32), b_im.astype(f32))
    c_mat = lax.complex(c_re.astype(f32), c_im.astype(f32))
    n_chunk = seq // CHUNK
    uf = u.astype(f32)
    u_chunks = jnp.moveaxis(uf.reshape(bsz, n_chunk, CHUNK, C_GROUPS, C_GROUP), 1, 0)
    a_elem = jnp.broadcast_to(lam_bar, (bsz, CHUNK, C_GROUPS, C_STATE))

    def combine(e1, e2):
        a1, x1 = e1
        a2, x2 = e2
        return a1 * a2, a2 * x1 + x2

    def chunk_step(state, u_c):
        bu = jnp.einsum('bcgi,gpi->bcgp', u_c.astype(jnp.complex64), b_bar)
        bu = bu.at[:, 0].add(lam_bar * state)
        _, states = lax.associative_scan(combine, (a_elem, bu), axis=1)
        y = jnp.einsum('bcgp,gip->bcgi', states, c_mat).real
        return states[:, -1], y

    state0 = jnp.zeros((bsz, C_GROUPS, C_STATE), jnp.complex64)
    _, y = lax.scan(chunk_step, state0, u_chunks)
    y = jnp.moveaxis(y, 0, 1).reshape(bsz, seq, C_WIDTH) + d_skip.astype(f32) * uf
    z = jax.nn.gelu(y)
    out = z * jax.nn.sigmoid(z @ w_glu.astype(f32) + b_glu.astype(f32))
    return out.astype(u.dtype)


def memory_cross_attention(xn, memn, wq, wkv, wo):
    f32 = jnp.float32
    bsz, seq, _ = xn.shape
    q = (xn @ wq).reshape(bsz, seq, X_HEADS, X_HEAD_DIM)
    kv = (memn @ wkv).reshape(bsz, memn.shape[1], 2, X_HEADS, X_HEAD_DIM)
    s = jnp.einsum('bshd,bmhd->bshm', q, kv[:, :, 0]).astype(f32) * (X_HEAD_DIM ** -0.5)
    p = jax.nn.softmax(s, axis=-1).astype(xn.dtype)
    o = jnp.einsum('bshm,bmhd->bshd', p, kv[:, :, 1]).reshape(bsz, seq, X_HEADS * X_HEAD_DIM)
    return o @ wo


def squared_relu_mlp(xn, w_up, w_down):
    return jnp.square(jax.nn.relu(xn @ w_up)) @ w_down


def setup_inputs(seed: int = 0) -> dict:
    key = jax.random.key(seed)
    ks = iter(jax.random.split(key, 64))
    f32 = jnp.float32

    def nrm(shape, scale):
        return scale * jax.random.normal(next(ks), shape, f32)

    def gain(shape):
        return 1.0 + 0.02 * jax.random.normal(next(ks), shape, f32)

    ramp = (jnp.arange(B_WIDTH, dtype=f32) / (B_WIDTH - 1)) ** 0.85
    inp = {}
    inp['x'] = nrm((BATCH, SEQ, D_MODEL), 1.0)
    inp['mem'] = nrm((BATCH, MEM_LEN, D_MODEL), 1.0)
    inp['start_frame'] = jax.random.randint(next(ks), (BATCH,), 0, 256, dtype=jnp.int32) * CHUNK
    inp['norm_mix'] = gain((DEPTH, D_MODEL))
    inp['norm_xattn'] = gain((DEPTH, D_MODEL))
    inp['norm_mem'] = gain((DEPTH, D_MODEL))
    inp['norm_ffn'] = gain((DEPTH, D_MODEL))
    inp['final_norm'] = gain((D_MODEL,))
    inp['xattn_wq'] = nrm((DEPTH, D_MODEL, X_HEADS * X_HEAD_DIM), D_MODEL ** -0.5)
    inp['xattn_wkv'] = nrm((DEPTH, D_MODEL, 2 * X_HEADS * X_HEAD_DIM), D_MODEL ** -0.5)
    inp['xattn_wo'] = nrm((DEPTH, X_HEADS * X_HEAD_DIM, D_MODEL), (X_HEADS * X_HEAD_DIM) ** -0.5)
    inp['ffn_up'] = nrm((DEPTH, D_MODEL, FFN_DIM), D_MODEL ** -0.5)
    inp['ffn_down'] = nrm((DEPTH, FFN_DIM, D_MODEL), FFN_DIM ** -0.5)
    inp['even_w_in'] = nrm((N_EVEN, D_MODEL, EVEN_COLS), D_MODEL ** -0.5)
    inp['even_w_out'] = nrm((N_EVEN, A_WIDTH + B_WIDTH, D_MODEL), (A_WIDTH + B_WIDTH) ** -0.5)
    inp['dsa_cq_norm'] = gain((N_EVEN, A_Q_RANK))
    inp['dsa_ckv_norm'] = gain((N_EVEN, A_KV_RANK))
    inp['dsa_kidx_norm'] = gain((N_EVEN, IDX_DIM))
    inp['dsa_w_uq'] = nrm((N_EVEN, A_Q_RANK, A_HEADS, A_HEAD_DIM), A_Q_RANK ** -0.5)
    inp['dsa_w_uk'] = nrm((N_EVEN, A_KV_RANK, A_HEADS, A_NOPE_DIM), A_KV_RANK ** -0.5)
    inp['dsa_w_uv'] = nrm((N_EVEN, A_KV_RANK, A_HEADS, A_V_DIM), A_KV_RANK ** -0.5)
    inp['dsa_w_qidx'] = nrm((N_EVEN, A_Q_RANK, IDX_HEADS, IDX_DIM), A_Q_RANK ** -0.5)
    inp['rwkv_mu'] = jax.random.uniform(next(ks), (N_EVEN, B_COLS), f32)
    inp['rwkv_w0'] = -6.0 + 5.0 * ramp + nrm((N_EVEN, B_WIDTH), 0.1)
    inp['rwkv_w2'] = nrm((N_EVEN, B_DECAY_LORA, B_WIDTH), 0.5 * B_DECAY_LORA ** -0.5)
    inp['rwkv_a0'] = nrm((N_EVEN, B_WIDTH), 0.1)
    inp['rwkv_a2'] = nrm((N_EVEN, B_A_LORA, B_WIDTH), B_A_LORA ** -0.5)
    inp['rwkv_g2'] = nrm((N_EVEN, B_GATE_LORA, B_WIDTH), B_GATE_LORA ** -0.5)
    inp['rwkv_k_k'] = 0.85 + nrm((N_EVEN, B_WIDTH), 0.02)
    inp['rwkv_k_a'] = gain((N_EVEN, B_WIDTH))
    inp['rwkv_r_k'] = -0.04 + nrm((N_EVEN, B_HEADS, B_HEAD), 0.1)
    inp['rwkv_ln_w'] = gain((N_EVEN, B_WIDTH))
    inp['rwkv_ln_b'] = nrm((N_EVEN, B_WIDTH), 0.02)
    inp['odd_w_in'] = nrm((N_ODD, D_MODEL, C_WIDTH), D_MODEL ** -0.5)
    inp['odd_w_out'] = nrm((N_ODD, C_WIDTH, D_MODEL), C_WIDTH ** -0.5)
    inp['s5_lam_re'] = -0.5 + nrm((N_ODD, C_GROUPS, C_STATE), 0.01)
    inp['s5_lam_im'] = math.pi * jnp.arange(C_STATE, dtype=f32) + nrm((N_ODD, C_GROUPS, C_STATE), 0.01)
    inp['s5_log_dt'] = jax.random.uniform(next(ks), (N_ODD, C_GROUPS), f32, math.log(1e-3), math.log(1e-1))
    inp['s5_b_re'] = nrm((N_ODD, C_GROUPS, C_STATE, C_GROUP), (2 * C_GROUP) ** -0.5)
    inp['s5_b_im'] = nrm((N_ODD, C_GROUPS, C_STATE, C_GROUP), (2 * C_GROUP) ** -0.5)
    inp['s5_c_re'] = nrm((N_ODD, C_GROUPS, C_GROUP, C_STATE), C_STATE ** -0.5)
    inp['s5_c_im'] = nrm((N_ODD, C_GROUPS, C_GROUP, C_STATE), C_STATE ** -0.5)
    inp['s5_d'] = nrm((N_ODD, C_WIDTH), 1.0)
    inp['s5_w_glu'] = nrm((N_ODD, C_WIDTH, C_WIDTH), C_WIDTH ** -0.5)
    inp['s5_b_glu'] = nrm((N_ODD, C_WIDTH), 0.02)
    return inp


def reference(x, mem, start_frame, norm_mix, norm_xattn, norm_mem, norm_ffn, final_norm,
              xattn_wq, xattn_wkv, xattn_wo, ffn_up, ffn_down, even_w_in, even_w_out,
              dsa_cq_norm, dsa_ckv_norm, dsa_kidx_norm, dsa_w_uq, dsa_w_uk, dsa_w_uv, dsa_w_qidx,
              rwkv_mu, rwkv_w0, rwkv_w2, rwkv_a0, rwkv_a2, rwkv_g2, rwkv_k_k, rwkv_k_a, rwkv_r_k,
              rwkv_ln_w, rwkv_ln_b, odd_w_in, odd_w_out, s5_lam_re, s5_lam_im, s5_log_dt,
              s5_b_re, s5_b_im, s5_c_re, s5_c_im, s5_d, s5_w_glu, s5_b_glu):
    bsz, seq, _ = x.shape
    pos = (start_frame[:, None] + jnp.arange(seq, dtype=jnp.int32)[None, :]).astype(jnp.float32)
    cos_a, sin_a = rope_tables(pos, A_ROPE_DIM)
    cos_i, sin_i = rope_tables(pos, IDX_ROPE_DIM)
    for layer in range(DEPTH):
        i = layer // 2
        xn = rms_norm(x, norm_mix[layer])
        if layer % 2 == 0:
            h = xn @ even_w_in[i]
            y_a = dsa_mixer(h[..., :A_COLS], cos_a, sin_a, cos_i, sin_i, dsa_cq_norm[i], dsa_ckv_norm[i],
                            dsa_kidx_norm[i], dsa_w_uq[i], dsa_w_uk[i], dsa_w_uv[i], dsa_w_qidx[i])
            y_b = rwkv7_mixer(h[..., A_COLS:], rwkv_mu[i], rwkv_w0[i], rwkv_w2[i], rwkv_a0[i], rwkv_a2[i],
                              rwkv_g2[i], rwkv_k_k[i], rwkv_k_a[i], rwkv_r_k[i], rwkv_ln_w[i], rwkv_ln_b[i])
            mix = jnp.concatenate([y_a, y_b], axis=-1) @ even_w_out[i]
        else:
            y_c = s5_mixer(xn @ odd_w_in[i], s5_lam_re[i], s5_lam_im[i], s5_log_dt[i], s5_b_re[i], s5_b_im[i],
                           s5_c_re[i], s5_c_im[i], s5_d[i], s5_w_glu[i], s5_b_glu[i])
            mix = y_c @ odd_w_out[i]
        x = x + mix
        x = x + memory_cross_attention(rms_norm(x, norm_xattn[layer]), rms_norm(mem, norm_mem[layer]),
                                       xattn_wq[layer], xattn_wkv[layer], xattn_wo[layer])
        x = x + squared_relu_mlp(rms_norm(x, norm_ffn[layer]), ffn_up[layer], ffn_down[layer])
    return rms_norm(x, final_norm)
```

```python
import numpy as np
from contextlib import ExitStack
import concourse.bass as bass
import concourse.mybir as mybir
from concourse.bass_utils import run_bass_kernel_spmd

F32 = mybir.dt.float32
BF16 = mybir.dt.bfloat16
I32 = mybir.dt.int32
ALU = mybir.AluOpType
AF = mybir.ActivationFunctionType
AX = mybir.AxisListType

import os as _os
SAFE_SAME_ENGINE = _os.environ.get("UNSAFE_SAME", "0") != "1"


class V:
    __slots__ = ("ap", "buf")

    def __init__(self, ap, buf):
        self.ap = ap
        self.buf = buf

    def __getitem__(self, idx):
        return V(self.ap[idx], self.buf)

    def rearrange(self, *a, **k):
        return V(self.ap.rearrange(*a, **k), self.buf)

    def broadcast_to(self, shape):
        return V(self.ap.broadcast_to(shape), self.buf)

    def bitcast(self, dt):
        return V(self.ap.bitcast(dt), self.buf)

    @property
    def shape(self):
        return self.ap.shape


class Buf:
    __slots__ = ("name", "last_write", "readers", "psum")

    def __init__(self, name, psum=False):
        self.name = name
        self.psum = psum
        self.last_write = None
        self.readers = []


class Prog:
    NDMA = 8

    def __init__(self, nc, es):
        self.nc = nc
        self.eng = {"pe": nc.tensor, "act": nc.scalar, "dve": nc.vector, "pool": nc.gpsimd, "sp": nc.sync}
        self.sem = {}
        self.count = {}
        for e in self.eng:
            self.sem[e] = es.enter_context(nc.semaphore("c_" + e))
            self.count[e] = 0
        self.dsem = {}
        self.dcount = {}
        self.dnext = {}
        for q in ("sp", "act", "pool"):
            for i in range(self.NDMA):
                key = ("d", q, i)
                self.sem[key] = es.enter_context(nc.semaphore("d_%s%d" % (q, i)))
                self.count[key] = 0
            self.dnext[q] = 0
        self.known = {e: {} for e in self.eng}
        self.bufs = []
        self.ninst = 0

    def sb(self, es, name, shape, dt):
        t = es.enter_context(self.nc.sbuf_tensor(name, list(shape), dt))
        b = Buf(name)
        self.bufs.append(b)
        return V(t[:], b)

    def ps(self, es, name, shape, dt):
        t = es.enter_context(self.nc.psum_tensor(name, list(shape), dt))
        b = Buf(name, True)
        self.bufs.append(b)
        return V(t[:], b)

    def _need(self, e, needs):
        kn = self.known[e]
        for key, val in needs.items():
            if key == e and (e == "pe" or not SAFE_SAME_ENGINE) and not isinstance(key, tuple):
                continue
            if kn.get(key, 0) >= val:
                continue
            self.eng[e].wait_ge(self.sem[key], val)
            kn[key] = val

    def I(self, e, method, **kw):
        needs = {}
        reads, writes = [], []
        args = {}
        for k, v in kw.items():
            if isinstance(v, V):
                args[k] = v.ap
                if v.buf is not None:
                    (writes if k in ("out", "accum_out", "ap") else reads).append(v.buf)
            else:
                args[k] = v

        def add(tok):
            if tok is not None:
                if needs.get(tok[0], 0) < tok[1]:
                    needs[tok[0]] = tok[1]

        for b in reads:
            add(b.last_write)
            if b.psum:
                for r in b.readers:
                    if r[0] != e:
                        add(r)
        for b in writes:
            add(b.last_write)
            for r in b.readers:
                add(r)
        is_dma = method == "dma_start"
        if is_dma:
            q = e
            i = self.dnext[q]
            self.dnext[q] = (i + 1) % self.NDMA
            key = ("d", q, i)
            if self.count[key] > 0:
                add((key, self.count[key]))
        self._need(e, needs)
        inst = getattr(self.eng[e], method)(**args)
        self.ninst += 1
        if is_dma:
            self.count[key] += 16
            inst.then_inc(self.sem[key], 16)
            tok = (key, self.count[key])
        else:
            self.count[e] += 1
            inst.then_inc(self.sem[e], 1)
            tok = (e, self.count[e])
        for b in reads:
            b.readers.append(tok)
            if len(b.readers) > 64:
                m = {}
                for r in b.readers:
                    if m.get(r[0], 0) < r[1]:
                        m[r[0]] = r[1]
                b.readers = list(m.items())
        for b in writes:
            b.last_write = tok
            b.readers = []
        return inst

    def barrier(self):
        needs = {k: c for k, c in self.count.items() if c > 0}
        for e in self.eng:
            self._need(e, dict(needs))
        for b in self.bufs:
            b.last_write = None
            b.readers = []
        self.bufs = []

    def barrier_dram(self):
        needs = {k: c for k, c in self.count.items() if c > 0 and isinstance(k, tuple)}
        for e in ("sp", "act", "pool"):
            self._need(e, dict(needs))

    def dma(self, q, out, in_):
        return self.I(q, "dma_start", out=out, in_=in_)

    def mm(self, out, lhsT, rhs, start=True, stop=True):
        return self.I("pe", "matmul", out=out, lhsT=lhsT, rhs=rhs, start=start, stop=stop)

    def tr(self, out, in_, ident):
        return self.I("pe", "transpose", out=out, in_=in_, identity=ident)

    def act(self, out, in_, func, bias=None, scale=None, accum_out=None, e="act"):
        kw = dict(out=out, in_=in_, func=func)
        if bias is not None:
            kw["bias"] = bias
        if scale is not None:
            kw["scale"] = scale
        if accum_out is not None:
            kw["accum_out"] = accum_out
        return self.I(e, "activation", **kw)

    def tt(self, e, out, in0, in1, op):
        return self.I(e, "tensor_tensor", out=out, in0=in0, in1=in1, op=op)

    def ts(self, e, out, in0, s1, op0, s2=None, op1=None, accum_out=None):
        kw = dict(out=out, in0=in0, scalar1=s1, scalar2=s2, op0=op0)
        if op1 is not None:
            kw["op1"] = op1
        if accum_out is not None:
            kw["accum_out"] = accum_out
        return self.I(e, "tensor_scalar", **kw)

    def stt(self, e, out, in0, scalar, in1, op0, op1):
        return self.I(e, "scalar_tensor_tensor", out=out, in0=in0, scalar=scalar, in1=in1, op0=op0, op1=op1)

    def copy(self, e, out, in_):
        if e == "act":
            return self.I(e, "copy", out=out, in_=in_)
        return self.I(e, "tensor_copy", out=out, in_=in_)

    def memset(self, e, out, val):
        return self.I(e, "memset", ap=out, constant=val)

    def recip(self, out, in_):
        return self.I("dve", "reciprocal", out=out, in_=in_)

    def reduce(self, out, in_, op, axis=None):
        return self.I("dve", "tensor_reduce", out=out, in_=in_, axis=axis or AX.X, op=op)

    def scan(self, out, d0, d1, initial, op0, op1):
        return self.I("dve", "tensor_tensor_scan", out=out, data0=d0, data1=d1, initial=initial, op0=op0, op1=op1)


class Ring:
    def __init__(self, items):
        self.items = items
        self.i = 0

    def next(self):
        v = self.items[self.i % len(self.items)]
        self.i += 1
        return v


def dram(nc, name, shape, dt, kind="Internal"):
    return V(nc.dram_tensor(name, list(shape), dt, kind=kind).ap(), None)


def kchunks(K):
    return [(k0, min(128, K - k0)) for k0 in range(0, K, 128)]


def st_norm(P, xT, g, outT, D, S, eps, consts, tag, out_dt=BF16):
    nc = P.nc
    pp = min(D, 128)
    kc = D // pp
    TT = min(512, S)
    with ExitStack() as es:
        gs = P.sb(es, tag + "g", [pp, kc], F32)
        P.dma("sp", gs, g)
        xs = [P.sb(es, tag + "x%d" % i, [pp, kc, TT], F32) for i in range(2)]
        sq = P.sb(es, tag + "sq", [pp, kc, TT], F32)
        os_ = [P.sb(es, tag + "o%d" % i, [pp, kc, TT], out_dt) for i in range(2)]
        rs = P.sb(es, tag + "rs", [pp, TT], F32)
        pss = [P.ps(es, tag + "ps%d" % i, [pp, TT], F32) for i in range(2)]
        xv = xT.rearrange("(c p) s -> p c s", p=pp)
        ov = outT.rearrange("(c p) s -> p c s", p=pp)
        ones = consts["ones_f32"]
        for it, t0 in enumerate(range(0, S, TT)):
            x = xs[it % 2]
            o = os_[it % 2]
            ps = pss[it % 2]
            P.dma("sp", x, xv[:, :, t0:t0 + TT])
            P.act(sq, x, AF.Square)
            for c in range(kc):
                P.mm(ps, ones[0:pp, 0:pp], sq[:, c, :], start=(c == 0), stop=(c == kc - 1))
            P.act(rs, ps, AF.Sqrt, bias=consts["eps_%g" % eps][0:pp, :], scale=1.0 / D)
            P.recip(rs, rs)
            for c in range(kc):
                P.stt("dve", o[:, c, :], x[:, c, :], gs[:, c:c + 1], rs, ALU.mult, ALU.mult)
            P.dma("sp", ov[:, :, t0:t0 + TT], o)
        P.barrier()


def st_mm(P, aT, K, S, jobs, consts, tag, TT=2048, CG=512, a_dt=BF16, w_dt=F32, wscr=None):
    kcs = kchunks(K)
    kc = len(kcs)
    pp = kcs[0][1]
    TT = min(TT, S)
    ntt = (S + TT - 1) // TT
    regular = (K % 128 == 0 or K < 128)
    pre = wscr is not None and w_dt == F32 and ntt >= 2 and regular
    groups = []
    for ji, (W, n, epi) in enumerate(jobs):
        for g0 in range(0, n, CG):
            groups.append((ji, g0, min(CG, n - g0)))
    gstride = pp * kc * CG
    if pre:
        with ExitStack() as esp:
            wfp = [P.sb(esp, tag + "pwf%d" % i, [pp, kc, CG], F32) for i in range(2)]
            wbp = [P.sb(esp, tag + "pwb%d" % i, [pp, kc, CG], BF16) for i in range(2)]
            for gi, (ji, g0, gsz) in enumerate(groups):
                W = jobs[ji][0]
                f, b = wfp[gi % 2], wbp[gi % 2]
                P.dma("act" if gi % 2 else "sp", f[:, :, 0:gsz], W.rearrange("(c p) n -> p c n", p=pp)[:, :, g0:g0 + gsz])
                P.copy("dve" if gi % 2 else "act", b[:, :, 0:gsz], f[:, :, 0:gsz])
                dst = wscr[gi * gstride:(gi + 1) * gstride].rearrange("(p c n) -> p c n", p=pp, c=kc)
                P.dma("pool", dst[:, :, 0:gsz], b[:, :, 0:gsz])
            P.barrier()
    with ExitStack() as es:
        na = 2 if (kc * TT * 2 <= 65536 and a_dt == BF16) else 1
        a_s = [P.sb(es, tag + "a%d" % i, [pp, kc, TT], BF16) for i in range(na)]
        af = P.sb(es, tag + "af", [pp, kc, TT], F32) if a_dt == F32 else None
        wf = [P.sb(es, tag + "wf%d" % i, [pp, kc, CG], F32) for i in range(2)] if (w_dt == F32 and not pre) else None
        nwb = 3 if pre else 2
        wb = [P.sb(es, tag + "wb%d" % i, [pp, kc, CG], BF16) for i in range(nwb)]
        pss = Ring([P.ps(es, tag + "ps%d" % i, [128, 512], F32) for i in range(4)])
        state = {"iw": 0}

        def load_f32(gi, ji, g0, gsz):
            iw = state["iw"]
            state["iw"] += 1
            W = jobs[ji][0]
            b = wb[iw % nwb]
            if w_dt != F32:
                if regular:
                    P.dma("act" if iw % 2 else "sp", b[:, :, 0:gsz], W.rearrange("(c p) n -> p c n", p=pp)[:, :, g0:g0 + gsz])
                else:
                    for c, (k0, ksz) in enumerate(kcs):
                        P.dma("sp", b[0:ksz, c, 0:gsz], W[k0:k0 + ksz, g0:g0 + gsz])
                return b
            f = wf[iw % 2]
            ce = "dve" if iw % 2 else "act"
            if regular:
                P.dma("act" if iw % 2 else "sp", f[:, :, 0:gsz], W.rearrange("(c p) n -> p c n", p=pp)[:, :, g0:g0 + gsz])
                P.copy(ce, b[:, :, 0:gsz], f[:, :, 0:gsz])
            else:
                for c, (k0, ksz) in enumerate(kcs):
                    P.dma("sp", f[0:ksz, c, 0:gsz], W[k0:k0 + ksz, g0:g0 + gsz])
                    P.copy(ce, b[0:ksz, c, 0:gsz], f[0:ksz, c, 0:gsz])
            return b

        for it, t0 in enumerate(range(0, S, TT)):
            a = a_s[it % len(a_s)]
            ald = af if a_dt == F32 else a
            if regular:
                P.dma("sp", ald, aT.rearrange("(c p) s -> p c s", p=pp)[:, :, t0:t0 + TT])
                if a_dt == F32:
                    P.copy("dve", a, af)
            else:
                for c, (k0, ksz) in enumerate(kcs):
                    P.dma("sp", ald[0:ksz, c, :], aT[k0:k0 + ksz, t0:t0 + TT])
                    if a_dt == F32:
                        P.copy("dve", a[0:ksz, c, :], af[0:ksz, c, :])
            for gi, (ji, g0, gsz) in enumerate(groups):
                epi = jobs[ji][2]
                if pre:
                    iw = state["iw"]
                    state["iw"] += 1
                    b = wb[iw % nwb]
                    src = wscr[gi * gstride:(gi + 1) * gstride].rearrange("(p c n) -> p c n", p=pp, c=kc)
                    P.dma("act" if iw % 2 else "sp", b[:, :, 0:gsz], src[:, :, 0:gsz])
                else:
                    b = load_f32(gi, ji, g0, gsz)
                for m0 in range(0, gsz, 128):
                    msz = min(128, gsz - m0)
                    for n0 in range(0, TT, 512):
                        nsz = min(512, TT - n0)
                        ps = pss.next()
                        for c, (k0, ksz) in enumerate(kcs):
                            P.mm(ps[0:msz, 0:nsz], b[0:ksz, c, m0:m0 + msz], a[0:ksz, c, n0:n0 + nsz],
                                 start=(c == 0), stop=(c == kc - 1))
                        epi(P, ps[0:msz, 0:nsz], g0 + m0, msz, t0 + n0, nsz)
        P.barrier()


STORE_Q = _os.environ.get("STORE_Q", "pool")


class EpiStore:
    def __init__(self, P, es, outT, dt, tag, func=None, nbuf=3):
        self.outT = outT
        self.ring = Ring([P.sb(es, tag + "e%d" % i, [128, 512], dt) for i in range(nbuf)])
        self.func = func
        self.i = 0

    def __call__(self, P, ps, c0, csz, t0, tsz):
        o = self.ring.next()[0:csz, 0:tsz]
        if self.i % 2 == 0:
            P.act(o, ps, self.func or AF.Copy)
        else:
            if self.func is None:
                P.copy("dve", o, ps)
            else:
                P.act(o, ps, self.func)
        self.i += 1
        P.dma(STORE_Q, self.outT[c0:c0 + csz, t0:t0 + tsz], o)


class EpiResid:
    def __init__(self, P, es, xT, tag, nbuf=3):
        self.xT = xT
        self.ring = Ring([P.sb(es, tag + "r%d" % i, [128, 512], F32) for i in range(nbuf)])

    def __call__(self, P, ps, c0, csz, t0, tsz):
        o = self.ring.next()[0:csz, 0:tsz]
        P.dma("act", o, self.xT[c0:c0 + csz, t0:t0 + tsz])
        P.tt("dve", o, ps, o, ALU.add)
        P.dma(STORE_Q, self.xT[c0:c0 + csz, t0:t0 + tsz], o)


def make_consts(P, es, cd):
    c = {}
    for name, (shape, dt) in CONST_SPECS.items():
        t = P.sb(es, "k_" + name, shape, dt)
        P.dma("sp", t, cd[name])
        c[name] = t
    return c


CONST_SPECS = {
    "ones_f32": ([128, 128], F32),
    "ident_f32": ([128, 128], F32),
    "eps_1e-05": ([128, 1], F32),
    "eps_0.00064": ([128, 1], F32),
    "ones_bf": ([128, 128], BF16),
    "ident_bf": ([128, 128], BF16),
    "halfpi": ([128, 1], F32),
    "blockones_f32": ([128, 128], F32),
    "mask_su": ([64, 1, 64], F32),
    "mask_iu": ([64, 1, 64], F32),
    "mask_sl": ([64, 1, 64], F32),
    "ident64": ([64, 1, 64], F32),
    "rotA32": ([32, 32], F32),
    "rotI128": ([128, 128], F32),
    "freqA": ([128, 1], F32),
    "freqI": ([128, 1], F32),
    "pow2row": ([128, 24], F32),
}


def host_consts():
    import ml_dtypes
    d = {}
    d["ones_f32"] = np.ones((128, 128), np.float32)
    d["ident_f32"] = np.eye(128, dtype=np.float32)
    d["eps_1e-05"] = np.full((128, 1), 1e-5, np.float32)
    d["eps_0.00064"] = np.full((128, 1), 64e-5, np.float32)
    d["ones_bf"] = np.ones((128, 128), ml_dtypes.bfloat16)
    d["ident_bf"] = np.eye(128).astype(ml_dtypes.bfloat16)
    d["halfpi"] = np.full((128, 1), np.pi / 2, np.float32)
    bo = np.zeros((128, 128), np.float32)
    bo[:64, :64] = 1.0
    bo[64:, 64:] = 1.0
    d["blockones_f32"] = bo
    iu = np.arange(64)
    d["mask_su"] = (iu[:, None] < iu[None, :]).astype(np.float32).reshape(64, 1, 64)
    d["mask_iu"] = (iu[:, None] <= iu[None, :]).astype(np.float32).reshape(64, 1, 64)
    d["mask_sl"] = (iu[:, None] > iu[None, :]).astype(np.float32).reshape(64, 1, 64)
    d["ident64"] = np.eye(64, dtype=np.float32).reshape(64, 1, 64)
    ra = np.zeros((32, 32), np.float32)
    for m_ in range(16):
        ra[m_ + 16, m_] = -1.0
        ra[m_, m_ + 16] = 1.0
    d["rotA32"] = ra
    ri = np.zeros((128, 128), np.float32)
    for o_ in (0, 64):
        for m_ in range(8):
            ri[o_ + m_ + 8, o_ + m_] = -1.0
            ri[o_ + m_, o_ + m_ + 8] = 1.0
    d["rotI128"] = ri
    theta = np.float32(500000.0)
    fa = (theta ** (-np.arange(0, 32, 2, dtype=np.float32) / np.float32(32))).astype(np.float32)
    fi = (theta ** (-np.arange(0, 16, 2, dtype=np.float32) / np.float32(16))).astype(np.float32)
    fA = np.zeros((128, 1), np.float32)
    fA[0:16, 0] = fa
    fA[16:32, 0] = fa
    fI = np.zeros((128, 1), np.float32)
    for o_ in (0, 64):
        fI[o_:o_ + 8, 0] = fi
        fI[o_ + 8:o_ + 16, 0] = fi
    d["freqA"], d["freqI"] = fA, fI
    d["pow2row"] = np.tile((0.5 ** np.arange(1, 25, dtype=np.float64)).astype(np.float32)[None, :], (128, 1))
    return d


class EpiRelu2:
    def __init__(self, P, es, outT, tag, nbuf=3):
        self.outT = outT
        self.sq = Ring([P.sb(es, tag + "q%d" % i, [128, 512], F32) for i in range(nbuf)])
        self.ring = Ring([P.sb(es, tag + "e%d" % i, [128, 512], BF16) for i in range(nbuf)])

    def __call__(self, P, ps, c0, csz, t0, tsz):
        sq = self.sq.next()[0:csz, 0:tsz]
        o = self.ring.next()[0:csz, 0:tsz]
        P.act(sq, ps, AF.Square)
        P.stt("dve", o, ps, 0.0, sq, ALU.is_gt, ALU.mult)
        P.dma(STORE_Q, self.outT[c0:c0 + csz, t0:t0 + tsz], o)


class EpiGlu:
    def __init__(self, P, es, zT, bcol, outT, tag, nbuf=3):
        self.zT, self.outT, self.bcol = zT, outT, bcol
        self.z = Ring([P.sb(es, tag + "z%d" % i, [128, 512], F32) for i in range(nbuf)])
        self.sg = Ring([P.sb(es, tag + "s%d" % i, [128, 512], F32) for i in range(nbuf)])
        self.ring = Ring([P.sb(es, tag + "e%d" % i, [128, 512], BF16) for i in range(nbuf)])

    def __call__(self, P, ps, c0, csz, t0, tsz):
        z = self.z.next()[0:csz, 0:tsz]
        sg = self.sg.next()[0:csz, 0:tsz]
        o = self.ring.next()[0:csz, 0:tsz]
        P.dma("act", z, self.zT[c0:c0 + csz, t0:t0 + tsz])
        P.act(sg, ps, AF.Sigmoid, bias=self.bcol[0:csz, c0 // 128:c0 // 128 + 1])
        P.tt("dve", o, z, sg, ALU.mult)
        P.dma(STORE_Q, self.outT[c0:c0 + csz, t0:t0 + tsz], o)


def st_xattn_core(P, qT, kT, vtok, oT, S, consts, tag):
    H, M = 4, 256
    TT = 512
    sc = 128.0 ** -0.5
    with ExitStack() as es:
        k = P.sb(es, tag + "k", [128, H, M], BF16)
        v = P.sb(es, tag + "v", [128, 2, 512], BF16)
        P.dma("sp", k, kT.rearrange("(h d) m -> d h m", d=128))
        P.dma("sp", v, vtok.rearrange("(c m) n -> m c n", m=128))
        qs = [P.sb(es, tag + "q%d" % i, [128, H, TT], BF16) for i in range(2)]
        os_ = [P.sb(es, tag + "o%d" % i, [128, H, TT], BF16) for i in range(2)]
        pT = Ring([P.sb(es, tag + "p%d" % i, [128, 2, TT], BF16) for i in range(2)])
        rd = Ring([P.sb(es, tag + "rd%d" % i, [128, TT], F32) for i in range(2)])
        ps_s = Ring([P.ps(es, tag + "pss%d" % i, [128, TT], F32) for i in range(4)])
        ps_d = Ring([P.ps(es, tag + "psd%d" % i, [128, TT], F32) for i in range(2)])
        ps_o = Ring([P.ps(es, tag + "pso%d" % i, [128, TT], F32) for i in range(2)])
        ones = consts["ones_bf"]
        qv = qT.rearrange("(h d) s -> d h s", d=128)
        ov = oT.rearrange("(h d) s -> d h s", d=128)
        for it, t0 in enumerate(range(0, S, TT)):
            q = qs[it % 2]
            o = os_[it % 2]
            P.dma("sp", q, qv[:, :, t0:t0 + TT])
            for h in range(H):
                p = pT.next()
                for mc in range(2):
                    ps = ps_s.next()
                    P.mm(ps, k[:, h, mc * 128:(mc + 1) * 128], q[:, h, :])
                    P.act(p[:, mc, :], ps, AF.Exp, scale=sc)
                pd = ps_d.next()
                po = ps_o.next()
                for mc in range(2):
                    P.mm(pd, ones, p[:, mc, :], start=(mc == 0), stop=(mc == 1))
                for mc in range(2):
                    P.mm(po, v[:, mc, h * 128:(h + 1) * 128], p[:, mc, :], start=(mc == 0), stop=(mc == 1))
                r = rd.next()
                P.recip(r, pd)
                P.tt("dve", o[:, h, :], po, r, ALU.mult)
            P.dma("sp", ov[:, :, t0:t0 + TT], o)
        P.barrier()


def layer_tail(P, xT, memT, w, l, sc, S, consts, tag):
    D = 2048
    t = tag + "T"
    st_norm(P, xT, w["norm_xattn"][l], sc["xnT"], D, S, 1e-5, consts, t + "n2")
    st_norm(P, memT, w["norm_mem"][l], sc["memnT"], D, 256, 1e-5, consts, t + "nm")
    with ExitStack() as es:
        st_mm(P, sc["xnT"], D, S, [(w["xattn_wq"][l], 512, EpiStore(P, es, sc["qT"], BF16, t + "eq"))], consts, t + "mq", wscr=sc["wscr"])
    with ExitStack() as es:
        st_mm(P, sc["memnT"], D, 256, [(w["xattn_wkv"][l][:, 0:512], 512, EpiStore(P, es, sc["kT"], BF16, t + "ek"))],
              consts, t + "mk")
    with ExitStack() as es:
        st_mm(P, w["xattn_wkv"][l][:, 512:1024], D, 512, [(sc["memnT"], 256, EpiStore(P, es, sc["vtok"], BF16, t + "ev"))],
              consts, t + "mv", a_dt=F32, w_dt=BF16)
    st_xattn_core(P, sc["qT"], sc["kT"], sc["vtok"], sc["oT"], S, consts, t + "xc")
    with ExitStack() as es:
        st_mm(P, sc["oT"], 512, S, [(w["xattn_wo"][l], D, EpiResid(P, es, xT, t + "ro"))], consts, t + "mo", wscr=sc["wscr"])
    st_norm(P, xT, w["norm_ffn"][l], sc["xnT"], D, S, 1e-5, consts, t + "n3")
    with ExitStack() as es:
        st_mm(P, sc["xnT"], D, S, [(w["ffn_up"][l], 8192, EpiRelu2(P, es, sc["hT"], t + "eu"))], consts, t + "mu", wscr=sc["wscr"])
    with ExitStack() as es:
        st_mm(P, sc["hT"], 8192, S, [(w["ffn_down"][l], D, EpiResid(P, es, xT, t + "rd"))], consts, t + "md",
              TT=1024, CG=128, wscr=sc["wscr"])


TWO_PI = 2.0 * np.pi
CW1 = 6.28125
CW2 = TWO_PI - 6.28125


def sincos(P, ang, kq, r, sinv, cosv, consts, clamp_eng="dve"):
    npart = ang.shape[0]
    P.ts("dve", kq, ang, 1.0 / TWO_PI, ALU.mult)
    P.stt("dve", r, kq, -CW1, ang, ALU.mult, ALU.add)
    P.stt("dve", r, kq, -CW2, r, ALU.mult, ALU.add)
    P.ts(clamp_eng, r, r, float(np.pi), ALU.min, -float(np.pi), ALU.max)
    P.act(sinv, r, AF.Sin)
    P.act(r, r, AF.Abs)
    hp = consts["halfpi"][0:npart, :]
    P.act(cosv, r, AF.Sin, bias=hp, scale=-1.0)


def st_s5_core(P, uT, zT, w, i, S, consts, tag):
    TB = min(1024, S)
    NB = TB // 512
    with ExitStack() as es:
        sb = lambda n, sh, dt=F32: P.sb(es, tag + n, sh, dt)
        lamre, lamim, logdt = sb("lamre", [128, 64]), sb("lamim", [128, 64]), sb("logdt", [128, 64])
        P.dma("sp", lamre, w["s5_lamre_pc"][i])
        P.dma("sp", lamim, w["s5_lamim_pc"][i])
        P.dma("sp", logdt, w["s5_logdt_pc"][i])
        dcol, = [sb("dcol", [128, 16])]
        P.dma("sp", dcol, w["s5_d_pc"][i])
        lre, dt, rho, th, th2 = sb("lre", [128, 64]), sb("dt", [128, 64]), sb("rho", [128, 64]), sb("th", [128, 64]), sb("th2", [128, 64])
        P.ts("dve", lre, lamre, -1e-4, ALU.min)
        P.act(dt, logdt, AF.Exp)
        P.tt("dve", th2, lre, dt, ALU.mult)
        P.act(rho, th2, AF.Exp)
        P.tt("dve", th, lamim, dt, ALU.mult)
        kq0, r0, sth, cth = sb("kq0", [128, 64], I32), sb("r0", [128, 64]), sb("sth", [128, 64]), sb("cth", [128, 64])
        sincos(P, th, kq0, r0, sth, cth, consts)
        cr, ci, den, t1, t2 = sb("cr", [128, 64]), sb("ci", [128, 64]), sb("den", [128, 64]), sb("t1", [128, 64]), sb("t2", [128, 64])
        qr, qi, nqi = sb("qr", [128, 64]), sb("qi", [128, 64]), sb("nqi", [128, 64])
        P.tt("dve", cr, rho, cth, ALU.mult)
        P.ts("dve", cr, cr, -1.0, ALU.add)
        P.tt("dve", ci, rho, sth, ALU.mult)
        P.tt("dve", den, lre, lre, ALU.mult)
        P.tt("dve", t1, lamim, lamim, ALU.mult)
        P.tt("dve", den, den, t1, ALU.add)
        P.recip(den, den)
        P.tt("dve", t1, cr, lre, ALU.mult)
        P.tt("dve", t2, ci, lamim, ALU.mult)
        P.tt("dve", t1, t1, t2, ALU.add)
        P.tt("dve", qr, t1, den, ALU.mult)
        P.tt("dve", t1, ci, lre, ALU.mult)
        P.tt("dve", t2, cr, lamim, ALU.mult)
        P.tt("dve", t1, t1, t2, ALU.subtract)
        P.tt("dve", qi, t1, den, ALU.mult)
        P.ts("dve", nqi, qi, -1.0, ALU.mult)
        tfull = sb("tfull", [128, S])
        with ExitStack() as es2:
            tfi = P.sb(es2, tag + "tfi", [128, S], I32)
            P.I("pool", "iota", out=tfi, pattern=[[1, S]], base=0, channel_multiplier=0)
            P.copy("dve", tfull, tfi)
            P.barrier()
        f4 = lambda n: sb(n, [128, TB])
        bpad = [[sb("bp%d%d" % (a_, b_), [128, 128]) for b_ in range(2)] for a_ in range(2)]
        cpad = [[sb("cp%d%d" % (a_, b_), [128, 128]) for b_ in range(2)] for a_ in range(2)]
        bbf = [[[sb("bb%d_%d_%d" % (q, g, b_), [128, 128], BF16) for b_ in range(2)] for g in range(4)] for q in range(2)]
        cbf = [[[sb("cb%d_%d_%d" % (q, g, b_), [128, 128], BF16) for b_ in range(2)] for g in range(4)] for q in range(2)]
        state = [[[sb("st%d_%d_%d" % (q, g, b_), [128, 1]) for b_ in range(2)] for g in range(4)] for q in range(2)]
        ctmp = sb("ctmp", [128, 128])
        ubf = [sb("ubf%d" % k, [128, TB], BF16) for k in range(2)]
        uf = [sb("uf%d" % k, [128, TB]) for k in range(2)]
        ang2, kq2 = [f4("angq%d" % q) for q in range(2)], [sb("kqq%d" % q, [128, TB], I32) for q in range(2)]
        tabs3, tabc3 = [f4("tabs%d" % q) for q in range(3)], [f4("tabc%d" % q) for q in range(3)]
        raw2 = [[f4("raw%d_%d" % (q, b_)) for b_ in range(2)] for q in range(2)]
        mW, mX = [f4("mW%d" % k) for k in range(4)], [f4("mX%d" % k) for k in range(4)]
        wr = [f4("w%d" % b_) for b_ in range(2)]
        xh2 = [[f4("xh%d_%d" % (q, b_)) for b_ in range(2)] for q in range(2)]
        X = [[sb("X%d_%d" % (k, b_), [128, TB], BF16) for b_ in range(2)] for k in range(2)]
        yv, x2, zz = f4("yv"), f4("x2"), [f4("zz%d" % k) for k in range(2)]
        ps_raw = [[P.ps(es, tag + "pr%d_%d" % (b_, n), [128, 512], F32) for n in range(NB)] for b_ in range(2)]
        ps_y = [P.ps(es, tag + "py%d" % n, [128, 512], F32) for n in range(NB)]
        NTB = S // TB
        iters = [(ct, tbi, g) for ct in range(16) for tbi in range(NTB) for g in range(4)]

        def prep_ct(ct):
            q = ct % 2
            for g in range(4):
                gp = ct * 4 + g
                bp, cp = bpad[g % 2], cpad[g % 2]
                P.dma("sp", bp[0], w["s5_bre_pad"][i, gp])
                P.dma("act", bp[1], w["s5_bim_pad"][i, gp])
                P.dma("sp", cp[0], w["s5_cre_pad"][i, gp])
                P.dma("act", cp[1], w["s5_cim_pad"][i, gp])
                P.copy("pool", bbf[q][g][0], bp[0])
                P.copy("pool", bbf[q][g][1], bp[1])
                P.ts("dve", ctmp, cp[1], qi[:, gp:gp + 1], ALU.mult)
                P.stt("dve", cbf[q][g][0], cp[0], qr[:, gp:gp + 1], ctmp, ALU.mult, ALU.subtract)
                P.ts("dve", ctmp, cp[1], qr[:, gp:gp + 1], ALU.mult)
                P.stt("dve", cbf[q][g][1], cp[0], nqi[:, gp:gp + 1], ctmp, ALU.mult, ALU.subtract)
                P.memset("dve", state[q][g][0], 0.0)
                P.memset("dve", state[q][g][1], 0.0)

        def stA(k):
            ct, tbi, g = iters[k]
            t0 = tbi * TB
            gp = ct * 4 + g
            ui = (ct * NTB + tbi) % 2
            if tbi == 0 and g == 0:
                prep_ct(ct)
            if g == 0:
                P.dma("sp", uf[ui], uT[ct * 128:(ct + 1) * 128, t0:t0 + TB])
                P.copy("act", ubf[ui], uf[ui])
            u_b = ubf[ui]
            for b_ in range(2):
                for n in range(NB):
                    P.mm(ps_raw[b_][n], bbf[ct % 2][g][b_], u_b[:, n * 512:(n + 1) * 512])
            ang, kq = ang2[k % 2], kq2[k % 2]
            P.act(ang, tfull[:, t0:t0 + TB], AF.Copy, scale=th[:, gp:gp + 1])
            sincos(P, ang, kq, ang, tabs3[k % 3], tabc3[k % 3], consts)
            raw = raw2[k % 2]
            for b_ in range(2):
                for n in range(NB):
                    P.copy("act", raw[b_][:, n * 512:(n + 1) * 512], ps_raw[b_][n])

        def stB(k):
            ct, tbi, g = iters[k]
            gp = ct * 4 + g
            tabs, tabc, raw, xh = tabs3[k % 3], tabc3[k % 3], raw2[k % 2], xh2[k % 2]
            P.tt("pool", mW[0], tabc, raw[0], ALU.mult)
            P.tt("pool", mW[1], tabs, raw[1], ALU.mult)
            P.tt("pool", mW[2], tabc, raw[1], ALU.mult)
            P.tt("dve", mW[3], tabs, raw[0], ALU.mult)
            P.tt("dve", wr[0], mW[0], mW[1], ALU.add)
            P.tt("dve", wr[1], mW[2], mW[3], ALU.subtract)
            rb = rho[:, gp:gp + 1].broadcast_to([128, TB])
            st_ = state[ct % 2][g]
            for b_ in range(2):
                P.scan(xh[b_], rb, wr[b_], st_[b_], ALU.mult, ALU.add)
                P.copy("dve", st_[b_], xh[b_][:, TB - 1:TB])

        def stC(k):
            ct, tbi, g = iters[k]
            t0 = tbi * TB
            ui = (ct * NTB + tbi) % 2
            tabs, tabc, xh = tabs3[k % 3], tabc3[k % 3], xh2[k % 2]
            P.tt("pool", mX[0], tabc, xh[0], ALU.mult)
            P.tt("pool", mX[1], tabs, xh[1], ALU.mult)
            P.tt("pool", mX[2], tabs, xh[0], ALU.mult)
            P.tt("dve", mX[3], tabc, xh[1], ALU.mult)
            Xg = X[k % 2]
            P.tt("dve", Xg[0], mX[0], mX[1], ALU.subtract)
            P.tt("dve", Xg[1], mX[2], mX[3], ALU.add)
            cb = cbf[ct % 2][g]
            for n in range(NB):
                P.mm(ps_y[n], cb[0], Xg[0][:, n * 512:(n + 1) * 512], start=(g == 0), stop=False)
                P.mm(ps_y[n], cb[1], Xg[1][:, n * 512:(n + 1) * 512], start=False, stop=(g == 3))
            if g == 3:
                u_f = uf[ui]
                for n in range(NB):
                    sl = slice(n * 512, (n + 1) * 512)
                    P.stt("dve", yv[:, sl], u_f[:, sl], dcol[:, ct:ct + 1], ps_y[n], ALU.mult, ALU.add)
                z = zz[(ct * NTB + tbi) % 2]
                P.act(x2, yv, AF.Square)
                P.ts("dve", x2, x2, 0.044715, ALU.mult, 1.0, ALU.add)
                P.tt("dve", x2, x2, yv, ALU.mult)
                P.act(x2, x2, AF.Sigmoid, scale=2.0 * float(np.sqrt(2.0 / np.pi)))
                P.tt("dve", z, yv, x2, ALU.mult)
                P.dma("sp", zT[ct * 128:(ct + 1) * 128, t0:t0 + TB], z)

        NI = len(iters)
        stA(0)
        if NI > 1:
            stA(1)
        stB(0)
        for k in range(NI):
            if k + 2 < NI:
                stA(k + 2)
            if k + 1 < NI:
                stB(k + 1)
            stC(k)
        P.barrier()


def odd_layer(P, xT, memT, w, l, sc, S, consts, tag):
    i = l // 2
    D = 2048
    st_norm(P, xT, w["norm_mix"][l], sc["xnT"], D, S, 1e-5, consts, tag + "n1")
    with ExitStack() as es:
        st_mm(P, sc["xnT"], D, S, [(w["odd_w_in"][i], D, EpiStore(P, es, sc["uT"], F32, tag + "eu1"))], consts, tag + "mi", wscr=sc["wscr"])
    st_s5_core(P, sc["uT"], sc["zT"], w, i, S, consts, tag + "s5")
    with ExitStack() as es:
        bcol = P.sb(es, tag + "bglu", [128, 16], F32)
        P.dma("sp", bcol, w["s5_bglu_pc"][i])
        st_mm(P, sc["zT"], D, S, [(w["s5_w_glu"][i], D, EpiGlu(P, es, sc["zT"], bcol, sc["xnT"], tag + "eg"))], consts,
              tag + "mg", a_dt=F32, TT=512, wscr=sc["wscr"])
    with ExitStack() as es:
        st_mm(P, sc["xnT"], D, S, [(w["odd_w_out"][i], D, EpiResid(P, es, xT, tag + "ro1"))], consts, tag + "mo1", wscr=sc["wscr"])
    layer_tail(P, xT, memT, w, l, sc, S, consts, tag)


def host_layout_s5(inp):
    o = {}
    n = inp["s5_lam_re"].shape[0]

    def pc(a):
        return np.ascontiguousarray(a.reshape(n, 64, 2, 64).transpose(0, 2, 3, 1).reshape(n, 128, 64))

    o["s5_lamre_pc"] = pc(inp["s5_lam_re"])
    o["s5_lamim_pc"] = pc(inp["s5_lam_im"])
    o["s5_logdt_pc"] = pc(np.repeat(inp["s5_log_dt"][:, :, None], 64, axis=2))
    bpad = np.zeros((2, n, 64, 128, 128), np.float32)
    cpad = np.zeros((2, n, 64, 128, 128), np.float32)
    for k, (bn, cn) in enumerate((("s5_b_re", "s5_c_re"), ("s5_b_im", "s5_c_im"))):
        b = inp[bn].reshape(n, 64, 2, 64, 16)
        c = inp[cn].reshape(n, 64, 2, 16, 64)
        for gp in range(64):
            gq = gp % 4
            for gl in range(2):
                r0 = gq * 32 + gl * 16
                bpad[k, :, gp, r0:r0 + 16, gl * 64:(gl + 1) * 64] = b[:, gp, gl].transpose(0, 2, 1)
                cpad[k, :, gp, gl * 64:(gl + 1) * 64, r0:r0 + 16] = c[:, gp, gl].transpose(0, 2, 1)
    o["s5_bre_pad"], o["s5_bim_pad"] = bpad[0], bpad[1]
    o["s5_cre_pad"], o["s5_cim_pad"] = cpad[0], cpad[1]
    o["s5_d_pc"] = colvec(inp["s5_d"])
    o["s5_bglu_pc"] = colvec(inp["s5_b_glu"])
    return o


def colvec(a, pp=128):
    sh = a.shape
    return np.ascontiguousarray(np.swapaxes(a.reshape(sh[:-1] + (sh[-1] // pp, pp)), -1, -2))


KAPPA = float(np.exp(-0.5))
RW_COLS = ["rwkv_w0", "rwkv_a0", "rwkv_k_k", "rwkv_k_a", "rwkv_r_k", "rwkv_ln_w", "rwkv_ln_b"]


def st_rwkv_prep(P, hBT, w, i, sc, S, consts, tag):
    TB = 256
    NJ = 8
    with ExitStack() as es:
        sb = lambda n, sh, dt=F32: P.sb(es, tag + n, sh, dt)
        cols = sb("cols", [128, 7, NJ, 1])
        P.dma("sp", cols, w["rwkv_cols"][i])
        w0c, a0c, kkc, kac, rkc = [cols[:, k] for k in range(5)]
        omka = sb("omka", [128, NJ, 1])
        P.ts("dve", omka, kac, -1.0, ALU.mult, 1.0, ALU.add)
        mu3 = sb("mu3", [128, 3, NJ, 1])
        P.dma("sp", mu3, w["rwkv_mu_rkv"][i])
        muw, mua, mug1, mug2 = sb("muw", [64, 1]), sb("mua", [64, 1]), sb("mug1", [128, 1]), sb("mug2", [32, 1])
        P.dma("sp", muw, w["rwkv_mu_wl"][i])
        P.dma("sp", mua, w["rwkv_mu_al"][i])
        P.dma("sp", mug1, w["rwkv_mu_g1"][i])
        P.dma("sp", mug2, w["rwkv_mu_g2"][i])
        w2f, a2f, g2f1, g2f2 = sb("w2f", [64, 1024]), sb("a2f", [64, 1024]), sb("g2f1", [128, 1024]), sb("g2f2", [32, 1024])
        w2b, a2b, g2b1, g2b2 = sb("w2b", [64, 1024], BF16), sb("a2b", [64, 1024], BF16), sb("g2b1", [128, 1024], BF16), sb("g2b2", [32, 1024], BF16)
        P.dma("sp", w2f, w["rwkv_w2"][i])
        P.dma("sp", a2f, w["rwkv_a2"][i])
        P.dma("sp", g2f1, w["rwkv_g2"][i][0:128, :])
        P.dma("sp", g2f2, w["rwkv_g2"][i][128:160, :])
        for f, b in ((w2f, w2b), (a2f, a2b), (g2f1, g2b1), (g2f2, g2b2)):
            P.copy("act", b, f)
        cmask = sb("cmask", [128, NJ, TB])
        P.memset("dve", cmask, 1.0)
        P.memset("dve", cmask.rearrange("p j (c t) -> p j c t", t=64)[:, :, :, 0:1], 0.0)
        bones = consts["blockones_f32"]
        A3, B3 = sb("A3", [128, NJ, TB]), sb("B3", [128, NJ, TB])
        hmr, hmk, hmv = sb("hmr", [128, NJ, TB]), sb("hmk", [128, NJ, TB]), sb("hmv", [128, NJ, TB])
        As, Bs = sb("As", [128, TB]), sb("Bs", [128, TB])
        twl, alb, sg1, sg2 = sb("twl", [64, TB], BF16), sb("alb", [64, TB], BF16), sb("sg1", [128, TB], BF16), sb("sg2", [32, TB], BF16)
        sig, av, gv = sb("sig", [128, NJ, TB]), sb("av", [128, NJ, TB]), sb("gv", [128, NJ, TB])
        cs, e1, e2, e3 = sb("cs", [128, NJ, TB]), sb("e1", [128, NJ, TB]), sb("e2", [128, NJ, TB]), sb("e3", [128, NJ, TB])
        kraw, kk, kp, tmp = sb("kraw", [128, NJ, TB]), sb("kk", [128, NJ, TB]), sb("kp", [128, NJ, TB]), sb("tmp", [128, NJ, TB])
        oA, oB, oK, oR, oV = [sb("o" + n, [128, NJ, TB], BF16) for n in "ABKRV"]
        pcs = sb("pcs", [128, NJ, TB // 64])
        ps = Ring([P.ps(es, tag + "ps%d" % k, [128, TB], F32) for k in range(6)])
        hv = lambda r0: hBT[r0:r0 + 1024, :].rearrange("(j p) s -> p j s", p=128)
        out3 = lambda T_: T_.rearrange("(j p) s -> p j s", p=128)

        def shift3(view, dst, mu_b, t0):
            P.dma("sp", A3, view[:, :, t0:t0 + TB])
            if t0 == 0:
                P.memset("dve", B3[:, :, 0:1], 0.0)
                P.dma("act", B3[:, :, 1:TB], view[:, :, 0:TB - 1])
            else:
                P.dma("act", B3, view[:, :, t0 - 1:t0 + TB - 1])
            P.tt("dve", B3, B3, A3, ALU.subtract)
            P.tt("pool", B3, B3, mu_b.broadcast_to([128, NJ, TB]), ALU.mult)
            P.tt("dve", dst, B3, A3, ALU.add)

        def shift2(r0, nr, mu_c, t0, func, dst):
            a, b = As[0:nr, :], Bs[0:nr, :]
            P.dma("sp", a, hBT[r0:r0 + nr, t0:t0 + TB])
            if t0 == 0:
                P.memset("dve", b[:, 0:1], 0.0)
                P.dma("act", b[:, 1:TB], hBT[r0:r0 + nr, 0:TB - 1])
            else:
                P.dma("act", b, hBT[r0:r0 + nr, t0 - 1:t0 + TB - 1])
            P.tt("dve", b, b, a, ALU.subtract)
            P.stt("dve", b, b, mu_c, a, ALU.mult, ALU.add)
            P.act(dst, b, func)

        for t0 in range(0, S, TB):
            shift3(hv(0), hmr, mu3[:, 0], t0)
            shift3(hv(1024), hmk, mu3[:, 1], t0)
            shift3(hv(2048), hmv, mu3[:, 2], t0)
            shift2(3072, 64, muw, t0, AF.Tanh, twl)
            shift2(3136, 64, mua, t0, AF.Copy, alb)
            shift2(3200, 128, mug1, t0, AF.Sigmoid, sg1)
            shift2(3328, 32, mug2, t0, AF.Sigmoid, sg2)
            for j in range(NJ):
                cj = slice(j * 128, (j + 1) * 128)
                p1 = ps.next()
                P.mm(p1, w2b[:, cj], twl)
                P.act(sig[:, j, :], p1, AF.Sigmoid, bias=w0c[:, j, :])
                p2 = ps.next()
                P.mm(p2, a2b[:, cj], alb)
                P.act(av[:, j, :], p2, AF.Sigmoid, bias=a0c[:, j, :])
                p3 = ps.next()
                P.mm(p3, g2b1[:, cj], sg1, start=True, stop=False)
                P.mm(p3, g2b2[:, cj], sg2, start=False, stop=True)
                P.copy("dve", gv[:, j, :], p3)
            P.dma("sp", out3(sc["gT"])[:, :, t0:t0 + TB], gv)
            P.scan(cs.rearrange("p j t -> p (j t)"), cmask.rearrange("p j t -> p (j t)"), sig.rearrange("p j t -> p (j t)"),
                   0.0, ALU.mult, ALU.add)
            P.tt("dve", tmp, cs, sig, ALU.subtract)
            P.act(e1, tmp, AF.Exp, scale=-KAPPA)
            P.act(e2, cs, AF.Exp, scale=KAPPA)
            P.act(e3, cs, AF.Exp, scale=-KAPPA)
            P.copy("dve", pcs, e3.rearrange("p j (c t) -> p j c t", t=64)[:, :, :, 63])
            P.dma("sp", out3(sc["PCT"])[:, :, t0 // 64:(t0 + TB) // 64], pcs)
            P.tt("pool", kraw, hmk, kkc.broadcast_to([128, NJ, TB]), ALU.mult)
            P.act(tmp, kraw, AF.Square)
            for j in range(NJ):
                p1 = ps.next()
                P.mm(p1, bones, tmp[:, j, :])
                P.ts("dve", kk[:, j, :], p1, 1e-24, ALU.max)
            P.act(kk, kk, AF.Sqrt)
            P.recip(kk, kk)
            P.tt("dve", kk, kk, kraw, ALU.mult)
            P.tt("pool", tmp, av, kac.broadcast_to([128, NJ, TB]), ALU.mult)
            P.tt("pool", tmp, tmp, omka.broadcast_to([128, NJ, TB]), ALU.add)
            P.tt("dve", kp, hmk, tmp, ALU.mult)
            P.stt("dve", oA, kk, -1.0, e1, ALU.mult, ALU.mult)
            P.tt("pool", tmp, kk, av, ALU.mult)
            P.tt("dve", oB, tmp, e2, ALU.mult)
            P.tt("dve", oK, kp, e2, ALU.mult)
            P.tt("dve", oR, hmr, e3, ALU.mult)
            P.copy("act", oV, hmv)
            for nm, o in (("AhT", oA), ("BhT", oB), ("KhT", oK), ("RhT", oR), ("vT", oV)):
                P.dma("sp", out3(sc[nm])[:, :, t0:t0 + TB], o)
            P.tt("pool", tmp, hmr, kp, ALU.mult)
            P.tt("pool", tmp, tmp, rkc.broadcast_to([128, NJ, TB]), ALU.mult)
            for j in range(NJ):
                p1 = ps.next()
                P.mm(p1, bones, tmp[:, j, :])
                P.tt("dve", e1[:, j, :], p1, hmv[:, j, :], ALU.mult)
            P.dma("sp", out3(sc["bonusT"])[:, :, t0:t0 + TB], e1)
        P.barrier()


def st_rwkv_A(P, sc, S, consts, tag):
    CB = 4
    NCH = S // 64
    with ExitStack() as es:
        sb = lambda n, sh, dt=F32: P.sb(es, tag + n, sh, dt)
        names = ["AhT", "BhT", "KhT", "RhT", "vT"]
        blk = [[sb("bl%d_%d" % (k, q), [64, 16, CB * 64], BF16) for q in range(5)] for k in range(2)]
        msu = consts["mask_su"].broadcast_to([64, 16, 64])
        miu = consts["mask_iu"].broadcast_to([64, 16, 64])
        msl = consts["mask_sl"].broadcast_to([64, 16, 64])
        idb = consts["ident64"].broadcast_to([64, 16, 64])
        identb = consts["ident_bf"]
        N32s = [sb("N32_%d" % q, [64, 16, 64]) for q in range(2)]
        T32s = [sb("T32_%d" % q, [64, 16, 64]) for q in range(2)]
        Nbs = [[sb("Nb%d_%d" % (q, k), [64, 16, 64], BF16) for k in range(2)] for q in range(2)]
        NTbs = [[sb("NTb%d_%d" % (q, k), [64, 16, 64], BF16) for k in range(2)] for q in range(2)]
        Tbs = [sb("Tb%d" % q, [64, 16, 64], BF16) for q in range(2)]
        mats = [sb("mats%d" % k, [64, 4, 16, 64], BF16) for k in range(2)]
        tok = [sb("tok%d" % k, [64, 3, 16, 64], BF16) for k in range(2)]
        pr = Ring([P.ps(es, tag + "pr%d" % k, [64, 16, 64], F32) for k in range(3)])
        pt = Ring([P.ps(es, tag + "pt%d" % k, [64, 16, 64], BF16) for k in range(2)])
        view = lambda nm: sc[nm].rearrange("(h c) s -> c h s", c=64)

        def mmh(out, L, R):
            for h in range(16):
                P.mm(out[:, h, :], L[:, h, :], R[:, h, :])

        def chunk_gen(c, bl):
            q = c % 2
            N32, T32, Nb, NTb, Tb = N32s[q], T32s[q], Nbs[q], NTbs[q], Tbs[q]
            cc = slice((c % CB) * 64, (c % CB + 1) * 64)
            Ah, Bh, Kh, Rh, Vh = [bl[k][:, :, cc] for k in range(5)]
            mt = mats[q]
            tk = tok[q]
            p = pr.next()
            mmh(p, Ah, Bh)
            P.tt("dve", NTb[0], p, msl, ALU.mult)
            yield
            p = pr.next()
            mmh(p, Bh, Ah)
            P.tt("dve", N32, p, msu, ALU.mult)
            P.copy("act", Nb[0], N32)
            P.tt("pool", T32, N32, idb, ALU.add)
            P.copy("act", Tb, T32)
            yield
            p = pr.next()
            mmh(p, Kh, Ah)
            P.tt("dve", mt[:, 1], p, msu, ALU.mult)
            yield
            p = pr.next()
            mmh(p, Bh, Rh)
            P.tt("dve", mt[:, 2], p, miu, ALU.mult)
            yield
            p = pr.next()
            mmh(p, Kh, Rh)
            P.tt("dve", mt[:, 3], p, miu, ALU.mult)
            yield
            cur = 0
            srcs = (Bh, Kh, Vh)
            for j in range(1, 6):
                nxt = 1 - cur
                p = pr.next()
                mmh(p, Nb[cur], NTb[cur])
                P.copy("dve", NTb[nxt], p)
                yield
                if j < 5:
                    p = pr.next()
                    mmh(p, NTb[cur], Nb[cur])
                    P.copy("act", Nb[nxt], p)
                    yield
                if j <= 3:
                    tp = pt.next()
                    for h in range(16):
                        P.tr(tp[:, h, :], srcs[j - 1][:, h, :], identb[0:64, 0:64])
                    P.copy("act", tk[:, j - 1], tp)
                    yield
                p = pr.next()
                mmh(p, NTb[nxt], Tb)
                P.tt("dve", T32, T32, p, ALU.add)
                if j < 5:
                    P.copy("act", Tb, T32)
                yield
                cur = nxt
            P.copy("act", mt[:, 0], T32)
            P.dma("sp", sc["mats"][c], mt)
            P.dma("act", sc["tok3"][c], tk)
            yield

        for c0 in range(0, NCH, 2):
            if c0 % CB == 0:
                bl = blk[(c0 // CB) % 2]
                for k, nm in enumerate(names):
                    P.dma("sp" if k % 2 == 0 else "act", bl[k], view(nm)[:, :, c0 * 64:(c0 + CB) * 64])
            gens = [chunk_gen(c0, bl)]
            if c0 + 1 < NCH:
                gens.append(chunk_gen(c0 + 1, bl))
            alive = True
            while alive:
                alive = False
                for g in gens:
                    try:
                        next(g)
                        alive = True
                    except StopIteration:
                        pass
        P.barrier()


def st_rwkv_B(P, sc, S, consts, tag):
    CB = 4
    NCH = S // 64
    with ExitStack() as es:
        sb = lambda n, sh, dt=F32: P.sb(es, tag + n, sh, dt)
        blk = [[sb("bl%d_%d" % (k, q), [64, 16, CB * 64], BF16) for q in range(2)] for k in range(2)]
        pc = sb("pc", [64, 16, NCH])
        P.dma("sp", pc, sc["PCT"].rearrange("(h c) n -> c h n", c=64))
        mats = [sb("mats%d" % k, [64, 4, 16, 64], BF16) for k in range(3)]
        tok = [sb("tok%d" % k, [64, 3, 16, 64], BF16) for k in range(3)]
        S32, Sb = sb("S32", [64, 16, 64]), sb("Sb", [64, 16, 64], BF16)
        tmp = sb("tmp", [64, 16, 64])
        WTb = sb("WTb", [64, 16, 64], BF16)
        UTb = sb("UTb", [64, 16, 64], BF16)
        O = [sb("O%d" % k, [64, 16, 64]) for k in range(2)]
        P.memset("dve", S32, 0.0)
        P.memset("dve", Sb, 0.0)
        pW, pU, pO, pS = [P.ps(es, tag + n, [64, 16, 64], F32) for n in ("pW", "pU", "pO", "pS")]
        view = lambda nm: sc[nm].rearrange("(h c) s -> c h s", c=64)
        otok = sc["otok"].rearrange("(n t) (h v) -> n t h v", t=64, v=64)
        for c in range(NCH):
            if c % CB == 0:
                bl = blk[(c // CB) % 2]
                P.dma("sp", bl[0], view("AhT")[:, :, c * 64:(c + CB) * 64])
                P.dma("sp", bl[1], view("RhT")[:, :, c * 64:(c + CB) * 64])
            cc = slice((c % CB) * 64, (c % CB + 1) * 64)
            Ah, Rh = bl[0][:, :, cc], bl[1][:, :, cc]
            mt, tk = mats[c % 3], tok[c % 3]
            P.dma("sp", mt, sc["mats"][c])
            P.dma("act", tk, sc["tok3"][c])
            Tm, Nka, Mbr, Mkr = [mt[:, q] for q in range(4)]
            bt, kt, vt = [tk[:, q] for q in range(3)]
            for h in range(16):
                P.mm(pW[:, h, :], Ah[:, h, :], Sb[:, h, :], start=True, stop=False)
                P.mm(pW[:, h, :], Nka[:, h, :], vt[:, h, :], start=False, stop=True)
            P.copy("act", WTb, pW)
            for h in range(16):
                P.mm(pU[:, h, :], Tm[:, h, :], WTb[:, h, :])
            P.copy("act", UTb, pU)
            for h in range(16):
                P.mm(pO[:, h, :], Rh[:, h, :], Sb[:, h, :], start=True, stop=False)
                P.mm(pO[:, h, :], Mbr[:, h, :], UTb[:, h, :], start=False, stop=False)
                P.mm(pO[:, h, :], Mkr[:, h, :], vt[:, h, :], start=False, stop=True)
            o = O[c % 2]
            P.copy("act", o, pO)
            P.dma("sp", otok[c], o)
            for h in range(16):
                P.mm(pS[:, h, :], bt[:, h, :], UTb[:, h, :], start=True, stop=False)
                P.mm(pS[:, h, :], kt[:, h, :], vt[:, h, :], start=False, stop=True)
            P.tt("dve", tmp, pS, S32, ALU.add)
            P.tt("dve", S32, tmp, pc[:, :, c:c + 1].broadcast_to([64, 16, 64]), ALU.mult)
            P.copy("act", Sb, S32)
        P.barrier()


def st_rwkv_post(P, w, i, sc, S, consts, tag):
    TB = min(512, S)
    with ExitStack() as es:
        sb = lambda n, sh, dt=F32: P.sb(es, tag + n, sh, dt)
        cols = sb("cols", [128, 7, 8, 1])
        P.dma("sp", cols, w["rwkv_cols"][i])
        lnw, lnb = cols[:, 5], cols[:, 6]
        bon = [sb("bon%d" % k, [128, 8, TB]) for k in range(2)]
        gt = [sb("g%d" % k, [128, 8, TB]) for k in range(2)]
        yo = [sb("yo%d" % k, [128, 8, TB], BF16) for k in range(2)]
        ot = [sb("ot%d" % k, [128, 16, 64]) for k in range(2)]
        xc, sq = sb("xc", [128, 16, 64]), sb("sq", [128, 16, 64])
        sm, vr = sb("sm", [128, 16, 1]), sb("vr", [128, 16, 1])
        y1 = sb("y1", [128, 128])
        pT = Ring([P.ps(es, tag + "pT%d" % k, [128, 128], F32) for k in range(4)])
        identf = consts["ident_f32"]
        eps = consts["eps_0.00064"]
        f3 = lambda T_: T_.rearrange("(j p) s -> p j s", p=128)
        yv = sc["yT"][1024:2048, :].rearrange("(j p) s -> p j s", p=128)
        otv = sc["otok"].rearrange("(n t) (h v) -> n t h v", t=128, v=64)
        for ib, t0 in enumerate(range(0, S, TB)):
            b_, g_, y_ = bon[ib % 2], gt[ib % 2], yo[ib % 2]
            P.dma("sp", b_, f3(sc["bonusT"])[:, :, t0:t0 + TB])
            P.dma("act", g_, f3(sc["gT"])[:, :, t0:t0 + TB])
            for k in range(TB // 128):
                n = (t0 // 128) + k
                o = ot[n % 2]
                P.dma("sp", o, otv[n])
                P.reduce(sm.rearrange("p h o -> p (h o)"), o, ALU.add)
                P.ts("dve", sm, sm, 1.0 / 64, ALU.mult)
                P.tt("dve", xc, o, sm.broadcast_to([128, 16, 64]), ALU.subtract)
                P.act(sq, xc, AF.Square)
                P.reduce(vr.rearrange("p h o -> p (h o)"), sq, ALU.add)
                P.act(vr, vr, AF.Sqrt, bias=eps, scale=1.0 / 64)
                P.recip(vr, vr)
                P.tt("dve", xc, xc, vr.broadcast_to([128, 16, 64]), ALU.mult)
                for j in range(8):
                    p = pT.next()
                    P.tr(p, xc[:, 2 * j:2 * j + 2, :].rearrange("p a v -> p (a v)"), identf)
                    ts_ = slice(k * 128, (k + 1) * 128)
                    P.ts("dve", y1, p, lnw[:, j, :], ALU.mult, lnb[:, j, :], ALU.add)
                    P.tt("pool", y1, y1, b_[:, j, ts_], ALU.add)
                    P.tt("dve", y_[:, j, ts_], y1, g_[:, j, ts_], ALU.mult)
            P.dma("sp", yv[:, :, t0:t0 + TB], y_)
        P.barrier()


def host_layout_rwkv(inp):
    o = {}
    n = inp["rwkv_mu"].shape[0]
    mu = inp["rwkv_mu"]
    o["rwkv_mu_rkv"] = np.ascontiguousarray(np.stack([colvec(mu[:, k * 1024:(k + 1) * 1024]) for k in range(3)], 2)[..., None])
    o["rwkv_mu_wl"] = np.ascontiguousarray(mu[:, 3072:3136, None])
    o["rwkv_mu_al"] = np.ascontiguousarray(mu[:, 3136:3200, None])
    o["rwkv_mu_g1"] = np.ascontiguousarray(mu[:, 3200:3328, None])
    o["rwkv_mu_g2"] = np.ascontiguousarray(mu[:, 3328:3360, None])
    o["rwkv_cols"] = np.ascontiguousarray(np.stack([colvec(inp[k].reshape(n, 1024)) for k in RW_COLS], 2)[..., None])
    for k in ("rwkv_w2", "rwkv_a2", "rwkv_g2"):
        o[k] = inp[k]
    return o


def rwkv_scratch(nc, S, pre="", kind="Internal"):
    NCH = S // 64
    sc = {}
    _dram = dram
    dram_ = lambda nc_, n_, sh_, dt_: _dram(nc_, n_, sh_, dt_, kind)
    for nm in ("AhT", "BhT", "KhT", "RhT", "vT"):
        sc[nm] = dram_(nc, pre + nm, [1024, S], BF16)
    sc["PCT"] = dram_(nc, pre + "PCT", [1024, NCH], F32)
    sc["gT"] = dram_(nc, pre + "gT", [1024, S], F32)
    sc["bonusT"] = dram_(nc, pre + "bonusT", [1024, S], F32)
    sc["mats"] = dram_(nc, pre + "mats", [NCH, 64, 4, 16, 64], BF16)
    sc["tok3"] = dram_(nc, pre + "tok3", [NCH, 64, 3, 16, 64], BF16)
    sc["otok"] = dram_(nc, pre + "otok", [S, 1024], F32)
    return sc


def st_rope_tables(P, start_col, sc, S, consts, tag):
    TB = min(2048, S)
    with ExitStack() as es:
        sb = lambda n, sh, dt=F32: P.sb(es, tag + n, sh, dt)
        st_i, st_f = sb("sti", [128, 1], I32), sb("stf", [128, 1])
        P.dma("sp", st_i, start_col)
        P.copy("dve", st_f, st_i)
        ti, pos = sb("ti", [128, TB], I32), sb("pos", [128, TB])
        ang, kq, sn, cs = sb("ang", [128, TB]), sb("kq", [128, TB], I32), sb("sn", [128, TB]), sb("cs", [128, TB])
        for t0 in range(0, S, TB):
            P.I("pool", "iota", out=ti, pattern=[[1, TB]], base=t0, channel_multiplier=0)
            P.copy("dve", pos, ti)
            P.ts("dve", pos, pos, st_f, ALU.add)
            for nm, fq in (("A", consts["freqA"]), ("I", consts["freqI"])):
                P.ts("dve", ang, pos, fq, ALU.mult)
                sincos(P, ang, kq, ang, sn, cs, consts)
                P.dma("sp", sc["cos" + nm][:, t0:t0 + TB], cs)
                P.dma("sp", sc["sin" + nm][:, t0:t0 + TB], sn)
        P.barrier()


def st_dsa_kprep(P, sc, S, consts, tag):
    TB = 512
    with ExitStack() as es:
        sb = lambda n, sh, dt=F32: P.sb(es, tag + n, sh, dt)
        ring = lambda n, sh, dt=F32, k=2: Ring([P.sb(es, tag + n + str(q), sh, dt) for q in range(k)])
        kr, ki = ring("kr", [32, TB]), ring("ki", [64, TB])
        cA, sA, cI, sI = ring("cA", [32, TB]), ring("sA", [32, TB]), ring("cI", [64, TB]), ring("sI", [64, TB])
        t1, t2 = ring("t1", [64, TB]), ring("t2", [64, TB])
        okr, oki = ring("okr", [32, TB], BF16), ring("oki", [64, TB], BF16)
        ckv = ring("ckv", [128, 2, TB], BF16)
        ctok = ring("ctok", [128, TB // 128, 256], BF16)
        pr = Ring([P.ps(es, tag + "pr%d" % q, [64, TB], F32) for q in range(2)])
        pt = Ring([P.ps(es, tag + "pt%d" % q, [128, 2, 128], BF16) for q in range(2)])
        rotA, rotI = consts["rotA32"], consts["rotI128"]
        identb = consts["ident_bf"]
        for t0 in range(0, S, TB):
            ts_ = slice(t0, t0 + TB)
            for (src, x, c, s, cn, sn_, rot, npart, o, dst) in (
                    (sc["kropeT"], kr.next(), cA.next(), sA.next(), "cosA", "sinA", rotA, 32, okr.next(), sc["kvcatT"][256:288, :]),
                    (sc["kidxnT"], ki.next(), cI.next(), sI.next(), "cosI", "sinI", rotI, 64, oki.next(), sc["kidxT"])):
                P.dma("sp", x, src[:, ts_])
                P.dma("act", c, sc[cn][0:npart, ts_])
                P.dma("act", s, sc[sn_][0:npart, ts_])
                p = pr.next()
                P.mm(p[0:npart, :], rot[0:npart, 0:npart], x)
                a, b = t1.next()[0:npart, :], t2.next()[0:npart, :]
                P.tt("dve", a, p[0:npart, :], s, ALU.mult)
                P.tt("pool", b, x, c, ALU.mult)
                P.tt("dve", o, a, b, ALU.add)
                P.dma("sp", dst[:, ts_], o)
            ck = ckv.next()
            P.dma("sp", ck, sc["ckvnT"].rearrange("(c p) s -> p c s", p=128)[:, :, ts_])
            P.dma("act", sc["kvcatT"][0:256, :].rearrange("(c p) s -> p c s", p=128)[:, :, ts_], ck)
            ct = ctok.next()
            for k in range(TB // 128):
                tp = pt.next()
                for c2 in range(2):
                    P.tr(tp[:, c2, :], ck[:, c2, k * 128:(k + 1) * 128], identb)
                P.copy("act", ct[:, k, :].rearrange("p (c r) -> p c r", c=2), tp)
            P.dma("sp", sc["ckvtok"].rearrange("(n p) r -> p n r", p=128)[:, t0 // 128:(t0 + TB) // 128, :], ct)
        P.barrier()


def st_dsa_qprep(P, w, i, sc, S, consts, tag):
    TB = 512
    with ExitStack() as es:
        sb = lambda n, sh, dt=F32: P.sb(es, tag + n, sh, dt)
        ring = lambda n, sh, dt=F32, k=2: Ring([P.sb(es, tag + n + str(q), sh, dt) for q in range(k)])
        wf = sb("wf", [128, 4, 1024])
        wuq, wqi = sb("wuq", [128, 4, 1024], BF16), sb("wqi", [128, 4, 1024], BF16)
        P.dma("sp", wf, w["dsa_w_uq"][i].rearrange("(c p) h d -> p c (h d)", p=128))
        P.copy("act", wuq, wf)
        P.dma("sp", wf, w["dsa_w_qidx"][i].rearrange("(c p) h d -> p c (h d)", p=128))
        P.copy("act", wqi, wf)
        wkf = sb("wkf", [128, 8, 256])
        wuk = sb("wuk", [128, 8, 256], BF16)
        P.dma("sp", wkf, w["dsa_wukT_pad"][i].rearrange("h d r -> d h r"))
        P.copy("act", wuk, wkf)
        cq = ring("cq", [128, 4, TB], BF16)
        cA, sA, cI, sI = ring("cA", [32, TB]), ring("sA", [32, TB]), ring("cI", [128, TB]), ring("sI", [128, TB])
        qh = ring("qh", [128, TB], BF16)
        x32 = ring("x32", [32, TB])
        xi = ring("xi", [128, TB])
        t1, t2 = ring("t1", [128, TB]), ring("t2", [128, TB])
        ol = ring("ol", [128, 2, TB], BF16, 3)
        orp = ring("orp", [32, TB], BF16, 3)
        oi = ring("oi", [128, TB], BF16, 3)
        pq = Ring([P.ps(es, tag + "pq%d" % q, [128, TB], F32) for q in range(3)])
        prr = Ring([P.ps(es, tag + "prr%d" % q, [128, TB], F32) for q in range(2)])
        pl = Ring([P.ps(es, tag + "pl%d" % q, [128, TB], F32) for q in range(3)])
        rotA, rotI = consts["rotA32"], consts["rotI128"]
        for t0 in range(0, S, TB):
            ts_ = slice(t0, t0 + TB)
            c = cq.next()
            P.dma("sp", c, sc["cqnT"].rearrange("(c p) s -> p c s", p=128)[:, :, ts_])
            ca, sa, ci, si = cA.next(), sA.next(), cI.next(), sI.next()
            P.dma("act", ca, sc["cosA"][0:32, ts_])
            P.dma("act", sa, sc["sinA"][0:32, ts_])
            P.dma("act", ci, sc["cosI"][:, ts_])
            P.dma("act", si, sc["sinI"][:, ts_])
            import os
            dbg = int(os.environ.get("QDBG", "511"))
            for h in range(8 if dbg & 1 else 0):
                p = pq.next()
                for k in range(4):
                    P.mm(p, wuq[:, k, h * 128:(h + 1) * 128], c[:, k, :], start=(k == 0), stop=(k == 3))
                q_ = qh.next()
                P.copy("act", q_, p)
                if dbg & 4:
                    x = x32.next()
                    P.copy("dve", x, p[0:32, :])
                    pr_ = prr.next()
                    if dbg & 32:
                        P.mm(pr_[0:32, :], rotA, x)
                    a, b = t1.next()[0:32, :], t2.next()[0:32, :]
                    if dbg & 64:
                        P.tt("dve", a, pr_[0:32, :], sa, ALU.mult)
                    if dbg & 128:
                        P.tt("pool", b, x, ca, ALU.mult)
                    o_r = orp.next()
                    if dbg & 256:
                        P.tt("dve", o_r, a, b, ALU.add)
                    if dbg & 16:
                        P.dma("sp", sc["qcatT"][h, 256:288, ts_], o_r)
                o_l = ol.next()
                for c2 in range(2 if dbg & 8 else 0):
                    p2 = pl.next()
                    P.mm(p2, wuk[:, h, c2 * 128:(c2 + 1) * 128], q_)
                    P.copy("act" if c2 == 0 else "dve", o_l[:, c2, :], p2)
                if dbg & 8:
                    P.dma("sp", sc["qcatT"][h, 0:256, ts_].rearrange("(c p) s -> p c s", p=128), o_l)
            for j in range(8 if dbg & 2 else 0):
                p = pq.next()
                for k in range(4):
                    P.mm(p, wqi[:, k, j * 128:(j + 1) * 128], c[:, k, :], start=(k == 0), stop=(k == 3))
                x = xi.next()
                P.copy("act", x, p)
                pr_ = prr.next()
                P.mm(pr_, rotI, x)
                a, b = t1.next(), t2.next()
                P.tt("dve", a, pr_, si, ALU.mult)
                P.tt("pool", b, x, ci, ALU.mult)
                o_i = oi.next()
                P.tt("dve", o_i, a, b, ALU.add)
                P.dma("sp", sc["qidxT"][j * 128:(j + 1) * 128, ts_], o_i)
        P.barrier()


NEG_MASK = -30000.0


def st_dsa_attn(P, w, i, sc, S, consts, tag):
    QT = 128
    NQ = S // QT
    TOPK = min(256, S // 4)
    NS = 20
    scale = 128.0 ** -0.5
    with ExitStack() as es:
        sb = lambda n, sh, dt=F32: P.sb(es, tag + n, sh, dt)
        ring = lambda n, sh, dt=F32, k=2: Ring([P.sb(es, tag + n + str(q), sh, dt) for q in range(k)])
        kidx = sb("kidx", [64, S], BF16)
        kvc = sb("kvc", [128, 3, S], BF16)
        ctok = sb("ctok", [128, S // 128, 256], BF16)
        P.dma("sp", kidx, sc["kidxT"])
        P.dma("sp", kvc[:, 0:2, :], sc["kvcatT"][0:256, :].rearrange("(c p) s -> p c s", p=128))
        P.dma("sp", kvc[0:32, 2, :], sc["kvcatT"][256:288, :])
        P.dma("sp", ctok, sc["ckvtok"].rearrange("(n p) r -> p n r", p=128))
        wuv = sb("wuv", [128, 2, 1024], BF16)
        with ExitStack() as es2:
            wvf = P.sb(es2, tag + "wvf", [128, 2, 1024], F32)
            P.dma("sp", wvf, w["dsa_w_uv"][i].rearrange("(c p) h d -> p c (h d)", p=128))
            P.copy("act", wuv, wvf)
            P.barrier()
        qidx = [sb("qidx%d" % q, [64, 16, QT], BF16) for q in range(2)]
        qcat = [sb("qcat%d" % q, [128, 8, 3, QT], BF16) for q in range(2)]
        wT = [sb("wT%d" % q, [16, QT]) for q in range(2)]
        wtok = sb("wtok", [128, 16])
        Dm = sb("Dm", [128, 16, 128], BF16)
        rl = ring("rl", [128, 512], BF16, 4)
        scb = [sb("scb%d" % q, [128, S]) for q in range(2)]
        madd = [sb("madd%d" % q, [128, S], BF16) for q in range(2)]
        junk = sb("junk", [128, S], BF16)
        sm = [sb("sm%d" % q, [128, S]) for q in range(1)] * 2
        pb = [sb("pb%d" % q, [128, S], BF16) for q in range(2)]
        pTs = [sb("pTs%d" % q, [128, S // 128, 128], BF16) for q in range(2)]
        lo = [sb("lo%d" % q, [128, 1]) for q in range(2)]
        whalf = [sb("whalf%d" % q, [128, NS]) for q in range(2)]
        mxs, mns, w0s = sb("mxs", [128, 1]), sb("mns", [128, 1]), sb("w0s", [128, 1])
        mid, cnt, inc = sb("mid", [128, 1]), sb("cnt", [128, 1]), sb("inc", [128, 1])
        mx, rs = ring("mx", [128, 1]), ring("rs", [128, 1])
        olat = ring("olat", [128, 256], BF16)
        oT = ring("oT", [128, 2, 128], BF16)
        ya = ring("ya", [128, 8, QT], BF16)
        p_l = Ring([P.ps(es, tag + "pl%d" % q, [128, 512], F32) for q in range(2)])
        p_sc = P.ps(es, tag + "psc", [128, 512], F32)
        p_qk = Ring([P.ps(es, tag + "pqk%d" % q, [128, 512], F32) for q in range(2)])
        p_ts = Ring([P.ps(es, tag + "ptr%d" % q, [128, 4, 128], BF16) for q in range(2)])
        p_m = P.ps(es, tag + "pm", [128, 512], F32)
        p_mb = p_m.bitcast(BF16)
        p_o = p_m[:, 256:512]
        identb, identf = consts["ident_bf"], consts["ident_f32"]
        pow2 = consts["pow2row"]

        def idx_phase(qt):
            s_ = qt % 2
            t0 = qt * QT
            L = t0 + QT
            qi_, qc_, wT_ = qidx[s_], qcat[s_], wT[s_]
            P.dma("sp", qi_, sc["qidxT"].rearrange("(h d) s -> d h s", d=64)[:, :, t0:t0 + QT])
            for c2 in range(2):
                P.dma("act", qc_[:, :, c2, :], sc["qcatT"][:, c2 * 128:(c2 + 1) * 128, t0:t0 + QT].rearrange("h p s -> p h s"))
            P.dma("act", qc_[0:32, :, 2, :], sc["qcatT"][:, 256:288, t0:t0 + QT].rearrange("h p s -> p h s"))
            md = madd[s_]
            if t0 < TOPK:
                def gen0():
                    P.memset("dve", md[:, 0:L], 0.0)
                    P.memset("dve", md[0:64, t0 + 64:t0 + 128], NEG_MASK)
                    yield
                return gen0()
            P.dma("sp", wT_, sc["widxT"][:, t0:t0 + QT])
            P.tr(p_m[:, 0:16], wT_, identf[0:16, 0:16])
            P.ts("dve", wtok, p_m[:, 0:16], 1.0 / 32.0, ALU.mult)
            for h in range(16):
                P.ts("dve" if h % 2 else "pool", Dm[:, h, :], identb, wtok[:, h:h + 1], ALU.mult)
            sc_ = scb[s_]
            for kb in range(0, L, 512):
                kw = min(512, L - kb)
                rprev = None
                for h in range(17):
                    if h < 16:
                        p = p_l.next()
                        P.mm(p[:, 0:kw], qi_[:, h, :], kidx[:, kb:kb + kw])
                        r = rl.next()
                        P.act(r[:, 0:kw], p[:, 0:kw], AF.Relu)
                    if rprev is not None:
                        P.mm(p_sc[:, 0:kw], Dm[:, h - 1, :], rprev[:, 0:kw], start=(h == 1), stop=(h == 16))
                    rprev = r
                P.copy("act", sc_[:, kb:kb + kw], p_sc[:, 0:kw])

            def gen():
                lo_, wh = lo[s_], whalf[s_]
                P.reduce(mxs, sc_[:, 0:L], ALU.max)
                yield
                P.reduce(mns, sc_[:, 0:L], ALU.min)
                P.memset("dve", sc_[0:64, t0 + 64:t0 + 128], -1e30)
                P.ts("dve", lo_, mns, -1.0, ALU.add)
                P.tt("dve", w0s, mxs, lo_, ALU.subtract)
                P.ts("dve", wh, pow2[:, 0:NS], w0s, ALU.mult)
                yield
                for k in range(NS):
                    P.tt("dve", mid, lo_, wh[:, k:k + 1], ALU.add)
                    P.ts("dve", junk[:, 0:L], sc_[:, 0:L], mid, ALU.is_gt, 0.0, ALU.add, accum_out=cnt)
                    P.stt("dve", inc, cnt, TOPK - 0.5, wh[:, k:k + 1], ALU.is_gt, ALU.mult)
                    P.tt("dve", lo_, lo_, inc, ALU.add)
                    yield
                P.ts("dve", md[:, 0:L], sc_[:, 0:L], lo_, ALU.is_le, NEG_MASK, ALU.mult)
                yield
            return gen()

        ycur = {}
        olats = {}

        def stage1(qt, h):
            s_ = qt % 2
            t0 = qt * QT
            L = t0 + QT
            qc_, md = qcat[s_], madd[s_]
            hb = h % 2
            sm_, pb_ = sm[hb], pb[hb]
            for kb in range(0, L, 512):
                kw = min(512, L - kb)
                p = p_qk.next()
                P.mm(p[:, 0:kw], qc_[:, h, 0, :], kvc[:, 0, kb:kb + kw], start=True, stop=False)
                P.mm(p[:, 0:kw], qc_[:, h, 1, :], kvc[:, 1, kb:kb + kw], start=False, stop=False)
                P.mm(p[:, 0:kw], qc_[0:32, h, 2, :], kvc[0:32, 2, kb:kb + kw], start=False, stop=True)
                P.stt("dve", sm_[:, kb:kb + kw], p[:, 0:kw], scale, md[:, kb:kb + kw], ALU.mult, ALU.add)
            mx_, rs_ = mxr[hb], rsr[hb]
            P.reduce(mx_, sm_[:, 0:L], ALU.max)
            P.ts("dve", mx_, mx_, -1.0, ALU.mult)
            P.act(pb_[:, 0:L], sm_[:, 0:L], AF.Exp, bias=mx_, accum_out=rs_)
            P.recip(rs_, rs_)

        def stage2(qt, h):
            t0 = qt * QT
            L = t0 + QT
            hb = h % 2
            pb_, pT_, rs_ = pb[hb], pTs[hb], rsr[hb]
            if h == 0:
                ycur[qt] = ya.next()
            y_ = ycur[qt]
            nb = L // 128
            for b0 in range(0, nb, 4):
                nn = min(4, nb - b0)
                p_t = p_ts.next()
                for b in range(nn):
                    P.tr(p_t[:, b, :], pb_[:, (b0 + b) * 128:(b0 + b + 1) * 128], identb)
                P.copy("act", pT_[:, b0:b0 + nn, :], p_t[:, 0:nn, :])
            for b in range(nb):
                P.mm(p_o, pT_[:, b, :], ctok[:, b, :], start=(b == 0), stop=(b == nb - 1))
            ol_ = olat.next()
            P.ts("dve", ol_, p_o, rs_, ALU.mult)
            olats[(qt, h)] = ol_

        def stage3(qt, h):
            t0 = qt * QT
            y_ = ycur[qt]
            ol_ = olats.pop((qt, h))
            oT_ = oT.next()
            for c2 in range(2):
                P.tr(p_mb[:, c2 * 128:(c2 + 1) * 128], ol_[:, c2 * 128:(c2 + 1) * 128], identb)
            P.copy("act", oT_.rearrange("p c q -> p (c q)"), p_mb[:, 0:256])
            for c2 in range(2):
                P.mm(p_m[:, 128:256], wuv[:, c2, h * 128:(h + 1) * 128], oT_[:, c2, :], start=(c2 == 0), stop=(c2 == 1))
            P.copy("act", y_[:, h, :], p_m[:, 128:256])
            if h == 7:
                P.dma("sp", sc["yT"][0:1024, t0:t0 + QT].rearrange("(h d) s -> d h s", d=128), y_)
                del ycur[qt]

        mxr = [sb("mxr%d" % q, [128, 1]) for q in range(2)]
        rsr = [sb("rsr%d" % q, [128, 1]) for q in range(2)]
        items = [(qt, h) for qt in range(NQ) for h in range(8)]
        g = idx_phase(0)
        for _ in g:
            pass
        g = None
        stage1(0, 0)
        for k, (qt, h) in enumerate(items):
            if h == 0 and qt + 1 < NQ:
                g = idx_phase(qt + 1)
            if k + 1 < len(items):
                nqt, nh = items[k + 1]
                if nh == 0 and g is not None:
                    for _ in g:
                        pass
                    g = None
                stage1(nqt, nh)
            stage2(qt, h)
            if k >= 1:
                stage3(*items[k - 1])
            if g is not None:
                for _ in range(4):
                    next(g, None)
        stage3(*items[-1])
        P.barrier()


def host_layout_dsa(inp):
    o = {}
    n = inp["dsa_w_uk"].shape[0]
    wuk = inp["dsa_w_uk"]
    pad = np.zeros((n, 8, 128, 256), np.float32)
    pad[:, :, 32:, :] = wuk.transpose(0, 2, 3, 1)
    o["dsa_wukT_pad"] = pad
    o["dsa_cq_norm_pc"] = colvec(inp["dsa_cq_norm"])
    o["dsa_ckv_norm_pc"] = colvec(inp["dsa_ckv_norm"])
    o["dsa_kidx_norm_pc"] = colvec(inp["dsa_kidx_norm"], 64)
    for k in ("dsa_w_uq", "dsa_w_uv", "dsa_w_qidx"):
        o[k] = inp[k]
    return o


def dsa_scratch(nc, S, pre="", kind="Internal"):
    d = lambda n, sh, dt: dram(nc, pre + n, sh, dt, kind)
    sc = {}
    for nm in ("cosA", "sinA", "cosI", "sinI"):
        sc[nm] = d(nm, [128, S], F32)
    sc["cqT"] = d("cqT", [512, S], F32)
    sc["ckvT"] = d("ckvT", [256, S], F32)
    sc["kropeT"] = d("kropeT", [32, S], F32)
    sc["kidxrT"] = d("kidxrT", [64, S], F32)
    sc["widxT"] = d("widxT", [16, S], F32)
    sc["cqnT"] = d("cqnT", [512, S], BF16)
    sc["ckvnT"] = d("ckvnT", [256, S], BF16)
    sc["kidxnT"] = d("kidxnT", [64, S], F32)
    sc["kvcatT"] = d("kvcatT", [288, S], BF16)
    sc["kidxT"] = d("kidxT", [64, S], BF16)
    sc["ckvtok"] = d("ckvtok", [S, 256], BF16)
    sc["qcatT"] = d("qcatT", [8, 288, S], BF16)
    sc["qidxT"] = d("qidxT", [1024, S], BF16)
    return sc


def dsa_mixer_stages(P, w, i, sc, S, consts, tag):
    st_norm(P, sc["cqT"], w["dsa_cq_norm_pc"][i], sc["cqnT"], 512, S, 1e-5, consts, tag + "nq")
    st_norm(P, sc["ckvT"], w["dsa_ckv_norm_pc"][i], sc["ckvnT"], 256, S, 1e-5, consts, tag + "nk")
    st_norm(P, sc["kidxrT"], w["dsa_kidx_norm_pc"][i], sc["kidxnT"], 64, S, 1e-5, consts, tag + "ni", out_dt=F32)
    st_dsa_kprep(P, sc, S, consts, tag + "kp")
    st_dsa_qprep(P, w, i, sc, S, consts, tag + "qp")
    st_dsa_attn(P, w, i, sc, S, consts, tag + "at")


A_SPLITS = [("cqT", 0, 512), ("ckvT", 512, 256), ("kropeT", 768, 32), ("kidxrT", 800, 64), ("widxT", 864, 16)]


def even_layer(P, xT, memT, w, l, sc, S, consts, tag):
    i = l // 2
    D = 2048
    st_norm(P, xT, w["norm_mix"][l], sc["xnT"], D, S, 1e-5, consts, tag + "n1")
    with ExitStack() as es:
        jobs = []
        Win = w["even_w_in"][i]
        for nm, c0, n in A_SPLITS:
            jobs.append((Win[:, c0:c0 + n], n, EpiStore(P, es, sc[nm], F32, tag + "e" + nm, nbuf=2)))
        jobs.append((Win[:, 880:4240], 3360, EpiStore(P, es, sc["hBT"], F32, tag + "ehB")))
        st_mm(P, sc["xnT"], D, S, jobs, consts, tag + "mi", wscr=sc["wscr"])
    dsa_mixer_stages(P, w, i, sc, S, consts, tag + "d")
    st_rwkv_prep(P, sc["hBT"], w, i, sc, S, consts, tag + "rp")
    st_rwkv_A(P, sc, S, consts, tag + "ra")
    st_rwkv_B(P, sc, S, consts, tag + "rb")
    st_rwkv_post(P, w, i, sc, S, consts, tag + "rq")
    with ExitStack() as es:
        st_mm(P, sc["yT"], D, S, [(w["even_w_out"][i], D, EpiResid(P, es, xT, tag + "rmo"))], consts, tag + "mo", wscr=sc["wscr"])
    layer_tail(P, xT, memT, w, l, sc, S, consts, tag)


PLAIN_W = ["xattn_wq", "xattn_wkv", "xattn_wo", "ffn_up", "ffn_down", "even_w_in", "even_w_out",
           "odd_w_in", "odd_w_out", "s5_w_glu"]


def host_layout_all(inp):
    o = {}
    for k in PLAIN_W:
        o[k] = np.ascontiguousarray(inp[k], dtype=np.float32)
    for k in ("norm_mix", "norm_xattn", "norm_mem", "norm_ffn"):
        o[k] = colvec(np.asarray(inp[k], np.float32))
    o["final_norm"] = colvec(np.asarray(inp["final_norm"], np.float32))
    f = {k: np.asarray(v, np.float32) for k, v in inp.items() if k.startswith(("s5_", "rwkv_", "dsa_"))}
    o.update(host_layout_s5(f))
    o.update(host_layout_rwkv(f))
    o.update(host_layout_dsa(f))
    return o


def build_program(S, wshapes, depth=4, dbg_out=None):
    nc = bass.Bass("TRN2", target_bir_lowering=False)
    D = 2048
    xin = dram(nc, "xT_in", [D, S], F32, "ExternalInput")
    memT = dram(nc, "memT", [D, 256], F32, "ExternalInput")
    start = dram(nc, "start_col", [128, 1], I32, "ExternalInput")
    outT = dram(nc, "outT", [D, S], F32, "ExternalOutput")
    w = {k: dram(nc, k, list(sh), F32, "ExternalInput") for k, sh in wshapes.items()}
    hc = host_consts()
    cd = {k: dram(nc, "c_" + k, list(v.shape), CONST_SPECS[k][1], "ExternalInput") for k, v in hc.items()}
    sc = {}
    sc.update(dsa_scratch(nc, S))
    sc.update(rwkv_scratch(nc, S))
    sc["xT"] = dram(nc, "xres", [D, S], F32)
    sc["xnT"] = dram(nc, "xnT", [D, S], BF16)
    sc["hBT"] = dram(nc, "hBT", [3360, S], F32)
    sc["yT"] = dram(nc, "yT", [D, S], BF16)
    sc["uT"] = dram(nc, "uT", [D, S], F32)
    sc["zT"] = dram(nc, "zT", [D, S], F32)
    sc["memnT"] = dram(nc, "memnT", [D, 256], BF16)
    sc["qT"] = dram(nc, "qT", [512, S], BF16)
    sc["kT"] = dram(nc, "kT", [512, 256], BF16)
    sc["vtok"] = dram(nc, "vtok", [256, 512], BF16)
    sc["oT"] = dram(nc, "oT", [512, S], BF16)
    sc["hT"] = dram(nc, "hT", [8192, S], BF16)
    sc["wscr"] = dram(nc, "wscr", [20 * 1024 * 1024], BF16)
    with ExitStack() as es:
        P = Prog(nc, es)
        consts = make_consts(P, es, cd)
        xT = sc["xT"]
        for c in range(16):
            P.dma("sp" if c % 2 == 0 else "act", xT[c * 128:(c + 1) * 128, :], xin[c * 128:(c + 1) * 128, :])
        P.barrier()
        st_rope_tables(P, start, sc, S, consts, "rt")
        for l in range(depth):
            if l % 2 == 0:
                even_layer(P, xT, memT, w, l, sc, S, consts, "L%d" % l)
            else:
                odd_layer(P, xT, memT, w, l, sc, S, consts, "L%d" % l)
        st_norm(P, xT, w["final_norm"], outT, D, S, 1e-5, consts, "fn", out_dt=F32)
        P.barrier()
        nc._ninst = P.ninst
    return nc


_CACHE = {}


def kernel(**inputs):
    x = np.asarray(inputs["x"], np.float32)
    mem = np.asarray(inputs["mem"], np.float32)
    start = np.asarray(inputs["start_frame"]).astype(np.int32)
    B, S, D = x.shape
    hl = host_layout_all(inputs)
    hc = host_consts()
    wshapes = {k: v.shape[1:] if False else v.shape for k, v in hl.items()}
    nc = build_program(S, wshapes)
    in_maps = []
    for b in range(B):
        m = {"xT_in": np.ascontiguousarray(x[b].T), "memT": np.ascontiguousarray(mem[b].T),
             "start_col": np.full((128, 1), start[b], np.int32)}
        m.update(hl)
        for k, v in hc.items():
            m["c_" + k] = v
        in_maps.append(m)
    res = run_bass_kernel_spmd(nc, in_maps, core_ids=list(range(B)))
    out = np.stack([np.ascontiguousarray(np.asarray(res.results[b]["outT"]).T) for b in range(B)], 0)
    return out.astype(np.float32)
```

```python
import numpy as np
from contextlib import ExitStack
import concourse.bass as bass
import concourse.mybir as mybir
from concourse.bass_utils import run_bass_kernel_spmd

F32 = mybir.dt.float32
BF16 = mybir.dt.bfloat16
I32 = mybir.dt.int32
ALU = mybir.AluOpType
AF = mybir.ActivationFunctionType
AX = mybir.AxisListType

import os as _os
SAFE_SAME_ENGINE = _os.environ.get("UNSAFE_SAME", "0") != "1"


class V:
    __slots__ = ("ap", "buf")

    def __init__(self, ap, buf):
        self.ap = ap
        self.buf = buf

    def __getitem__(self, idx):
        return V(self.ap[idx], self.buf)

    def rearrange(self, *a, **k):
        return V(self.ap.rearrange(*a, **k), self.buf)

    def broadcast_to(self, shape):
        return V(self.ap.broadcast_to(shape), self.buf)

    def bitcast(self, dt):
        return V(self.ap.bitcast(dt), self.buf)

    @property
    def shape(self):
        return self.ap.shape


class Buf:
    __slots__ = ("name", "last_write", "readers", "psum")

    def __init__(self, name, psum=False):
        self.name = name
        self.psum = psum
        self.last_write = None
        self.readers = []


class Prog:
    NDMA = 8

    def __init__(self, nc, es):
        self.nc = nc
        self.eng = {"pe": nc.tensor, "act": nc.scalar, "dve": nc.vector, "pool": nc.gpsimd, "sp": nc.sync}
        self.sem = {}
        self.count = {}
        for e in self.eng:
            self.sem[e] = es.enter_context(nc.semaphore("c_" + e))
            self.count[e] = 0
        self.dsem = {}
        self.dcount = {}
        self.dnext = {}
        for q in ("sp", "act", "pool"):
            for i in range(self.NDMA):
                key = ("d", q, i)
                self.sem[key] = es.enter_context(nc.semaphore("d_%s%d" % (q, i)))
                self.count[key] = 0
            self.dnext[q] = 0
        self.known = {e: {} for e in self.eng}
        self.bufs = []
        self.ninst = 0

    def sb(self, es, name, shape, dt):
        t = es.enter_context(self.nc.sbuf_tensor(name, list(shape), dt))
        b = Buf(name)
        self.bufs.append(b)
        return V(t[:], b)

    def ps(self, es, name, shape, dt):
        t = es.enter_context(self.nc.psum_tensor(name, list(shape), dt))
        b = Buf(name, True)
        self.bufs.append(b)
        return V(t[:], b)

    def _need(self, e, needs):
        kn = self.known[e]
        for key, val in needs.items():
            if key == e and (e == "pe" or not SAFE_SAME_ENGINE) and not isinstance(key, tuple):
                continue
            if kn.get(key, 0) >= val:
                continue
            self.eng[e].wait_ge(self.sem[key], val)
            kn[key] = val

    def I(self, e, method, **kw):
        needs = {}
        reads, writes = [], []
        args = {}
        for k, v in kw.items():
            if isinstance(v, V):
                args[k] = v.ap
                if v.buf is not None:
                    (writes if k in ("out", "accum_out", "ap") else reads).append(v.buf)
            else:
                args[k] = v

        def add(tok):
            if tok is not None:
                if needs.get(tok[0], 0) < tok[1]:
                    needs[tok[0]] = tok[1]

        for b in reads:
            add(b.last_write)
            if b.psum:
                for r in b.readers:
                    if r[0] != e:
                        add(r)
        for b in writes:
            add(b.last_write)
            for r in b.readers:
                add(r)
        is_dma = method == "dma_start"
        if is_dma:
            q = e
            i = self.dnext[q]
            self.dnext[q] = (i + 1) % self.NDMA
            key = ("d", q, i)
            if self.count[key] > 0:
                add((key, self.count[key]))
        self._need(e, needs)
        inst = getattr(self.eng[e], method)(**args)
        self.ninst += 1
        if is_dma:
            self.count[key] += 16
            inst.then_inc(self.sem[key], 16)
            tok = (key, self.count[key])
        else:
            self.count[e] += 1
            inst.then_inc(self.sem[e], 1)
            tok = (e, self.count[e])
        for b in reads:
            b.readers.append(tok)
            if len(b.readers) > 64:
                m = {}
                for r in b.readers:
                    if m.get(r[0], 0) < r[1]:
                        m[r[0]] = r[1]
                b.readers = list(m.items())
        for b in writes:
            b.last_write = tok
            b.readers = []
        return inst

    def barrier(self):
        needs = {k: c for k, c in self.count.items() if c > 0}
        for e in self.eng:
            self._need(e, dict(needs))
        for b in self.bufs:
            b.last_write = None
            b.readers = []
        self.bufs = []

    def barrier_dram(self):
        needs = {k: c for k, c in self.count.items() if c > 0 and isinstance(k, tuple)}
        for e in ("sp", "act", "pool"):
            self._need(e, dict(needs))

    def dma(self, q, out, in_):
        return self.I(q, "dma_start", out=out, in_=in_)

    def mm(self, out, lhsT, rhs, start=True, stop=True):
        return self.I("pe", "matmul", out=out, lhsT=lhsT, rhs=rhs, start=start, stop=stop)

    def tr(self, out, in_, ident):
        return self.I("pe", "transpose", out=out, in_=in_, identity=ident)

    def act(self, out, in_, func, bias=None, scale=None, accum_out=None, e="act"):
        kw = dict(out=out, in_=in_, func=func)
        if bias is not None:
            kw["bias"] = bias
        if scale is not None:
            kw["scale"] = scale
        if accum_out is not None:
            kw["accum_out"] = accum_out
        return self.I(e, "activation", **kw)

    def tt(self, e, out, in0, in1, op):
        return self.I(e, "tensor_tensor", out=out, in0=in0, in1=in1, op=op)

    def ts(self, e, out, in0, s1, op0, s2=None, op1=None, accum_out=None):
        kw = dict(out=out, in0=in0, scalar1=s1, scalar2=s2, op0=op0)
        if op1 is not None:
            kw["op1"] = op1
        if accum_out is not None:
            kw["accum_out"] = accum_out
        return self.I(e, "tensor_scalar", **kw)

    def stt(self, e, out, in0, scalar, in1, op0, op1):
        return self.I(e, "scalar_tensor_tensor", out=out, in0=in0, scalar=scalar, in1=in1, op0=op0, op1=op1)

    def copy(self, e, out, in_):
        if e == "act":
            return self.I(e, "copy", out=out, in_=in_)
        return self.I(e, "tensor_copy", out=out, in_=in_)

    def memset(self, e, out, val):
        return self.I(e, "memset", ap=out, constant=val)

    def recip(self, out, in_):
        return self.I("dve", "reciprocal", out=out, in_=in_)

    def reduce(self, out, in_, op, axis=None):
        return self.I("dve", "tensor_reduce", out=out, in_=in_, axis=axis or AX.X, op=op)

    def scan(self, out, d0, d1, initial, op0, op1):
        return self.I("dve", "tensor_tensor_scan", out=out, data0=d0, data1=d1, initial=initial, op0=op0, op1=op1)


class Ring:
    def __init__(self, items):
        self.items = items
        self.i = 0

    def next(self):
        v = self.items[self.i % len(self.items)]
        self.i += 1
        return v


def dram(nc, name, shape, dt, kind="Internal"):
    return V(nc.dram_tensor(name, list(shape), dt, kind=kind).ap(), None)


def kchunks(K):
    return [(k0, min(128, K - k0)) for k0 in range(0, K, 128)]


def st_norm(P, xT, g, outT, D, S, eps, consts, tag, out_dt=BF16):
    nc = P.nc
    pp = min(D, 128)
    kc = D // pp
    TT = min(512, S)
    with ExitStack() as es:
        gs = P.sb(es, tag + "g", [pp, kc], F32)
        P.dma("sp", gs, g)
        xs = [P.sb(es, tag + "x%d" % i, [pp, kc, TT], F32) for i in range(2)]
        sq = P.sb(es, tag + "sq", [pp, kc, TT], BF16)
        os_ = [P.sb(es, tag + "o%d" % i, [pp, kc, TT], out_dt) for i in range(2)]
        rs = P.sb(es, tag + "rs", [pp, TT], F32)
        pss = [P.ps(es, tag + "ps%d" % i, [pp, TT], F32) for i in range(2)]
        xv = xT.rearrange("(c p) s -> p c s", p=pp)
        ov = outT.rearrange("(c p) s -> p c s", p=pp)
        ones = consts["ones_bf"]
        for it, t0 in enumerate(range(0, S, TT)):
            x = xs[it % 2]
            o = os_[it % 2]
            ps = pss[it % 2]
            P.dma("sp", x, xv[:, :, t0:t0 + TT])
            P.act(sq, x, AF.Square)
            for c in range(kc):
                P.mm(ps, ones[0:pp, 0:pp], sq[:, c, :], start=(c == 0), stop=(c == kc - 1))
            P.act(rs, ps, AF.Sqrt, bias=consts["eps_%g" % eps][0:pp, :], scale=1.0 / D)
            P.recip(rs, rs)
            for c in range(kc):
                P.stt("dve", o[:, c, :], x[:, c, :], gs[:, c:c + 1], rs, ALU.mult, ALU.mult)
            P.dma("sp", ov[:, :, t0:t0 + TT], o)
        P.barrier()


def st_mm(P, aT, K, S, jobs, consts, tag, TT=2048, CG=512, a_dt=BF16, w_dt=F32, wscr=None):
    kcs = kchunks(K)
    kc = len(kcs)
    pp = kcs[0][1]
    TT = min(TT, S)
    ntt = (S + TT - 1) // TT
    regular = (K % 128 == 0 or K < 128)
    pre = wscr is not None and w_dt == F32 and ntt >= 2 and regular
    groups = []
    for ji, (W, n, epi) in enumerate(jobs):
        for g0 in range(0, n, CG):
            groups.append((ji, g0, min(CG, n - g0)))
    gstride = pp * kc * CG
    if pre:
        with ExitStack() as esp:
            wfp = [P.sb(esp, tag + "pwf%d" % i, [pp, kc, CG], F32) for i in range(2)]
            wbp = [P.sb(esp, tag + "pwb%d" % i, [pp, kc, CG], BF16) for i in range(2)]
            for gi, (ji, g0, gsz) in enumerate(groups):
                W = jobs[ji][0]
                f, b = wfp[gi % 2], wbp[gi % 2]
                P.dma("act" if gi % 2 else "sp", f[:, :, 0:gsz], W.rearrange("(c p) n -> p c n", p=pp)[:, :, g0:g0 + gsz])
                P.copy("dve" if gi % 2 else "act", b[:, :, 0:gsz], f[:, :, 0:gsz])
                dst = wscr[gi * gstride:(gi + 1) * gstride].rearrange("(p c n) -> p c n", p=pp, c=kc)
                P.dma("pool", dst[:, :, 0:gsz], b[:, :, 0:gsz])
            P.barrier()
    with ExitStack() as es:
        na = 2 if (kc * TT * 2 <= 65536 and a_dt == BF16) else 1
        a_s = [P.sb(es, tag + "a%d" % i, [pp, kc, TT], BF16) for i in range(na)]
        af = P.sb(es, tag + "af", [pp, kc, TT], F32) if a_dt == F32 else None
        wf = [P.sb(es, tag + "wf%d" % i, [pp, kc, CG], F32) for i in range(2)] if (w_dt == F32 and not pre) else None
        nwb = 3 if pre else 2
        wb = [P.sb(es, tag + "wb%d" % i, [pp, kc, CG], BF16) for i in range(nwb)]
        pss = Ring([P.ps(es, tag + "ps%d" % i, [128, 512], F32) for i in range(4)])
        state = {"iw": 0}

        def load_f32(gi, ji, g0, gsz):
            iw = state["iw"]
            state["iw"] += 1
            W = jobs[ji][0]
            b = wb[iw % nwb]
            if w_dt != F32:
                if regular:
                    P.dma("act" if iw % 2 else "sp", b[:, :, 0:gsz], W.rearrange("(c p) n -> p c n", p=pp)[:, :, g0:g0 + gsz])
                else:
                    for c, (k0, ksz) in enumerate(kcs):
                        P.dma("sp", b[0:ksz, c, 0:gsz], W[k0:k0 + ksz, g0:g0 + gsz])
                return b
            f = wf[iw % 2]
            ce = "dve" if iw % 2 else "act"
            if regular:
                P.dma("act" if iw % 2 else "sp", f[:, :, 0:gsz], W.rearrange("(c p) n -> p c n", p=pp)[:, :, g0:g0 + gsz])
                P.copy(ce, b[:, :, 0:gsz], f[:, :, 0:gsz])
            else:
                for c, (k0, ksz) in enumerate(kcs):
                    P.dma("sp", f[0:ksz, c, 0:gsz], W[k0:k0 + ksz, g0:g0 + gsz])
                    P.copy(ce, b[0:ksz, c, 0:gsz], f[0:ksz, c, 0:gsz])
            return b

        for it, t0 in enumerate(range(0, S, TT)):
            a = a_s[it % len(a_s)]
            ald = af if a_dt == F32 else a
            if regular:
                P.dma("sp", ald, aT.rearrange("(c p) s -> p c s", p=pp)[:, :, t0:t0 + TT])
                if a_dt == F32:
                    P.copy("dve", a, af)
            else:
                for c, (k0, ksz) in enumerate(kcs):
                    P.dma("sp", ald[0:ksz, c, :], aT[k0:k0 + ksz, t0:t0 + TT])
                    if a_dt == F32:
                        P.copy("dve", a[0:ksz, c, :], af[0:ksz, c, :])
            for gi, (ji, g0, gsz) in enumerate(groups):
                epi = jobs[ji][2]
                if pre:
                    iw = state["iw"]
                    state["iw"] += 1
                    b = wb[iw % nwb]
                    src = wscr[gi * gstride:(gi + 1) * gstride].rearrange("(p c n) -> p c n", p=pp, c=kc)
                    P.dma("act" if iw % 2 else "sp", b[:, :, 0:gsz], src[:, :, 0:gsz])
                else:
                    b = load_f32(gi, ji, g0, gsz)
                for m0 in range(0, gsz, 128):
                    msz = min(128, gsz - m0)
                    for n0 in range(0, TT, 512):
                        nsz = min(512, TT - n0)
                        ps = pss.next()
                        for c, (k0, ksz) in enumerate(kcs):
                            P.mm(ps[0:msz, 0:nsz], b[0:ksz, c, m0:m0 + msz], a[0:ksz, c, n0:n0 + nsz],
                                 start=(c == 0), stop=(c == kc - 1))
                        epi(P, ps[0:msz, 0:nsz], g0 + m0, msz, t0 + n0, nsz)
        P.barrier()


STORE_Q = _os.environ.get("STORE_Q", "pool")


class EpiStore:
    def __init__(self, P, es, outT, dt, tag, func=None, nbuf=3):
        self.outT = outT
        self.ring = Ring([P.sb(es, tag + "e%d" % i, [128, 512], dt) for i in range(nbuf)])
        self.func = func
        self.i = 0

    def __call__(self, P, ps, c0, csz, t0, tsz):
        o = self.ring.next()[0:csz, 0:tsz]
        if self.i % 2 == 0:
            P.act(o, ps, self.func or AF.Copy)
        else:
            if self.func is None:
                P.copy("dve", o, ps)
            else:
                P.act(o, ps, self.func)
        self.i += 1
        P.dma(STORE_Q, self.outT[c0:c0 + csz, t0:t0 + tsz], o)


class EpiResid:
    def __init__(self, P, es, xT, tag, nbuf=3):
        self.xT = xT
        self.ring = Ring([P.sb(es, tag + "r%d" % i, [128, 512], F32) for i in range(nbuf)])

    def __call__(self, P, ps, c0, csz, t0, tsz):
        o = self.ring.next()[0:csz, 0:tsz]
        P.dma("act", o, self.xT[c0:c0 + csz, t0:t0 + tsz])
        P.tt("dve", o, ps, o, ALU.add)
        P.dma(STORE_Q, self.xT[c0:c0 + csz, t0:t0 + tsz], o)


def make_consts(P, es, cd):
    c = {}
    for name, (shape, dt) in CONST_SPECS.items():
        t = P.sb(es, "k_" + name, shape, dt)
        P.dma("sp", t, cd[name])
        c[name] = t
    return c


CONST_SPECS = {
    "ones_f32": ([128, 128], F32),
    "ident_f32": ([128, 128], F32),
    "eps_1e-05": ([128, 1], F32),
    "eps_0.00064": ([128, 1], F32),
    "ones_bf": ([128, 128], BF16),
    "ident_bf": ([128, 128], BF16),
    "halfpi": ([128, 1], F32),
    "blockones_f32": ([128, 128], F32),
    "mask_su": ([64, 1, 64], F32),
    "mask_iu": ([64, 1, 64], F32),
    "mask_sl": ([64, 1, 64], F32),
    "ident64": ([64, 1, 64], F32),
    "rotA32": ([32, 32], F32),
    "rotI128": ([128, 128], F32),
    "freqA": ([128, 1], F32),
    "freqI": ([128, 1], F32),
    "pow2row": ([128, 24], F32),
}


def host_consts():
    import ml_dtypes
    d = {}
    d["ones_f32"] = np.ones((128, 128), np.float32)
    d["ident_f32"] = np.eye(128, dtype=np.float32)
    d["eps_1e-05"] = np.full((128, 1), 1e-5, np.float32)
    d["eps_0.00064"] = np.full((128, 1), 64e-5, np.float32)
    d["ones_bf"] = np.ones((128, 128), ml_dtypes.bfloat16)
    d["ident_bf"] = np.eye(128).astype(ml_dtypes.bfloat16)
    d["halfpi"] = np.full((128, 1), np.pi / 2, np.float32)
    bo = np.zeros((128, 128), np.float32)
    bo[:64, :64] = 1.0
    bo[64:, 64:] = 1.0
    d["blockones_f32"] = bo
    iu = np.arange(64)
    d["mask_su"] = (iu[:, None] < iu[None, :]).astype(np.float32).reshape(64, 1, 64)
    d["mask_iu"] = (iu[:, None] <= iu[None, :]).astype(np.float32).reshape(64, 1, 64)
    d["mask_sl"] = (iu[:, None] > iu[None, :]).astype(np.float32).reshape(64, 1, 64)
    d["ident64"] = np.eye(64, dtype=np.float32).reshape(64, 1, 64)
    ra = np.zeros((32, 32), np.float32)
    for m_ in range(16):
        ra[m_ + 16, m_] = -1.0
        ra[m_, m_ + 16] = 1.0
    d["rotA32"] = ra
    ri = np.zeros((128, 128), np.float32)
    for o_ in (0, 64):
        for m_ in range(8):
            ri[o_ + m_ + 8, o_ + m_] = -1.0
            ri[o_ + m_, o_ + m_ + 8] = 1.0
    d["rotI128"] = ri
    theta = np.float32(500000.0)
    fa = (theta ** (-np.arange(0, 32, 2, dtype=np.float32) / np.float32(32))).astype(np.float32)
    fi = (theta ** (-np.arange(0, 16, 2, dtype=np.float32) / np.float32(16))).astype(np.float32)
    fA = np.zeros((128, 1), np.float32)
    fA[0:16, 0] = fa
    fA[16:32, 0] = fa
    fI = np.zeros((128, 1), np.float32)
    for o_ in (0, 64):
        fI[o_:o_ + 8, 0] = fi
        fI[o_ + 8:o_ + 16, 0] = fi
    d["freqA"], d["freqI"] = fA, fI
    d["pow2row"] = np.tile((0.5 ** np.arange(1, 25, dtype=np.float64)).astype(np.float32)[None, :], (128, 1))
    return d


class EpiRelu2:
    def __init__(self, P, es, outT, tag, nbuf=3):
        self.outT = outT
        self.sq = Ring([P.sb(es, tag + "q%d" % i, [128, 512], F32) for i in range(nbuf)])
        self.ring = Ring([P.sb(es, tag + "e%d" % i, [128, 512], BF16) for i in range(nbuf)])

    def __call__(self, P, ps, c0, csz, t0, tsz):
        sq = self.sq.next()[0:csz, 0:tsz]
        o = self.ring.next()[0:csz, 0:tsz]
        P.act(sq, ps, AF.Square)
        P.stt("dve", o, ps, 0.0, sq, ALU.is_gt, ALU.mult)
        P.dma(STORE_Q, self.outT[c0:c0 + csz, t0:t0 + tsz], o)


class EpiGlu:
    def __init__(self, P, es, zT, bcol, outT, tag, nbuf=3):
        self.zT, self.outT, self.bcol = zT, outT, bcol
        self.z = Ring([P.sb(es, tag + "z%d" % i, [128, 512], F32) for i in range(nbuf)])
        self.sg = Ring([P.sb(es, tag + "s%d" % i, [128, 512], F32) for i in range(nbuf)])
        self.ring = Ring([P.sb(es, tag + "e%d" % i, [128, 512], BF16) for i in range(nbuf)])

    def __call__(self, P, ps, c0, csz, t0, tsz):
        z = self.z.next()[0:csz, 0:tsz]
        sg = self.sg.next()[0:csz, 0:tsz]
        o = self.ring.next()[0:csz, 0:tsz]
        P.dma("act", z, self.zT[c0:c0 + csz, t0:t0 + tsz])
        P.act(sg, ps, AF.Sigmoid, bias=self.bcol[0:csz, c0 // 128:c0 // 128 + 1])
        P.tt("dve", o, z, sg, ALU.mult)
        P.dma(STORE_Q, self.outT[c0:c0 + csz, t0:t0 + tsz], o)


def st_xattn_core(P, qT, kT, vtok, oT, S, consts, tag):
    H, M = 4, 256
    TT = 512
    sc = 128.0 ** -0.5
    with ExitStack() as es:
        k = P.sb(es, tag + "k", [128, H, M], BF16)
        v = P.sb(es, tag + "v", [128, 2, 512], BF16)
        P.dma("sp", k, kT.rearrange("(h d) m -> d h m", d=128))
        P.dma("sp", v, vtok.rearrange("(c m) n -> m c n", m=128))
        qs = [P.sb(es, tag + "q%d" % i, [128, H, TT], BF16) for i in range(2)]
        os_ = [P.sb(es, tag + "o%d" % i, [128, H, TT], BF16) for i in range(2)]
        pT = Ring([P.sb(es, tag + "p%d" % i, [128, 2, TT], BF16) for i in range(2)])
        rd = Ring([P.sb(es, tag + "rd%d" % i, [128, TT], F32) for i in range(2)])
        ps_s = Ring([P.ps(es, tag + "pss%d" % i, [128, TT], F32) for i in range(4)])
        ps_d = Ring([P.ps(es, tag + "psd%d" % i, [128, TT], F32) for i in range(2)])
        ps_o = Ring([P.ps(es, tag + "pso%d" % i, [128, TT], F32) for i in range(2)])
        ones = consts["ones_bf"]
        qv = qT.rearrange("(h d) s -> d h s", d=128)
        ov = oT.rearrange("(h d) s -> d h s", d=128)
        for it, t0 in enumerate(range(0, S, TT)):
            q = qs[it % 2]
            o = os_[it % 2]
            P.dma("sp", q, qv[:, :, t0:t0 + TT])
            for h in range(H):
                p = pT.next()
                for mc in range(2):
                    ps = ps_s.next()
                    P.mm(ps, k[:, h, mc * 128:(mc + 1) * 128], q[:, h, :])
                    P.act(p[:, mc, :], ps, AF.Exp, scale=sc)
                pd = ps_d.next()
                po = ps_o.next()
                for mc in range(2):
                    P.mm(pd, ones, p[:, mc, :], start=(mc == 0), stop=(mc == 1))
                for mc in range(2):
                    P.mm(po, v[:, mc, h * 128:(h + 1) * 128], p[:, mc, :], start=(mc == 0), stop=(mc == 1))
                r = rd.next()
                P.recip(r, pd)
                P.tt("dve", o[:, h, :], po, r, ALU.mult)
            P.dma("sp", ov[:, :, t0:t0 + TT], o)
        P.barrier()


def layer_tail(P, xT, memT, w, l, sc, S, consts, tag):
    D = 2048
    t = tag + "T"
    st_norm(P, xT, w["norm_xattn"][l], sc["xnT"], D, S, 1e-5, consts, t + "n2")
    st_norm(P, memT, w["norm_mem"][l], sc["memnT"], D, 256, 1e-5, consts, t + "nm")
    with ExitStack() as es:
        st_mm(P, sc["xnT"], D, S, [(w["xattn_wq"][l], 512, EpiStore(P, es, sc["qT"], BF16, t + "eq"))], consts, t + "mq", wscr=sc["wscr"])
    with ExitStack() as es:
        st_mm(P, sc["memnT"], D, 256, [(w["xattn_wkv"][l][:, 0:512], 512, EpiStore(P, es, sc["kT"], BF16, t + "ek"))],
              consts, t + "mk")
    with ExitStack() as es:
        st_mm(P, w["xattn_wkv"][l][:, 512:1024], D, 512, [(sc["memnT"], 256, EpiStore(P, es, sc["vtok"], BF16, t + "ev"))],
              consts, t + "mv", a_dt=F32, w_dt=BF16)
    st_xattn_core(P, sc["qT"], sc["kT"], sc["vtok"], sc["oT"], S, consts, t + "xc")
    with ExitStack() as es:
        st_mm(P, sc["oT"], 512, S, [(w["xattn_wo"][l], D, EpiResid(P, es, xT, t + "ro"))], consts, t + "mo", wscr=sc["wscr"])
    st_norm(P, xT, w["norm_ffn"][l], sc["xnT"], D, S, 1e-5, consts, t + "n3")
    with ExitStack() as es:
        st_mm(P, sc["xnT"], D, S, [(w["ffn_up"][l], 8192, EpiRelu2(P, es, sc["hT"], t + "eu"))], consts, t + "mu", wscr=sc["wscr"])
    with ExitStack() as es:
        st_mm(P, sc["hT"], 8192, S, [(w["ffn_down"][l], D, EpiResid(P, es, xT, t + "rd"))], consts, t + "md",
              TT=1024, CG=128, wscr=sc["wscr"])


TWO_PI = 2.0 * np.pi
CW1 = 6.28125
CW2 = TWO_PI - 6.28125


def sincos(P, ang, kq, r, sinv, cosv, consts, clamp_eng="dve"):
    npart = ang.shape[0]
    P.ts("dve", kq, ang, 1.0 / TWO_PI, ALU.mult)
    P.stt("dve", r, kq, -CW1, ang, ALU.mult, ALU.add)
    P.stt("dve", r, kq, -CW2, r, ALU.mult, ALU.add)
    P.ts(clamp_eng, r, r, float(np.pi), ALU.min, -float(np.pi), ALU.max)
    P.act(sinv, r, AF.Sin)
    P.act(r, r, AF.Abs)
    hp = consts["halfpi"][0:npart, :]
    P.act(cosv, r, AF.Sin, bias=hp, scale=-1.0)


def st_s5_core(P, uT, zT, w, i, S, consts, tag):
    TB = min(1024, S)
    NB = TB // 512
    with ExitStack() as es:
        sb = lambda n, sh, dt=F32: P.sb(es, tag + n, sh, dt)
        lamre, lamim, logdt = sb("lamre", [128, 64]), sb("lamim", [128, 64]), sb("logdt", [128, 64])
        P.dma("sp", lamre, w["s5_lamre_pc"][i])
        P.dma("sp", lamim, w["s5_lamim_pc"][i])
        P.dma("sp", logdt, w["s5_logdt_pc"][i])
        dcol, = [sb("dcol", [128, 16])]
        P.dma("sp", dcol, w["s5_d_pc"][i])
        lre, dt, rho, th, th2 = sb("lre", [128, 64]), sb("dt", [128, 64]), sb("rho", [128, 64]), sb("th", [128, 64]), sb("th2", [128, 64])
        P.ts("dve", lre, lamre, -1e-4, ALU.min)
        P.act(dt, logdt, AF.Exp)
        P.tt("dve", th2, lre, dt, ALU.mult)
        P.act(rho, th2, AF.Exp)
        P.tt("dve", th, lamim, dt, ALU.mult)
        kq0, r0, sth, cth = sb("kq0", [128, 64], I32), sb("r0", [128, 64]), sb("sth", [128, 64]), sb("cth", [128, 64])
        sincos(P, th, kq0, r0, sth, cth, consts)
        cr, ci, den, t1, t2 = sb("cr", [128, 64]), sb("ci", [128, 64]), sb("den", [128, 64]), sb("t1", [128, 64]), sb("t2", [128, 64])
        qr, qi, nqi = sb("qr", [128, 64]), sb("qi", [128, 64]), sb("nqi", [128, 64])
        P.tt("dve", cr, rho, cth, ALU.mult)
        P.ts("dve", cr, cr, -1.0, ALU.add)
        P.tt("dve", ci, rho, sth, ALU.mult)
        P.tt("dve", den, lre, lre, ALU.mult)
        P.tt("dve", t1, lamim, lamim, ALU.mult)
        P.tt("dve", den, den, t1, ALU.add)
        P.recip(den, den)
        P.tt("dve", t1, cr, lre, ALU.mult)
        P.tt("dve", t2, ci, lamim, ALU.mult)
        P.tt("dve", t1, t1, t2, ALU.add)
        P.tt("dve", qr, t1, den, ALU.mult)
        P.tt("dve", t1, ci, lre, ALU.mult)
        P.tt("dve", t2, cr, lamim, ALU.mult)
        P.tt("dve", t1, t1, t2, ALU.subtract)
        P.tt("dve", qi, t1, den, ALU.mult)
        P.ts("dve", nqi, qi, -1.0, ALU.mult)
        tfull = sb("tfull", [128, S])
        with ExitStack() as es2:
            tfi = P.sb(es2, tag + "tfi", [128, S], I32)
            P.I("pool", "iota", out=tfi, pattern=[[1, S]], base=0, channel_multiplier=0)
            P.copy("dve", tfull, tfi)
            P.barrier()
        f4 = lambda n: sb(n, [128, TB])
        bpad = [[sb("bp%d%d" % (a_, b_), [128, 128]) for b_ in range(2)] for a_ in range(2)]
        cpad = [[sb("cp%d%d" % (a_, b_), [128, 128]) for b_ in range(2)] for a_ in range(2)]
        bbf = [[[sb("bb%d_%d_%d" % (q, g, b_), [128, 128], BF16) for b_ in range(2)] for g in range(4)] for q in range(2)]
        cbf = [[[sb("cb%d_%d_%d" % (q, g, b_), [128, 128], BF16) for b_ in range(2)] for g in range(4)] for q in range(2)]
        state = [[[sb("st%d_%d_%d" % (q, g, b_), [128, 1]) for b_ in range(2)] for g in range(4)] for q in range(2)]
        ctmp = sb("ctmp", [128, 128])
        ubf = [sb("ubf%d" % k, [128, TB], BF16) for k in range(2)]
        uf = [sb("uf%d" % k, [128, TB]) for k in range(2)]
        ang2, kq2 = [f4("angq%d" % q) for q in range(2)], [sb("kqq%d" % q, [128, TB], I32) for q in range(2)]
        tabs3, tabc3 = [f4("tabs%d" % q) for q in range(3)], [f4("tabc%d" % q) for q in range(3)]
        raw2 = [[f4("raw%d_%d" % (q, b_)) for b_ in range(2)] for q in range(2)]
        mW, mX = [f4("mW%d" % k) for k in range(4)], [f4("mX%d" % k) for k in range(4)]
        wr = [f4("w%d" % b_) for b_ in range(2)]
        xh2 = [[f4("xh%d_%d" % (q, b_)) for b_ in range(2)] for q in range(2)]
        X = [[sb("X%d_%d" % (k, b_), [128, TB], BF16) for b_ in range(2)] for k in range(2)]
        yv, x2, zz = f4("yv"), f4("x2"), [f4("zz%d" % k) for k in range(2)]
        ps_raw = [[P.ps(es, tag + "pr%d_%d" % (b_, n), [128, 512], F32) for n in range(NB)] for b_ in range(2)]
        ps_y = [P.ps(es, tag + "py%d" % n, [128, 512], F32) for n in range(NB)]
        NTB = S // TB
        iters = [(ct, tbi, g) for ct in range(16) for tbi in range(NTB) for g in range(4)]

        def prep_ct(ct):
            q = ct % 2
            for g in range(4):
                gp = ct * 4 + g
                bp, cp = bpad[g % 2], cpad[g % 2]
                P.dma("sp", bp[0], w["s5_bre_pad"][i, gp])
                P.dma("act", bp[1], w["s5_bim_pad"][i, gp])
                P.dma("sp", cp[0], w["s5_cre_pad"][i, gp])
                P.dma("act", cp[1], w["s5_cim_pad"][i, gp])
                P.copy("pool", bbf[q][g][0], bp[0])
                P.copy("pool", bbf[q][g][1], bp[1])
                P.ts("dve", ctmp, cp[1], qi[:, gp:gp + 1], ALU.mult)
                P.stt("dve", cbf[q][g][0], cp[0], qr[:, gp:gp + 1], ctmp, ALU.mult, ALU.subtract)
                P.ts("dve", ctmp, cp[1], qr[:, gp:gp + 1], ALU.mult)
                P.stt("dve", cbf[q][g][1], cp[0], nqi[:, gp:gp + 1], ctmp, ALU.mult, ALU.subtract)
                P.memset("dve", state[q][g][0], 0.0)
                P.memset("dve", state[q][g][1], 0.0)

        def stA(k):
            ct, tbi, g = iters[k]
            t0 = tbi * TB
            gp = ct * 4 + g
            ui = (ct * NTB + tbi) % 2
            if tbi == 0 and g == 0:
                prep_ct(ct)
            if g == 0:
                P.dma("sp", uf[ui], uT[ct * 128:(ct + 1) * 128, t0:t0 + TB])
                P.copy("act", ubf[ui], uf[ui])
            u_b = ubf[ui]
            for b_ in range(2):
                for n in range(NB):
                    P.mm(ps_raw[b_][n], bbf[ct % 2][g][b_], u_b[:, n * 512:(n + 1) * 512])
            ang, kq = ang2[k % 2], kq2[k % 2]
            P.act(ang, tfull[:, t0:t0 + TB], AF.Copy, scale=th[:, gp:gp + 1])
            sincos(P, ang, kq, ang, tabs3[k % 3], tabc3[k % 3], consts)
            raw = raw2[k % 2]
            for b_ in range(2):
                for n in range(NB):
                    P.copy("act", raw[b_][:, n * 512:(n + 1) * 512], ps_raw[b_][n])

        def stB(k):
            ct, tbi, g = iters[k]
            gp = ct * 4 + g
            tabs, tabc, raw, xh = tabs3[k % 3], tabc3[k % 3], raw2[k % 2], xh2[k % 2]
            P.tt("pool", mW[0], tabc, raw[0], ALU.mult)
            P.tt("pool", mW[1], tabs, raw[1], ALU.mult)
            P.tt("pool", mW[2], tabc, raw[1], ALU.mult)
            P.tt("dve", mW[3], tabs, raw[0], ALU.mult)
            P.tt("dve", wr[0], mW[0], mW[1], ALU.add)
            P.tt("dve", wr[1], mW[2], mW[3], ALU.subtract)
            rb = rho[:, gp:gp + 1].broadcast_to([128, TB])
            st_ = state[ct % 2][g]
            for b_ in range(2):
                P.scan(xh[b_], rb, wr[b_], st_[b_], ALU.mult, ALU.add)
                P.copy("dve", st_[b_], xh[b_][:, TB - 1:TB])

        def stC(k):
            ct, tbi, g = iters[k]
            t0 = tbi * TB
            ui = (ct * NTB + tbi) % 2
            tabs, tabc, xh = tabs3[k % 3], tabc3[k % 3], xh2[k % 2]
            P.tt("pool", mX[0], tabc, xh[0], ALU.mult)
            P.tt("pool", mX[1], tabs, xh[1], ALU.mult)
            P.tt("pool", mX[2], tabs, xh[0], ALU.mult)
            P.tt("dve", mX[3], tabc, xh[1], ALU.mult)
            Xg = X[k % 2]
            P.tt("dve", Xg[0], mX[0], mX[1], ALU.subtract)
            P.tt("dve", Xg[1], mX[2], mX[3], ALU.add)
            cb = cbf[ct % 2][g]
            for n in range(NB):
                P.mm(ps_y[n], cb[0], Xg[0][:, n * 512:(n + 1) * 512], start=(g == 0), stop=False)
                P.mm(ps_y[n], cb[1], Xg[1][:, n * 512:(n + 1) * 512], start=False, stop=(g == 3))
            if g == 3:
                u_f = uf[ui]
                for n in range(NB):
                    sl = slice(n * 512, (n + 1) * 512)
                    P.stt("dve", yv[:, sl], u_f[:, sl], dcol[:, ct:ct + 1], ps_y[n], ALU.mult, ALU.add)
                z = zz[(ct * NTB + tbi) % 2]
                P.act(x2, yv, AF.Square)
                P.ts("dve", x2, x2, 0.044715, ALU.mult, 1.0, ALU.add)
                P.tt("dve", x2, x2, yv, ALU.mult)
                P.act(x2, x2, AF.Sigmoid, scale=2.0 * float(np.sqrt(2.0 / np.pi)))
                P.tt("dve", z, yv, x2, ALU.mult)
                P.dma("sp", zT[ct * 128:(ct + 1) * 128, t0:t0 + TB], z)

        NI = len(iters)
        stA(0)
        if NI > 1:
            stA(1)
        stB(0)
        for k in range(NI):
            if k + 2 < NI:
                stA(k + 2)
            if k + 1 < NI:
                stB(k + 1)
            stC(k)
        P.barrier()


def odd_layer(P, xT, memT, w, l, sc, S, consts, tag):
    i = l // 2
    D = 2048
    st_norm(P, xT, w["norm_mix"][l], sc["xnT"], D, S, 1e-5, consts, tag + "n1")
    with ExitStack() as es:
        st_mm(P, sc["xnT"], D, S, [(w["odd_w_in"][i], D, EpiStore(P, es, sc["uT"], F32, tag + "eu1"))], consts, tag + "mi", wscr=sc["wscr"])
    st_s5_core(P, sc["uT"], sc["zT"], w, i, S, consts, tag + "s5")
    with ExitStack() as es:
        bcol = P.sb(es, tag + "bglu", [128, 16], F32)
        P.dma("sp", bcol, w["s5_bglu_pc"][i])
        st_mm(P, sc["zT"], D, S, [(w["s5_w_glu"][i], D, EpiGlu(P, es, sc["zT"], bcol, sc["xnT"], tag + "eg"))], consts,
              tag + "mg", a_dt=F32, TT=512, wscr=sc["wscr"])
    with ExitStack() as es:
        st_mm(P, sc["xnT"], D, S, [(w["odd_w_out"][i], D, EpiResid(P, es, xT, tag + "ro1"))], consts, tag + "mo1", wscr=sc["wscr"])
    layer_tail(P, xT, memT, w, l, sc, S, consts, tag)


def host_layout_s5(inp):
    o = {}
    n = inp["s5_lam_re"].shape[0]

    def pc(a):
        return np.ascontiguousarray(a.reshape(n, 64, 2, 64).transpose(0, 2, 3, 1).reshape(n, 128, 64))

    o["s5_lamre_pc"] = pc(inp["s5_lam_re"])
    o["s5_lamim_pc"] = pc(inp["s5_lam_im"])
    o["s5_logdt_pc"] = pc(np.repeat(inp["s5_log_dt"][:, :, None], 64, axis=2))
    bpad = np.zeros((2, n, 64, 128, 128), np.float32)
    cpad = np.zeros((2, n, 64, 128, 128), np.float32)
    for k, (bn, cn) in enumerate((("s5_b_re", "s5_c_re"), ("s5_b_im", "s5_c_im"))):
        b = inp[bn].reshape(n, 64, 2, 64, 16)
        c = inp[cn].reshape(n, 64, 2, 16, 64)
        for gp in range(64):
            gq = gp % 4
            for gl in range(2):
                r0 = gq * 32 + gl * 16
                bpad[k, :, gp, r0:r0 + 16, gl * 64:(gl + 1) * 64] = b[:, gp, gl].transpose(0, 2, 1)
                cpad[k, :, gp, gl * 64:(gl + 1) * 64, r0:r0 + 16] = c[:, gp, gl].transpose(0, 2, 1)
    o["s5_bre_pad"], o["s5_bim_pad"] = bpad[0], bpad[1]
    o["s5_cre_pad"], o["s5_cim_pad"] = cpad[0], cpad[1]
    o["s5_d_pc"] = colvec(inp["s5_d"])
    o["s5_bglu_pc"] = colvec(inp["s5_b_glu"])
    return o


def colvec(a, pp=128):
    sh = a.shape
    return np.ascontiguousarray(np.swapaxes(a.reshape(sh[:-1] + (sh[-1] // pp, pp)), -1, -2))


KAPPA = float(np.exp(-0.5))
RW_COLS = ["rwkv_w0", "rwkv_a0", "rwkv_k_k", "rwkv_k_a", "rwkv_r_k", "rwkv_ln_w", "rwkv_ln_b"]


def st_rwkv_prep(P, hBT, w, i, sc, S, consts, tag):
    TB = 256
    NJ = 8
    with ExitStack() as es:
        sb = lambda n, sh, dt=F32: P.sb(es, tag + n, sh, dt)
        cols = sb("cols", [128, 7, NJ, 1])
        P.dma("sp", cols, w["rwkv_cols"][i])
        w0c, a0c, kkc, kac, rkc = [cols[:, k] for k in range(5)]
        omka = sb("omka", [128, NJ, 1])
        P.ts("dve", omka, kac, -1.0, ALU.mult, 1.0, ALU.add)
        mu3 = sb("mu3", [128, 3, NJ, 1])
        P.dma("sp", mu3, w["rwkv_mu_rkv"][i])
        muw, mua, mug1, mug2 = sb("muw", [64, 1]), sb("mua", [64, 1]), sb("mug1", [128, 1]), sb("mug2", [32, 1])
        P.dma("sp", muw, w["rwkv_mu_wl"][i])
        P.dma("sp", mua, w["rwkv_mu_al"][i])
        P.dma("sp", mug1, w["rwkv_mu_g1"][i])
        P.dma("sp", mug2, w["rwkv_mu_g2"][i])
        w2f, a2f, g2f1, g2f2 = sb("w2f", [64, 1024]), sb("a2f", [64, 1024]), sb("g2f1", [128, 1024]), sb("g2f2", [32, 1024])
        w2b, a2b, g2b1, g2b2 = sb("w2b", [64, 1024], BF16), sb("a2b", [64, 1024], BF16), sb("g2b1", [128, 1024], BF16), sb("g2b2", [32, 1024], BF16)
        P.dma("sp", w2f, w["rwkv_w2"][i])
        P.dma("sp", a2f, w["rwkv_a2"][i])
        P.dma("sp", g2f1, w["rwkv_g2"][i][0:128, :])
        P.dma("sp", g2f2, w["rwkv_g2"][i][128:160, :])
        for f, b in ((w2f, w2b), (a2f, a2b), (g2f1, g2b1), (g2f2, g2b2)):
            P.copy("act", b, f)
        cmask = sb("cmask", [128, NJ, TB])
        P.memset("dve", cmask, 1.0)
        P.memset("dve", cmask.rearrange("p j (c t) -> p j c t", t=64)[:, :, :, 0:1], 0.0)
        bones = consts["blockones_f32"]
        A3, B3 = sb("A3", [128, NJ, TB]), sb("B3", [128, NJ, TB])
        hmr, hmk, hmv = sb("hmr", [128, NJ, TB]), sb("hmk", [128, NJ, TB]), sb("hmv", [128, NJ, TB])
        As, Bs = sb("As", [128, TB]), sb("Bs", [128, TB])
        twl, alb, sg1, sg2 = sb("twl", [64, TB], BF16), sb("alb", [64, TB], BF16), sb("sg1", [128, TB], BF16), sb("sg2", [32, TB], BF16)
        sig, av, gv = sb("sig", [128, NJ, TB]), sb("av", [128, NJ, TB]), sb("gv", [128, NJ, TB])
        cs, e1, e2, e3 = sb("cs", [128, NJ, TB]), sb("e1", [128, NJ, TB]), sb("e2", [128, NJ, TB]), sb("e3", [128, NJ, TB])
        kraw, kk, kp, tmp = sb("kraw", [128, NJ, TB]), sb("kk", [128, NJ, TB]), sb("kp", [128, NJ, TB]), sb("tmp", [128, NJ, TB])
        oA, oB, oK, oR, oV = [sb("o" + n, [128, NJ, TB], BF16) for n in "ABKRV"]
        pcs = sb("pcs", [128, NJ, TB // 64])
        ps = Ring([P.ps(es, tag + "ps%d" % k, [128, TB], F32) for k in range(6)])
        hv = lambda r0: hBT[r0:r0 + 1024, :].rearrange("(j p) s -> p j s", p=128)
        out3 = lambda T_: T_.rearrange("(j p) s -> p j s", p=128)

        def shift3(view, dst, mu_b, t0):
            P.dma("sp", A3, view[:, :, t0:t0 + TB])
            if t0 == 0:
                P.memset("dve", B3[:, :, 0:1], 0.0)
                P.dma("act", B3[:, :, 1:TB], view[:, :, 0:TB - 1])
            else:
                P.dma("act", B3, view[:, :, t0 - 1:t0 + TB - 1])
            P.tt("dve", B3, B3, A3, ALU.subtract)
            P.tt("pool", B3, B3, mu_b.broadcast_to([128, NJ, TB]), ALU.mult)
            P.tt("dve", dst, B3, A3, ALU.add)

        def shift2(r0, nr, mu_c, t0, func, dst):
            a, b = As[0:nr, :], Bs[0:nr, :]
            P.dma("sp", a, hBT[r0:r0 + nr, t0:t0 + TB])
            if t0 == 0:
                P.memset("dve", b[:, 0:1], 0.0)
                P.dma("act", b[:, 1:TB], hBT[r0:r0 + nr, 0:TB - 1])
            else:
                P.dma("act", b, hBT[r0:r0 + nr, t0 - 1:t0 + TB - 1])
            P.tt("dve", b, b, a, ALU.subtract)
            P.stt("dve", b, b, mu_c, a, ALU.mult, ALU.add)
            P.act(dst, b, func)

        for t0 in range(0, S, TB):
            shift3(hv(0), hmr, mu3[:, 0], t0)
            shift3(hv(1024), hmk, mu3[:, 1], t0)
            shift3(hv(2048), hmv, mu3[:, 2], t0)
            shift2(3072, 64, muw, t0, AF.Tanh, twl)
            shift2(3136, 64, mua, t0, AF.Copy, alb)
            shift2(3200, 128, mug1, t0, AF.Sigmoid, sg1)
            shift2(3328, 32, mug2, t0, AF.Sigmoid, sg2)
            for j in range(NJ):
                cj = slice(j * 128, (j + 1) * 128)
                p1 = ps.next()
                P.mm(p1, w2b[:, cj], twl)
                P.act(sig[:, j, :], p1, AF.Sigmoid, bias=w0c[:, j, :])
                p2 = ps.next()
                P.mm(p2, a2b[:, cj], alb)
                P.act(av[:, j, :], p2, AF.Sigmoid, bias=a0c[:, j, :])
                p3 = ps.next()
                P.mm(p3, g2b1[:, cj], sg1, start=True, stop=False)
                P.mm(p3, g2b2[:, cj], sg2, start=False, stop=True)
                P.copy("dve", gv[:, j, :], p3)
            P.dma("sp", out3(sc["gT"])[:, :, t0:t0 + TB], gv)
            P.scan(cs.rearrange("p j t -> p (j t)"), cmask.rearrange("p j t -> p (j t)"), sig.rearrange("p j t -> p (j t)"),
                   0.0, ALU.mult, ALU.add)
            P.tt("dve", tmp, cs, sig, ALU.subtract)
            P.act(e1, tmp, AF.Exp, scale=-KAPPA)
            P.act(e2, cs, AF.Exp, scale=KAPPA)
            P.act(e3, cs, AF.Exp, scale=-KAPPA)
            P.copy("dve", pcs, e3.rearrange("p j (c t) -> p j c t", t=64)[:, :, :, 63])
            P.dma("sp", out3(sc["PCT"])[:, :, t0 // 64:(t0 + TB) // 64], pcs)
            P.tt("pool", kraw, hmk, kkc.broadcast_to([128, NJ, TB]), ALU.mult)
            P.act(tmp, kraw, AF.Square)
            for j in range(NJ):
                p1 = ps.next()
                P.mm(p1, bones, tmp[:, j, :])
                P.ts("dve", kk[:, j, :], p1, 1e-24, ALU.max)
            P.act(kk, kk, AF.Sqrt)
            P.recip(kk, kk)
            P.tt("dve", kk, kk, kraw, ALU.mult)
            P.tt("pool", tmp, av, kac.broadcast_to([128, NJ, TB]), ALU.mult)
            P.tt("pool", tmp, tmp, omka.broadcast_to([128, NJ, TB]), ALU.add)
            P.tt("dve", kp, hmk, tmp, ALU.mult)
            P.stt("dve", oA, kk, -1.0, e1, ALU.mult, ALU.mult)
            P.tt("pool", tmp, kk, av, ALU.mult)
            P.tt("dve", oB, tmp, e2, ALU.mult)
            P.tt("dve", oK, kp, e2, ALU.mult)
            P.tt("dve", oR, hmr, e3, ALU.mult)
            P.copy("act", oV, hmv)
            for nm, o in (("AhT", oA), ("BhT", oB), ("KhT", oK), ("RhT", oR), ("vT", oV)):
                P.dma("sp", out3(sc[nm])[:, :, t0:t0 + TB], o)
            P.tt("pool", tmp, hmr, kp, ALU.mult)
            P.tt("pool", tmp, tmp, rkc.broadcast_to([128, NJ, TB]), ALU.mult)
            for j in range(NJ):
                p1 = ps.next()
                P.mm(p1, bones, tmp[:, j, :])
                P.tt("dve", e1[:, j, :], p1, hmv[:, j, :], ALU.mult)
            P.dma("sp", out3(sc["bonusT"])[:, :, t0:t0 + TB], e1)
        P.barrier()


def st_rwkv_A(P, sc, S, consts, tag):
    CB = 4
    NCH = S // 64
    with ExitStack() as es:
        sb = lambda n, sh, dt=F32: P.sb(es, tag + n, sh, dt)
        names = ["AhT", "BhT", "KhT", "RhT", "vT"]
        blk = [[sb("bl%d_%d" % (k, q), [64, 16, CB * 64], BF16) for q in range(5)] for k in range(2)]
        msu = consts["mask_su"].broadcast_to([64, 16, 64])
        miu = consts["mask_iu"].broadcast_to([64, 16, 64])
        msl = consts["mask_sl"].broadcast_to([64, 16, 64])
        idb = consts["ident64"].broadcast_to([64, 16, 64])
        identb = consts["ident_bf"]
        N32s = [sb("N32_%d" % q, [64, 16, 64]) for q in range(2)]
        T32s = [sb("T32_%d" % q, [64, 16, 64]) for q in range(2)]
        Nbs = [[sb("Nb%d_%d" % (q, k), [64, 16, 64], BF16) for k in range(2)] for q in range(2)]
        NTbs = [[sb("NTb%d_%d" % (q, k), [64, 16, 64], BF16) for k in range(2)] for q in range(2)]
        Tbs = [sb("Tb%d" % q, [64, 16, 64], BF16) for q in range(2)]
        mats = [sb("mats%d" % k, [64, 4, 16, 64], BF16) for k in range(2)]
        tok = [sb("tok%d" % k, [64, 3, 16, 64], BF16) for k in range(2)]
        pr = Ring([P.ps(es, tag + "pr%d" % k, [64, 16, 64], F32) for k in range(3)])
        pt = Ring([P.ps(es, tag + "pt%d" % k, [64, 16, 64], BF16) for k in range(2)])
        view = lambda nm: sc[nm].rearrange("(h c) s -> c h s", c=64)

        def mmh(out, L, R):
            for h in range(16):
                P.mm(out[:, h, :], L[:, h, :], R[:, h, :])

        def chunk_gen(c, bl):
            q = c % 2
            N32, T32, Nb, NTb, Tb = N32s[q], T32s[q], Nbs[q], NTbs[q], Tbs[q]
            cc = slice((c % CB) * 64, (c % CB + 1) * 64)
            Ah, Bh, Kh, Rh, Vh = [bl[k][:, :, cc] for k in range(5)]
            mt = mats[q]
            tk = tok[q]
            p = pr.next()
            mmh(p, Ah, Bh)
            P.tt("dve", NTb[0], p, msl, ALU.mult)
            yield
            p = pr.next()
            mmh(p, Bh, Ah)
            P.tt("dve", N32, p, msu, ALU.mult)
            P.copy("act", Nb[0], N32)
            P.tt("pool", T32, N32, idb, ALU.add)
            P.copy("act", Tb, T32)
            yield
            p = pr.next()
            mmh(p, Kh, Ah)
            P.tt("dve", mt[:, 1], p, msu, ALU.mult)
            yield
            p = pr.next()
            mmh(p, Bh, Rh)
            P.tt("dve", mt[:, 2], p, miu, ALU.mult)
            yield
            p = pr.next()
            mmh(p, Kh, Rh)
            P.tt("dve", mt[:, 3], p, miu, ALU.mult)
            yield
            cur = 0
            srcs = (Bh, Kh, Vh)
            for j in range(1, 6):
                nxt = 1 - cur
                p = pr.next()
                mmh(p, Nb[cur], NTb[cur])
                P.copy("dve", NTb[nxt], p)
                yield
                if j < 5:
                    p = pr.next()
                    mmh(p, NTb[cur], Nb[cur])
                    P.copy("act", Nb[nxt], p)
                    yield
                if j <= 3:
                    tp = pt.next()
                    for h in range(16):
                        P.tr(tp[:, h, :], srcs[j - 1][:, h, :], identb[0:64, 0:64])
                    P.copy("act", tk[:, j - 1], tp)
                    yield
                p = pr.next()
                mmh(p, NTb[nxt], Tb)
                P.tt("dve", T32, T32, p, ALU.add)
                if j < 5:
                    P.copy("act", Tb, T32)
                yield
                cur = nxt
            P.copy("act", mt[:, 0], T32)
            P.dma("sp", sc["mats"][c], mt)
            P.dma("act", sc["tok3"][c], tk)
            yield

        for c0 in range(0, NCH, 2):
            if c0 % CB == 0:
                bl = blk[(c0 // CB) % 2]
                for k, nm in enumerate(names):
                    P.dma("sp" if k % 2 == 0 else "act", bl[k], view(nm)[:, :, c0 * 64:(c0 + CB) * 64])
            gens = [chunk_gen(c0, bl)]
            if c0 + 1 < NCH:
                gens.append(chunk_gen(c0 + 1, bl))
            alive = True
            while alive:
                alive = False
                for g in gens:
                    try:
                        next(g)
                        alive = True
                    except StopIteration:
                        pass
        P.barrier()


def st_rwkv_B(P, sc, S, consts, tag):
    CB = 4
    NCH = S // 64
    with ExitStack() as es:
        sb = lambda n, sh, dt=F32: P.sb(es, tag + n, sh, dt)
        blk = [[sb("bl%d_%d" % (k, q), [64, 16, CB * 64], BF16) for q in range(2)] for k in range(2)]
        pc = sb("pc", [64, 16, NCH])
        P.dma("sp", pc, sc["PCT"].rearrange("(h c) n -> c h n", c=64))
        mats = [sb("mats%d" % k, [64, 4, 16, 64], BF16) for k in range(3)]
        tok = [sb("tok%d" % k, [64, 3, 16, 64], BF16) for k in range(3)]
        S32, Sb = sb("S32", [64, 16, 64]), sb("Sb", [64, 16, 64], BF16)
        tmp = sb("tmp", [64, 16, 64])
        WTb = sb("WTb", [64, 16, 64], BF16)
        UTb = sb("UTb", [64, 16, 64], BF16)
        O = [sb("O%d" % k, [64, 16, 64]) for k in range(2)]
        P.memset("dve", S32, 0.0)
        P.memset("dve", Sb, 0.0)
        pW, pU, pO, pS = [P.ps(es, tag + n, [64, 16, 64], F32) for n in ("pW", "pU", "pO", "pS")]
        view = lambda nm: sc[nm].rearrange("(h c) s -> c h s", c=64)
        otok = sc["otok"].rearrange("(n t) (h v) -> n t h v", t=64, v=64)
        for c in range(NCH):
            if c % CB == 0:
                bl = blk[(c // CB) % 2]
                P.dma("sp", bl[0], view("AhT")[:, :, c * 64:(c + CB) * 64])
                P.dma("sp", bl[1], view("RhT")[:, :, c * 64:(c + CB) * 64])
            cc = slice((c % CB) * 64, (c % CB + 1) * 64)
            Ah, Rh = bl[0][:, :, cc], bl[1][:, :, cc]
            mt, tk = mats[c % 3], tok[c % 3]
            P.dma("sp", mt, sc["mats"][c])
            P.dma("act", tk, sc["tok3"][c])
            Tm, Nka, Mbr, Mkr = [mt[:, q] for q in range(4)]
            bt, kt, vt = [tk[:, q] for q in range(3)]
            for h in range(16):
                P.mm(pW[:, h, :], Ah[:, h, :], Sb[:, h, :], start=True, stop=False)
                P.mm(pW[:, h, :], Nka[:, h, :], vt[:, h, :], start=False, stop=True)
            P.copy("act", WTb, pW)
            for h in range(16):
                P.mm(pU[:, h, :], Tm[:, h, :], WTb[:, h, :])
            P.copy("act", UTb, pU)
            for h in range(16):
                P.mm(pO[:, h, :], Rh[:, h, :], Sb[:, h, :], start=True, stop=False)
                P.mm(pO[:, h, :], Mbr[:, h, :], UTb[:, h, :], start=False, stop=False)
                P.mm(pO[:, h, :], Mkr[:, h, :], vt[:, h, :], start=False, stop=True)
            o = O[c % 2]
            P.copy("act", o, pO)
            P.dma("sp", otok[c], o)
            for h in range(16):
                P.mm(pS[:, h, :], bt[:, h, :], UTb[:, h, :], start=True, stop=False)
                P.mm(pS[:, h, :], kt[:, h, :], vt[:, h, :], start=False, stop=True)
            P.tt("dve", tmp, pS, S32, ALU.add)
            P.tt("dve", S32, tmp, pc[:, :, c:c + 1].broadcast_to([64, 16, 64]), ALU.mult)
            P.copy("act", Sb, S32)
        P.barrier()


def st_rwkv_post(P, w, i, sc, S, consts, tag):
    TB = min(512, S)
    with ExitStack() as es:
        sb = lambda n, sh, dt=F32: P.sb(es, tag + n, sh, dt)
        cols = sb("cols", [128, 7, 8, 1])
        P.dma("sp", cols, w["rwkv_cols"][i])
        lnw, lnb = cols[:, 5], cols[:, 6]
        bon = [sb("bon%d" % k, [128, 8, TB]) for k in range(2)]
        gt = [sb("g%d" % k, [128, 8, TB]) for k in range(2)]
        yo = [sb("yo%d" % k, [128, 8, TB], BF16) for k in range(2)]
        ot = [sb("ot%d" % k, [128, 16, 64]) for k in range(2)]
        xc, sq = sb("xc", [128, 16, 64]), sb("sq", [128, 16, 64])
        sm, vr = sb("sm", [128, 16, 1]), sb("vr", [128, 16, 1])
        y1 = sb("y1", [128, 128])
        pT = Ring([P.ps(es, tag + "pT%d" % k, [128, 128], F32) for k in range(4)])
        identf = consts["ident_f32"]
        eps = consts["eps_0.00064"]
        f3 = lambda T_: T_.rearrange("(j p) s -> p j s", p=128)
        yv = sc["yT"][1024:2048, :].rearrange("(j p) s -> p j s", p=128)
        otv = sc["otok"].rearrange("(n t) (h v) -> n t h v", t=128, v=64)
        for ib, t0 in enumerate(range(0, S, TB)):
            b_, g_, y_ = bon[ib % 2], gt[ib % 2], yo[ib % 2]
            P.dma("sp", b_, f3(sc["bonusT"])[:, :, t0:t0 + TB])
            P.dma("act", g_, f3(sc["gT"])[:, :, t0:t0 + TB])
            for k in range(TB // 128):
                n = (t0 // 128) + k
                o = ot[n % 2]
                P.dma("sp", o, otv[n])
                P.reduce(sm.rearrange("p h o -> p (h o)"), o, ALU.add)
                P.ts("dve", sm, sm, 1.0 / 64, ALU.mult)
                P.tt("dve", xc, o, sm.broadcast_to([128, 16, 64]), ALU.subtract)
                P.act(sq, xc, AF.Square)
                P.reduce(vr.rearrange("p h o -> p (h o)"), sq, ALU.add)
                P.act(vr, vr, AF.Sqrt, bias=eps, scale=1.0 / 64)
                P.recip(vr, vr)
                P.tt("dve", xc, xc, vr.broadcast_to([128, 16, 64]), ALU.mult)
                for j in range(8):
                    p = pT.next()
                    P.tr(p, xc[:, 2 * j:2 * j + 2, :].rearrange("p a v -> p (a v)"), identf)
                    ts_ = slice(k * 128, (k + 1) * 128)
                    P.ts("dve", y1, p, lnw[:, j, :], ALU.mult, lnb[:, j, :], ALU.add)
                    P.tt("pool", y1, y1, b_[:, j, ts_], ALU.add)
                    P.tt("dve", y_[:, j, ts_], y1, g_[:, j, ts_], ALU.mult)
            P.dma("sp", yv[:, :, t0:t0 + TB], y_)
        P.barrier()


def host_layout_rwkv(inp):
    o = {}
    n = inp["rwkv_mu"].shape[0]
    mu = inp["rwkv_mu"]
    o["rwkv_mu_rkv"] = np.ascontiguousarray(np.stack([colvec(mu[:, k * 1024:(k + 1) * 1024]) for k in range(3)], 2)[..., None])
    o["rwkv_mu_wl"] = np.ascontiguousarray(mu[:, 3072:3136, None])
    o["rwkv_mu_al"] = np.ascontiguousarray(mu[:, 3136:3200, None])
    o["rwkv_mu_g1"] = np.ascontiguousarray(mu[:, 3200:3328, None])
    o["rwkv_mu_g2"] = np.ascontiguousarray(mu[:, 3328:3360, None])
    o["rwkv_cols"] = np.ascontiguousarray(np.stack([colvec(inp[k].reshape(n, 1024)) for k in RW_COLS], 2)[..., None])
    for k in ("rwkv_w2", "rwkv_a2", "rwkv_g2"):
        o[k] = inp[k]
    return o


def rwkv_scratch(nc, S, pre="", kind="Internal"):
    NCH = S // 64
    sc = {}
    _dram = dram
    dram_ = lambda nc_, n_, sh_, dt_: _dram(nc_, n_, sh_, dt_, kind)
    for nm in ("AhT", "BhT", "KhT", "RhT", "vT"):
        sc[nm] = dram_(nc, pre + nm, [1024, S], BF16)
    sc["PCT"] = dram_(nc, pre + "PCT", [1024, NCH], F32)
    sc["gT"] = dram_(nc, pre + "gT", [1024, S], F32)
    sc["bonusT"] = dram_(nc, pre + "bonusT", [1024, S], F32)
    sc["mats"] = dram_(nc, pre + "mats", [NCH, 64, 4, 16, 64], BF16)
    sc["tok3"] = dram_(nc, pre + "tok3", [NCH, 64, 3, 16, 64], BF16)
    sc["otok"] = dram_(nc, pre + "otok", [S, 1024], F32)
    return sc


def st_rope_tables(P, start_col, sc, S, consts, tag):
    TB = min(2048, S)
    with ExitStack() as es:
        sb = lambda n, sh, dt=F32: P.sb(es, tag + n, sh, dt)
        st_i, st_f = sb("sti", [128, 1], I32), sb("stf", [128, 1])
        P.dma("sp", st_i, start_col)
        P.copy("dve", st_f, st_i)
        ti, pos = sb("ti", [128, TB], I32), sb("pos", [128, TB])
        ang, kq, sn, cs = sb("ang", [128, TB]), sb("kq", [128, TB], I32), sb("sn", [128, TB]), sb("cs", [128, TB])
        for t0 in range(0, S, TB):
            P.I("pool", "iota", out=ti, pattern=[[1, TB]], base=t0, channel_multiplier=0)
            P.copy("dve", pos, ti)
            P.ts("dve", pos, pos, st_f, ALU.add)
            for nm, fq in (("A", consts["freqA"]), ("I", consts["freqI"])):
                P.ts("dve", ang, pos, fq, ALU.mult)
                sincos(P, ang, kq, ang, sn, cs, consts)
                P.dma("sp", sc["cos" + nm][:, t0:t0 + TB], cs)
                P.dma("sp", sc["sin" + nm][:, t0:t0 + TB], sn)
        P.barrier()


def st_dsa_kprep(P, sc, S, consts, tag):
    TB = 512
    with ExitStack() as es:
        sb = lambda n, sh, dt=F32: P.sb(es, tag + n, sh, dt)
        ring = lambda n, sh, dt=F32, k=2: Ring([P.sb(es, tag + n + str(q), sh, dt) for q in range(k)])
        kr, ki = ring("kr", [32, TB]), ring("ki", [64, TB])
        cA, sA, cI, sI = ring("cA", [32, TB]), ring("sA", [32, TB]), ring("cI", [64, TB]), ring("sI", [64, TB])
        t1, t2 = ring("t1", [64, TB]), ring("t2", [64, TB])
        okr, oki = ring("okr", [32, TB], BF16), ring("oki", [64, TB], BF16)
        ckv = ring("ckv", [128, 2, TB], BF16)
        ctok = ring("ctok", [128, TB // 128, 256], BF16)
        pr = Ring([P.ps(es, tag + "pr%d" % q, [64, TB], F32) for q in range(2)])
        pt = Ring([P.ps(es, tag + "pt%d" % q, [128, 2, 128], BF16) for q in range(2)])
        rotA, rotI = consts["rotA32"], consts["rotI128"]
        identb = consts["ident_bf"]
        for t0 in range(0, S, TB):
            ts_ = slice(t0, t0 + TB)
            for (src, x, c, s, cn, sn_, rot, npart, o, dst) in (
                    (sc["kropeT"], kr.next(), cA.next(), sA.next(), "cosA", "sinA", rotA, 32, okr.next(), sc["kvcatT"][256:288, :]),
                    (sc["kidxnT"], ki.next(), cI.next(), sI.next(), "cosI", "sinI", rotI, 64, oki.next(), sc["kidxT"])):
                P.dma("sp", x, src[:, ts_])
                P.dma("act", c, sc[cn][0:npart, ts_])
                P.dma("act", s, sc[sn_][0:npart, ts_])
                p = pr.next()
                P.mm(p[0:npart, :], rot[0:npart, 0:npart], x)
                a, b = t1.next()[0:npart, :], t2.next()[0:npart, :]
                P.tt("dve", a, p[0:npart, :], s, ALU.mult)
                P.tt("pool", b, x, c, ALU.mult)
                P.tt("dve", o, a, b, ALU.add)
                P.dma("sp", dst[:, ts_], o)
            ck = ckv.next()
            P.dma("sp", ck, sc["ckvnT"].rearrange("(c p) s -> p c s", p=128)[:, :, ts_])
            P.dma("act", sc["kvcatT"][0:256, :].rearrange("(c p) s -> p c s", p=128)[:, :, ts_], ck)
            ct = ctok.next()
            for k in range(TB // 128):
                tp = pt.next()
                for c2 in range(2):
                    P.tr(tp[:, c2, :], ck[:, c2, k * 128:(k + 1) * 128], identb)
                P.copy("act", ct[:, k, :].rearrange("p (c r) -> p c r", c=2), tp)
            P.dma("sp", sc["ckvtok"].rearrange("(n p) r -> p n r", p=128)[:, t0 // 128:(t0 + TB) // 128, :], ct)
        P.barrier()


def st_dsa_qprep(P, w, i, sc, S, consts, tag):
    TB = 512
    with ExitStack() as es:
        sb = lambda n, sh, dt=F32: P.sb(es, tag + n, sh, dt)
        ring = lambda n, sh, dt=F32, k=2: Ring([P.sb(es, tag + n + str(q), sh, dt) for q in range(k)])
        wf = sb("wf", [128, 4, 1024])
        wuq, wqi = sb("wuq", [128, 4, 1024], BF16), sb("wqi", [128, 4, 1024], BF16)
        P.dma("sp", wf, w["dsa_w_uq"][i].rearrange("(c p) h d -> p c (h d)", p=128))
        P.copy("act", wuq, wf)
        P.dma("sp", wf, w["dsa_w_qidx"][i].rearrange("(c p) h d -> p c (h d)", p=128))
        P.copy("act", wqi, wf)
        wkf = sb("wkf", [128, 8, 256])
        wuk = sb("wuk", [128, 8, 256], BF16)
        P.dma("sp", wkf, w["dsa_wukT_pad"][i].rearrange("h d r -> d h r"))
        P.copy("act", wuk, wkf)
        cq = ring("cq", [128, 4, TB], BF16)
        cA, sA, cI, sI = ring("cA", [32, TB]), ring("sA", [32, TB]), ring("cI", [128, TB]), ring("sI", [128, TB])
        qh = ring("qh", [128, TB], BF16)
        x32 = ring("x32", [32, TB])
        xi = ring("xi", [128, TB])
        t1, t2 = ring("t1", [128, TB]), ring("t2", [128, TB])
        ol = ring("ol", [128, 2, TB], BF16, 3)
        orp = ring("orp", [32, TB], BF16, 3)
        oi = ring("oi", [128, TB], BF16, 3)
        pq = Ring([P.ps(es, tag + "pq%d" % q, [128, TB], F32) for q in range(3)])
        prr = Ring([P.ps(es, tag + "prr%d" % q, [128, TB], F32) for q in range(2)])
        pl = Ring([P.ps(es, tag + "pl%d" % q, [128, TB], F32) for q in range(3)])
        rotA, rotI = consts["rotA32"], consts["rotI128"]
        for t0 in range(0, S, TB):
            ts_ = slice(t0, t0 + TB)
            c = cq.next()
            P.dma("sp", c, sc["cqnT"].rearrange("(c p) s -> p c s", p=128)[:, :, ts_])
            ca, sa, ci, si = cA.next(), sA.next(), cI.next(), sI.next()
            P.dma("act", ca, sc["cosA"][0:32, ts_])
            P.dma("act", sa, sc["sinA"][0:32, ts_])
            P.dma("act", ci, sc["cosI"][:, ts_])
            P.dma("act", si, sc["sinI"][:, ts_])
            import os
            dbg = int(os.environ.get("QDBG", "511"))
            for h in range(8 if dbg & 1 else 0):
                p = pq.next()
                for k in range(4):
                    P.mm(p, wuq[:, k, h * 128:(h + 1) * 128], c[:, k, :], start=(k == 0), stop=(k == 3))
                q_ = qh.next()
                P.copy("act", q_, p)
                if dbg & 4:
                    x = x32.next()
                    P.copy("dve", x, p[0:32, :])
                    pr_ = prr.next()
                    if dbg & 32:
                        P.mm(pr_[0:32, :], rotA, x)
                    a, b = t1.next()[0:32, :], t2.next()[0:32, :]
                    if dbg & 64:
                        P.tt("dve", a, pr_[0:32, :], sa, ALU.mult)
                    if dbg & 128:
                        P.tt("pool", b, x, ca, ALU.mult)
                    o_r = orp.next()
                    if dbg & 256:
                        P.tt("dve", o_r, a, b, ALU.add)
                    if dbg & 16:
                        P.dma("sp", sc["qcatT"][h, 256:288, ts_], o_r)
                o_l = ol.next()
                for c2 in range(2 if dbg & 8 else 0):
                    p2 = pl.next()
                    P.mm(p2, wuk[:, h, c2 * 128:(c2 + 1) * 128], q_)
                    P.copy("act" if c2 == 0 else "dve", o_l[:, c2, :], p2)
                if dbg & 8:
                    P.dma("sp", sc["qcatT"][h, 0:256, ts_].rearrange("(c p) s -> p c s", p=128), o_l)
            for j in range(8 if dbg & 2 else 0):
                p = pq.next()
                for k in range(4):
                    P.mm(p, wqi[:, k, j * 128:(j + 1) * 128], c[:, k, :], start=(k == 0), stop=(k == 3))
                x = xi.next()
                P.copy("act", x, p)
                pr_ = prr.next()
                P.mm(pr_, rotI, x)
                a, b = t1.next(), t2.next()
                P.tt("dve", a, pr_, si, ALU.mult)
                P.tt("pool", b, x, ci, ALU.mult)
                o_i = oi.next()
                P.tt("dve", o_i, a, b, ALU.add)
                P.dma("sp", sc["qidxT"][j * 128:(j + 1) * 128, ts_], o_i)
        P.barrier()


NEG_MASK = -30000.0


def st_dsa_attn(P, w, i, sc, S, consts, tag):
    QT = 128
    NQ = S // QT
    TOPK = min(256, S // 4)
    NS = 20
    scale = 128.0 ** -0.5
    with ExitStack() as es:
        sb = lambda n, sh, dt=F32: P.sb(es, tag + n, sh, dt)
        ring = lambda n, sh, dt=F32, k=2: Ring([P.sb(es, tag + n + str(q), sh, dt) for q in range(k)])
        kidx = sb("kidx", [64, S], BF16)
        kvc = sb("kvc", [128, 3, S], BF16)
        ctok = sb("ctok", [128, S // 128, 256], BF16)
        P.dma("sp", kidx, sc["kidxT"])
        P.dma("sp", kvc[:, 0:2, :], sc["kvcatT"][0:256, :].rearrange("(c p) s -> p c s", p=128))
        P.dma("sp", kvc[0:32, 2, :], sc["kvcatT"][256:288, :])
        P.dma("sp", ctok, sc["ckvtok"].rearrange("(n p) r -> p n r", p=128))
        wuv = sb("wuv", [128, 2, 1024], BF16)
        with ExitStack() as es2:
            wvf = P.sb(es2, tag + "wvf", [128, 2, 1024], F32)
            P.dma("sp", wvf, w["dsa_w_uv"][i].rearrange("(c p) h d -> p c (h d)", p=128))
            P.copy("act", wuv, wvf)
            P.barrier()
        qidx = [sb("qidx%d" % q, [64, 16, QT], BF16) for q in range(2)]
        qcat = [sb("qcat%d" % q, [128, 8, 3, QT], BF16) for q in range(2)]
        wT = [sb("wT%d" % q, [16, QT]) for q in range(2)]
        wtok = sb("wtok", [128, 16])
        Dm = sb("Dm", [128, 16, 128], BF16)
        rl = ring("rl", [128, 512], BF16, 4)
        scb = [sb("scb%d" % q, [128, S]) for q in range(2)]
        madd = [sb("madd%d" % q, [128, S], BF16) for q in range(2)]
        junk = sb("junk", [128, S], BF16)
        sm = [sb("sm%d" % q, [128, S]) for q in range(1)] * 2
        pb = [sb("pb%d" % q, [128, S], BF16) for q in range(2)]
        pTs = [sb("pTs%d" % q, [128, S // 128, 128], BF16) for q in range(2)]
        lo = [sb("lo%d" % q, [128, 1]) for q in range(2)]
        whalf = [sb("whalf%d" % q, [128, NS]) for q in range(2)]
        mxs, mns, w0s = sb("mxs", [128, 1]), sb("mns", [128, 1]), sb("w0s", [128, 1])
        mid, cnt, inc = sb("mid", [128, 1]), sb("cnt", [128, 1]), sb("inc", [128, 1])
        mx, rs = ring("mx", [128, 1]), ring("rs", [128, 1])
        olat = ring("olat", [128, 256], BF16)
        oT = ring("oT", [128, 2, 128], BF16)
        ya = ring("ya", [128, 8, QT], BF16)
        p_l = Ring([P.ps(es, tag + "pl%d" % q, [128, 512], F32) for q in range(2)])
        p_sc = P.ps(es, tag + "psc", [128, 512], F32)
        p_qk = Ring([P.ps(es, tag + "pqk%d" % q, [128, 512], F32) for q in range(2)])
        p_ts = Ring([P.ps(es, tag + "ptr%d" % q, [128, 4, 128], BF16) for q in range(2)])
        p_m = P.ps(es, tag + "pm", [128, 512], F32)
        p_mb = p_m.bitcast(BF16)
        p_o = p_m[:, 256:512]
        identb, identf = consts["ident_bf"], consts["ident_f32"]
        pow2 = consts["pow2row"]

        def idx_phase(qt):
            s_ = qt % 2
            t0 = qt * QT
            L = t0 + QT
            qi_, qc_, wT_ = qidx[s_], qcat[s_], wT[s_]
            P.dma("sp", qi_, sc["qidxT"].rearrange("(h d) s -> d h s", d=64)[:, :, t0:t0 + QT])
            for c2 in range(2):
                P.dma("act", qc_[:, :, c2, :], sc["qcatT"][:, c2 * 128:(c2 + 1) * 128, t0:t0 + QT].rearrange("h p s -> p h s"))
            P.dma("act", qc_[0:32, :, 2, :], sc["qcatT"][:, 256:288, t0:t0 + QT].rearrange("h p s -> p h s"))
            md = madd[s_]
            if t0 < TOPK:
                def gen0():
                    P.memset("dve", md[:, 0:L], 0.0)
                    P.memset("dve", md[0:64, t0 + 64:t0 + 128], NEG_MASK)
                    yield
                return gen0()
            P.dma("sp", wT_, sc["widxT"][:, t0:t0 + QT])
            P.tr(p_m[:, 0:16], wT_, identf[0:16, 0:16])
            P.ts("dve", wtok, p_m[:, 0:16], 1.0 / 32.0, ALU.mult)
            for h in range(16):
                P.ts("dve" if h % 2 else "pool", Dm[:, h, :], identb, wtok[:, h:h + 1], ALU.mult)
            sc_ = scb[s_]
            for kb in range(0, L, 512):
                kw = min(512, L - kb)
                rprev = None
                for h in range(17):
                    if h < 16:
                        p = p_l.next()
                        P.mm(p[:, 0:kw], qi_[:, h, :], kidx[:, kb:kb + kw])
                        r = rl.next()
                        P.act(r[:, 0:kw], p[:, 0:kw], AF.Relu)
                    if rprev is not None:
                        P.mm(p_sc[:, 0:kw], Dm[:, h - 1, :], rprev[:, 0:kw], start=(h == 1), stop=(h == 16))
                    rprev = r
                P.copy("act", sc_[:, kb:kb + kw], p_sc[:, 0:kw])

            def gen():
                lo_, wh = lo[s_], whalf[s_]
                P.reduce(mxs, sc_[:, 0:L], ALU.max)
                yield
                P.reduce(mns, sc_[:, 0:L], ALU.min)
                P.memset("dve", sc_[0:64, t0 + 64:t0 + 128], -1e30)
                P.ts("dve", lo_, mns, -1.0, ALU.add)
                P.tt("dve", w0s, mxs, lo_, ALU.subtract)
                P.ts("dve", wh, pow2[:, 0:NS], w0s, ALU.mult)
                yield
                for k in range(NS):
                    P.tt("dve", mid, lo_, wh[:, k:k + 1], ALU.add)
                    P.ts("dve", junk[:, 0:L], sc_[:, 0:L], mid, ALU.is_gt, 0.0, ALU.add, accum_out=cnt)
                    P.stt("dve", inc, cnt, TOPK - 0.5, wh[:, k:k + 1], ALU.is_gt, ALU.mult)
                    P.tt("dve", lo_, lo_, inc, ALU.add)
                    yield
                P.ts("dve", md[:, 0:L], sc_[:, 0:L], lo_, ALU.is_le, NEG_MASK, ALU.mult)
                yield
            return gen()

        ycur = {}
        olats = {}

        def stage1(qt, h):
            s_ = qt % 2
            t0 = qt * QT
            L = t0 + QT
            qc_, md = qcat[s_], madd[s_]
            hb = h % 2
            sm_, pb_ = sm[hb], pb[hb]
            for kb in range(0, L, 512):
                kw = min(512, L - kb)
                p = p_qk.next()
                P.mm(p[:, 0:kw], qc_[:, h, 0, :], kvc[:, 0, kb:kb + kw], start=True, stop=False)
                P.mm(p[:, 0:kw], qc_[:, h, 1, :], kvc[:, 1, kb:kb + kw], start=False, stop=False)
                P.mm(p[:, 0:kw], qc_[0:32, h, 2, :], kvc[0:32, 2, kb:kb + kw], start=False, stop=True)
                P.stt("dve", sm_[:, kb:kb + kw], p[:, 0:kw], scale, md[:, kb:kb + kw], ALU.mult, ALU.add)
            mx_, rs_ = mxr[hb], rsr[hb]
            P.reduce(mx_, sm_[:, 0:L], ALU.max)
            P.ts("dve", mx_, mx_, -1.0, ALU.mult)
            P.act(pb_[:, 0:L], sm_[:, 0:L], AF.Exp, bias=mx_, accum_out=rs_)
            P.recip(rs_, rs_)

        def stage2(qt, h):
            t0 = qt * QT
            L = t0 + QT
            hb = h % 2
            pb_, pT_, rs_ = pb[hb], pTs[hb], rsr[hb]
            if h == 0:
                ycur[qt] = ya.next()
            y_ = ycur[qt]
            nb = L // 128
            for b0 in range(0, nb, 4):
                nn = min(4, nb - b0)
                p_t = p_ts.next()
                for b in range(nn):
                    P.tr(p_t[:, b, :], pb_[:, (b0 + b) * 128:(b0 + b + 1) * 128], identb)
                P.copy("act", pT_[:, b0:b0 + nn, :], p_t[:, 0:nn, :])
            for b in range(nb):
                P.mm(p_o, pT_[:, b, :], ctok[:, b, :], start=(b == 0), stop=(b == nb - 1))
            ol_ = olat.next()
            P.ts("dve", ol_, p_o, rs_, ALU.mult)
            olats[(qt, h)] = ol_

        def stage3(qt, h):
            t0 = qt * QT
            y_ = ycur[qt]
            ol_ = olats.pop((qt, h))
            oT_ = oT.next()
            for c2 in range(2):
                P.tr(p_mb[:, c2 * 128:(c2 + 1) * 128], ol_[:, c2 * 128:(c2 + 1) * 128], identb)
            P.copy("act", oT_.rearrange("p c q -> p (c q)"), p_mb[:, 0:256])
            for c2 in range(2):
                P.mm(p_m[:, 128:256], wuv[:, c2, h * 128:(h + 1) * 128], oT_[:, c2, :], start=(c2 == 0), stop=(c2 == 1))
            P.copy("act", y_[:, h, :], p_m[:, 128:256])
            if h == 7:
                P.dma("sp", sc["yT"][0:1024, t0:t0 + QT].rearrange("(h d) s -> d h s", d=128), y_)
                del ycur[qt]

        mxr = [sb("mxr%d" % q, [128, 1]) for q in range(2)]
        rsr = [sb("rsr%d" % q, [128, 1]) for q in range(2)]
        items = [(qt, h) for qt in range(NQ) for h in range(8)]
        g = idx_phase(0)
        for _ in g:
            pass
        g = None
        stage1(0, 0)
        for k, (qt, h) in enumerate(items):
            stage2(qt, h)
            if k >= 1:
                stage3(*items[k - 1])
            if k + 1 < len(items):
                nqt, nh = items[k + 1]
                if nh == 0 and g is not None:
                    for _ in g:
                        pass
                    g = None
                stage1(nqt, nh)
            if h == 0 and qt + 1 < NQ:
                g = idx_phase(qt + 1)
            if g is not None:
                for _ in range(4):
                    next(g, None)
        stage3(*items[-1])
        P.barrier()


def host_layout_dsa(inp):
    o = {}
    n = inp["dsa_w_uk"].shape[0]
    wuk = inp["dsa_w_uk"]
    pad = np.zeros((n, 8, 128, 256), np.float32)
    pad[:, :, 32:, :] = wuk.transpose(0, 2, 3, 1)
    o["dsa_wukT_pad"] = pad
    o["dsa_cq_norm_pc"] = colvec(inp["dsa_cq_norm"])
    o["dsa_ckv_norm_pc"] = colvec(inp["dsa_ckv_norm"])
    o["dsa_kidx_norm_pc"] = colvec(inp["dsa_kidx_norm"], 64)
    for k in ("dsa_w_uq", "dsa_w_uv", "dsa_w_qidx"):
        o[k] = inp[k]
    return o


def dsa_scratch(nc, S, pre="", kind="Internal"):
    d = lambda n, sh, dt: dram(nc, pre + n, sh, dt, kind)
    sc = {}
    for nm in ("cosA", "sinA", "cosI", "sinI"):
        sc[nm] = d(nm, [128, S], F32)
    sc["cqT"] = d("cqT", [512, S], F32)
    sc["ckvT"] = d("ckvT", [256, S], F32)
    sc["kropeT"] = d("kropeT", [32, S], F32)
    sc["kidxrT"] = d("kidxrT", [64, S], F32)
    sc["widxT"] = d("widxT", [16, S], F32)
    sc["cqnT"] = d("cqnT", [512, S], BF16)
    sc["ckvnT"] = d("ckvnT", [256, S], BF16)
    sc["kidxnT"] = d("kidxnT", [64, S], F32)
    sc["kvcatT"] = d("kvcatT", [288, S], BF16)
    sc["kidxT"] = d("kidxT", [64, S], BF16)
    sc["ckvtok"] = d("ckvtok", [S, 256], BF16)
    sc["qcatT"] = d("qcatT", [8, 288, S], BF16)
    sc["qidxT"] = d("qidxT", [1024, S], BF16)
    return sc


def dsa_mixer_stages(P, w, i, sc, S, consts, tag):
    st_norm(P, sc["cqT"], w["dsa_cq_norm_pc"][i], sc["cqnT"], 512, S, 1e-5, consts, tag + "nq")
    st_norm(P, sc["ckvT"], w["dsa_ckv_norm_pc"][i], sc["ckvnT"], 256, S, 1e-5, consts, tag + "nk")
    st_norm(P, sc["kidxrT"], w["dsa_kidx_norm_pc"][i], sc["kidxnT"], 64, S, 1e-5, consts, tag + "ni", out_dt=F32)
    st_dsa_kprep(P, sc, S, consts, tag + "kp")
    st_dsa_qprep(P, w, i, sc, S, consts, tag + "qp")
    st_dsa_attn(P, w, i, sc, S, consts, tag + "at")


A_SPLITS = [("cqT", 0, 512), ("ckvT", 512, 256), ("kropeT", 768, 32), ("kidxrT", 800, 64), ("widxT", 864, 16)]


def even_layer(P, xT, memT, w, l, sc, S, consts, tag):
    i = l // 2
    D = 2048
    st_norm(P, xT, w["norm_mix"][l], sc["xnT"], D, S, 1e-5, consts, tag + "n1")
    with ExitStack() as es:
        jobs = []
        Win = w["even_w_in"][i]
        for nm, c0, n in A_SPLITS:
            jobs.append((Win[:, c0:c0 + n], n, EpiStore(P, es, sc[nm], F32, tag + "e" + nm, nbuf=2)))
        jobs.append((Win[:, 880:4240], 3360, EpiStore(P, es, sc["hBT"], F32, tag + "ehB")))
        st_mm(P, sc["xnT"], D, S, jobs, consts, tag + "mi", wscr=sc["wscr"])
    dsa_mixer_stages(P, w, i, sc, S, consts, tag + "d")
    st_rwkv_prep(P, sc["hBT"], w, i, sc, S, consts, tag + "rp")
    st_rwkv_A(P, sc, S, consts, tag + "ra")
    st_rwkv_B(P, sc, S, consts, tag + "rb")
    st_rwkv_post(P, w, i, sc, S, consts, tag + "rq")
    with ExitStack() as es:
        st_mm(P, sc["yT"], D, S, [(w["even_w_out"][i], D, EpiResid(P, es, xT, tag + "rmo"))], consts, tag + "mo", wscr=sc["wscr"])
    layer_tail(P, xT, memT, w, l, sc, S, consts, tag)


PLAIN_W = ["xattn_wq", "xattn_wkv", "xattn_wo", "ffn_up", "ffn_down", "even_w_in", "even_w_out",
           "odd_w_in", "odd_w_out", "s5_w_glu"]


def host_layout_all(inp):
    o = {}
    for k in PLAIN_W:
        o[k] = np.ascontiguousarray(inp[k], dtype=np.float32)
    for k in ("norm_mix", "norm_xattn", "norm_mem", "norm_ffn"):
        o[k] = colvec(np.asarray(inp[k], np.float32))
    o["final_norm"] = colvec(np.asarray(inp["final_norm"], np.float32))
    f = {k: np.asarray(v, np.float32) for k, v in inp.items() if k.startswith(("s5_", "rwkv_", "dsa_"))}
    o.update(host_layout_s5(f))
    o.update(host_layout_rwkv(f))
    o.update(host_layout_dsa(f))
    return o


def build_program(S, wshapes, depth=4, dbg_out=None):
    nc = bass.Bass("TRN2", target_bir_lowering=False)
    D = 2048
    xin = dram(nc, "xT_in", [D, S], F32, "ExternalInput")
    memT = dram(nc, "memT", [D, 256], F32, "ExternalInput")
    start = dram(nc, "start_col", [128, 1], I32, "ExternalInput")
    outT = dram(nc, "outT", [D, S], F32, "ExternalOutput")
    w = {k: dram(nc, k, list(sh), F32, "ExternalInput") for k, sh in wshapes.items()}
    hc = host_consts()
    cd = {k: dram(nc, "c_" + k, list(v.shape), CONST_SPECS[k][1], "ExternalInput") for k, v in hc.items()}
    sc = {}
    sc.update(dsa_scratch(nc, S))
    sc.update(rwkv_scratch(nc, S))
    sc["xT"] = dram(nc, "xres", [D, S], F32)
    sc["xnT"] = dram(nc, "xnT", [D, S], BF16)
    sc["hBT"] = dram(nc, "hBT", [3360, S], F32)
    sc["yT"] = dram(nc, "yT", [D, S], BF16)
    sc["uT"] = dram(nc, "uT", [D, S], F32)
    sc["zT"] = dram(nc, "zT", [D, S], F32)
    sc["memnT"] = dram(nc, "memnT", [D, 256], BF16)
    sc["qT"] = dram(nc, "qT", [512, S], BF16)
    sc["kT"] = dram(nc, "kT", [512, 256], BF16)
    sc["vtok"] = dram(nc, "vtok", [256, 512], BF16)
    sc["oT"] = dram(nc, "oT", [512, S], BF16)
    sc["hT"] = dram(nc, "hT", [8192, S], BF16)
    sc["wscr"] = dram(nc, "wscr", [20 * 1024 * 1024], BF16)
    with ExitStack() as es:
        P = Prog(nc, es)
        consts = make_consts(P, es, cd)
        xT = sc["xT"]
        for c in range(16):
            P.dma("sp" if c % 2 == 0 else "act", xT[c * 128:(c + 1) * 128, :], xin[c * 128:(c + 1) * 128, :])
        P.barrier()
        st_rope_tables(P, start, sc, S, consts, "rt")
        for l in range(depth):
            if l % 2 == 0:
                even_layer(P, xT, memT, w, l, sc, S, consts, "L%d" % l)
            else:
                odd_layer(P, xT, memT, w, l, sc, S, consts, "L%d" % l)
        st_norm(P, xT, w["final_norm"], outT, D, S, 1e-5, consts, "fn", out_dt=F32)
        P.barrier()
        nc._ninst = P.ninst
    return nc


_CACHE = {}


def kernel(**inputs):
    x = np.asarray(inputs["x"], np.float32)
    mem = np.asarray(inputs["mem"], np.float32)
    start = np.asarray(inputs["start_frame"]).astype(np.int32)
    B, S, D = x.shape
    hl = host_layout_all(inputs)
    hc = host_consts()
    wshapes = {k: v.shape[1:] if False else v.shape for k, v in hl.items()}
    nc = build_program(S, wshapes)
    in_maps = []
    for b in range(B):
        m = {"xT_in": np.ascontiguousarray(x[b].T), "memT": np.ascontiguousarray(mem[b].T),
             "start_col": np.full((128, 1), start[b], np.int32)}
        m.update(hl)
        for k, v in hc.items():
            m["c_" + k] = v
        in_maps.append(m)
    res = run_bass_kernel_spmd(nc, in_maps, core_ids=list(range(B)))
    out = np.stack([np.ascontiguousarray(np.asarray(res.results[b]["outT"]).T) for b in range(B)], 0)
    return out.astype(np.float32)
```

```python
import numpy as np
from contextlib import ExitStack
import concourse.bass as bass
import concourse.mybir as mybir
from concourse.bass_utils import run_bass_kernel_spmd

F32 = mybir.dt.float32
BF16 = mybir.dt.bfloat16
I32 = mybir.dt.int32
ALU = mybir.AluOpType
AF = mybir.ActivationFunctionType
AX = mybir.AxisListType

import os as _os
SAFE_SAME_ENGINE = _os.environ.get("UNSAFE_SAME", "0") != "1"


class V:
    __slots__ = ("ap", "buf")

    def __init__(self, ap, buf):
        self.ap = ap
        self.buf = buf

    def __getitem__(self, idx):
        return V(self.ap[idx], self.buf)

    def rearrange(self, *a, **k):
        return V(self.ap.rearrange(*a, **k), self.buf)

    def broadcast_to(self, shape):
        return V(self.ap.broadcast_to(shape), self.buf)

    def bitcast(self, dt):
        return V(self.ap.bitcast(dt), self.buf)

    @property
    def shape(self):
        return self.ap.shape


class Buf:
    __slots__ = ("name", "last_write", "readers", "psum")

    def __init__(self, name, psum=False):
        self.name = name
        self.psum = psum
        self.last_write = None
        self.readers = []


class Prog:
    NDMA = 8

    def __init__(self, nc, es):
        self.nc = nc
        self.eng = {"pe": nc.tensor, "act": nc.scalar, "dve": nc.vector, "pool": nc.gpsimd, "sp": nc.sync}
        self.sem = {}
        self.count = {}
        for e in self.eng:
            self.sem[e] = es.enter_context(nc.semaphore("c_" + e))
            self.count[e] = 0
        self.dsem = {}
        self.dcount = {}
        self.dnext = {}
        for q in ("sp", "act", "pool"):
            for i in range(self.NDMA):
                key = ("d", q, i)
                self.sem[key] = es.enter_context(nc.semaphore("d_%s%d" % (q, i)))
                self.count[key] = 0
            self.dnext[q] = 0
        self.known = {e: {} for e in self.eng}
        self.bufs = []
        self.ninst = 0

    def sb(self, es, name, shape, dt):
        t = es.enter_context(self.nc.sbuf_tensor(name, list(shape), dt))
        b = Buf(name)
        self.bufs.append(b)
        return V(t[:], b)

    def ps(self, es, name, shape, dt):
        t = es.enter_context(self.nc.psum_tensor(name, list(shape), dt))
        b = Buf(name, True)
        self.bufs.append(b)
        return V(t[:], b)

    def _need(self, e, needs):
        kn = self.known[e]
        for key, val in needs.items():
            if key == e and (e == "pe" or not SAFE_SAME_ENGINE) and not isinstance(key, tuple):
                continue
            if kn.get(key, 0) >= val:
                continue
            self.eng[e].wait_ge(self.sem[key], val)
            kn[key] = val

    def I(self, e, method, **kw):
        needs = {}
        reads, writes = [], []
        args = {}
        for k, v in kw.items():
            if isinstance(v, V):
                args[k] = v.ap
                if v.buf is not None:
                    (writes if k in ("out", "accum_out", "ap") else reads).append(v.buf)
            else:
                args[k] = v

        def add(tok):
            if tok is not None:
                if needs.get(tok[0], 0) < tok[1]:
                    needs[tok[0]] = tok[1]

        for b in reads:
            add(b.last_write)
            if b.psum:
                for r in b.readers:
                    if r[0] != e:
                        add(r)
        for b in writes:
            add(b.last_write)
            for r in b.readers:
                add(r)
        is_dma = method == "dma_start"
        if is_dma:
            q = e
            i = self.dnext[q]
            self.dnext[q] = (i + 1) % self.NDMA
            key = ("d", q, i)
            if self.count[key] > 0:
                add((key, self.count[key]))
        self._need(e, needs)
        inst = getattr(self.eng[e], method)(**args)
        self.ninst += 1
        if is_dma:
            self.count[key] += 16
            inst.then_inc(self.sem[key], 16)
            tok = (key, self.count[key])
        else:
            self.count[e] += 1
            inst.then_inc(self.sem[e], 1)
            tok = (e, self.count[e])
        for b in reads:
            b.readers.append(tok)
            if len(b.readers) > 64:
                m = {}
                for r in b.readers:
                    if m.get(r[0], 0) < r[1]:
                        m[r[0]] = r[1]
                b.readers = list(m.items())
        for b in writes:
            b.last_write = tok
            b.readers = []
        return inst

    def barrier(self):
        needs = {k: c for k, c in self.count.items() if c > 0}
        for e in self.eng:
            self._need(e, dict(needs))
        for b in self.bufs:
            b.last_write = None
            b.readers = []
        self.bufs = []

    def barrier_dram(self):
        needs = {k: c for k, c in self.count.items() if c > 0 and isinstance(k, tuple)}
        for e in ("sp", "act", "pool"):
            self._need(e, dict(needs))

    def dma(self, q, out, in_):
        return self.I(q, "dma_start", out=out, in_=in_)

    def mm(self, out, lhsT, rhs, start=True, stop=True):
        return self.I("pe", "matmul", out=out, lhsT=lhsT, rhs=rhs, start=start, stop=stop)

    def tr(self, out, in_, ident):
        return self.I("pe", "transpose", out=out, in_=in_, identity=ident)

    def act(self, out, in_, func, bias=None, scale=None, accum_out=None, e="act"):
        kw = dict(out=out, in_=in_, func=func)
        if bias is not None:
            kw["bias"] = bias
        if scale is not None:
            kw["scale"] = scale
        if accum_out is not None:
            kw["accum_out"] = accum_out
        return self.I(e, "activation", **kw)

    def tt(self, e, out, in0, in1, op):
        return self.I(e, "tensor_tensor", out=out, in0=in0, in1=in1, op=op)

    def ts(self, e, out, in0, s1, op0, s2=None, op1=None, accum_out=None):
        kw = dict(out=out, in0=in0, scalar1=s1, scalar2=s2, op0=op0)
        if op1 is not None:
            kw["op1"] = op1
        if accum_out is not None:
            kw["accum_out"] = accum_out
        return self.I(e, "tensor_scalar", **kw)

    def stt(self, e, out, in0, scalar, in1, op0, op1):
        return self.I(e, "scalar_tensor_tensor", out=out, in0=in0, scalar=scalar, in1=in1, op0=op0, op1=op1)

    def copy(self, e, out, in_):
        if e == "act":
            return self.I(e, "copy", out=out, in_=in_)
        return self.I(e, "tensor_copy", out=out, in_=in_)

    def memset(self, e, out, val):
        return self.I(e, "memset", ap=out, constant=val)

    def recip(self, out, in_):
        return self.I("dve", "reciprocal", out=out, in_=in_)

    def reduce(self, out, in_, op, axis=None):
        return self.I("dve", "tensor_reduce", out=out, in_=in_, axis=axis or AX.X, op=op)

    def scan(self, out, d0, d1, initial, op0, op1):
        return self.I("dve", "tensor_tensor_scan", out=out, data0=d0, data1=d1, initial=initial, op0=op0, op1=op1)


class Ring:
    def __init__(self, items):
        self.items = items
        self.i = 0

    def next(self):
        v = self.items[self.i % len(self.items)]
        self.i += 1
        return v


def dram(nc, name, shape, dt, kind="Internal"):
    return V(nc.dram_tensor(name, list(shape), dt, kind=kind).ap(), None)


def kchunks(K):
    return [(k0, min(128, K - k0)) for k0 in range(0, K, 128)]


def st_norm(P, xT, g, outT, D, S, eps, consts, tag, out_dt=BF16):
    nc = P.nc
    pp = min(D, 128)
    kc = D // pp
    TT = min(512, S)
    with ExitStack() as es:
        gs = P.sb(es, tag + "g", [pp, kc], F32)
        P.dma("sp", gs, g)
        xs = [P.sb(es, tag + "x%d" % i, [pp, kc, TT], F32) for i in range(2)]
        sq = P.sb(es, tag + "sq", [pp, kc, TT], BF16)
        os_ = [P.sb(es, tag + "o%d" % i, [pp, kc, TT], out_dt) for i in range(2)]
        rs = P.sb(es, tag + "rs", [pp, TT], F32)
        pss = [P.ps(es, tag + "ps%d" % i, [pp, TT], F32) for i in range(2)]
        xv = xT.rearrange("(c p) s -> p c s", p=pp)
        ov = outT.rearrange("(c p) s -> p c s", p=pp)
        ones = consts["ones_bf"]
        for it, t0 in enumerate(range(0, S, TT)):
            x = xs[it % 2]
            o = os_[it % 2]
            ps = pss[it % 2]
            P.dma("sp", x, xv[:, :, t0:t0 + TT])
            P.act(sq, x, AF.Square)
            for c in range(kc):
                P.mm(ps, ones[0:pp, 0:pp], sq[:, c, :], start=(c == 0), stop=(c == kc - 1))
            P.act(rs, ps, AF.Sqrt, bias=consts["eps_%g" % eps][0:pp, :], scale=1.0 / D)
            P.recip(rs, rs)
            for c in range(kc):
                P.stt("dve", o[:, c, :], x[:, c, :], gs[:, c:c + 1], rs, ALU.mult, ALU.mult)
            P.dma("sp", ov[:, :, t0:t0 + TT], o)
        P.barrier()


def st_mm(P, aT, K, S, jobs, consts, tag, TT=2048, CG=512, a_dt=BF16, w_dt=F32, wscr=None):
    kcs = kchunks(K)
    kc = len(kcs)
    pp = kcs[0][1]
    TT = min(TT, S)
    ntt = (S + TT - 1) // TT
    regular = (K % 128 == 0 or K < 128)
    pre = wscr is not None and w_dt == F32 and ntt >= 2 and regular
    groups = []
    for ji, (W, n, epi) in enumerate(jobs):
        for g0 in range(0, n, CG):
            groups.append((ji, g0, min(CG, n - g0)))
    gstride = pp * kc * CG
    if pre:
        with ExitStack() as esp:
            wfp = [P.sb(esp, tag + "pwf%d" % i, [pp, kc, CG], F32) for i in range(2)]
            wbp = [P.sb(esp, tag + "pwb%d" % i, [pp, kc, CG], BF16) for i in range(2)]
            for gi, (ji, g0, gsz) in enumerate(groups):
                W = jobs[ji][0]
                f, b = wfp[gi % 2], wbp[gi % 2]
                P.dma("act" if gi % 2 else "sp", f[:, :, 0:gsz], W.rearrange("(c p) n -> p c n", p=pp)[:, :, g0:g0 + gsz])
                P.copy("dve" if gi % 2 else "act", b[:, :, 0:gsz], f[:, :, 0:gsz])
                dst = wscr[gi * gstride:(gi + 1) * gstride].rearrange("(p c n) -> p c n", p=pp, c=kc)
                P.dma("pool", dst[:, :, 0:gsz], b[:, :, 0:gsz])
            P.barrier()
    with ExitStack() as es:
        na = 2 if (kc * TT * 2 <= 65536 and a_dt == BF16) else 1
        a_s = [P.sb(es, tag + "a%d" % i, [pp, kc, TT], BF16) for i in range(na)]
        af = P.sb(es, tag + "af", [pp, kc, TT], F32) if a_dt == F32 else None
        wf = [P.sb(es, tag + "wf%d" % i, [pp, kc, CG], F32) for i in range(2)] if (w_dt == F32 and not pre) else None
        nwb = 3 if pre else 2
        wb = [P.sb(es, tag + "wb%d" % i, [pp, kc, CG], BF16) for i in range(nwb)]
        pss = Ring([P.ps(es, tag + "ps%d" % i, [128, 512], F32) for i in range(4)])
        state = {"iw": 0}

        def load_f32(gi, ji, g0, gsz):
            iw = state["iw"]
            state["iw"] += 1
            W = jobs[ji][0]
            b = wb[iw % nwb]
            if w_dt != F32:
                if regular:
                    P.dma("act" if iw % 2 else "sp", b[:, :, 0:gsz], W.rearrange("(c p) n -> p c n", p=pp)[:, :, g0:g0 + gsz])
                else:
                    for c, (k0, ksz) in enumerate(kcs):
                        P.dma("sp", b[0:ksz, c, 0:gsz], W[k0:k0 + ksz, g0:g0 + gsz])
                return b
            f = wf[iw % 2]
            ce = "dve" if iw % 2 else "act"
            if regular:
                P.dma("act" if iw % 2 else "sp", f[:, :, 0:gsz], W.rearrange("(c p) n -> p c n", p=pp)[:, :, g0:g0 + gsz])
                P.copy(ce, b[:, :, 0:gsz], f[:, :, 0:gsz])
            else:
                for c, (k0, ksz) in enumerate(kcs):
                    P.dma("sp", f[0:ksz, c, 0:gsz], W[k0:k0 + ksz, g0:g0 + gsz])
                    P.copy(ce, b[0:ksz, c, 0:gsz], f[0:ksz, c, 0:gsz])
            return b

        for it, t0 in enumerate(range(0, S, TT)):
            a = a_s[it % len(a_s)]
            ald = af if a_dt == F32 else a
            if regular:
                P.dma("sp", ald, aT.rearrange("(c p) s -> p c s", p=pp)[:, :, t0:t0 + TT])
                if a_dt == F32:
                    P.copy("dve", a, af)
            else:
                for c, (k0, ksz) in enumerate(kcs):
                    P.dma("sp", ald[0:ksz, c, :], aT[k0:k0 + ksz, t0:t0 + TT])
                    if a_dt == F32:
                        P.copy("dve", a[0:ksz, c, :], af[0:ksz, c, :])
            for gi, (ji, g0, gsz) in enumerate(groups):
                epi = jobs[ji][2]
                if pre:
                    iw = state["iw"]
                    state["iw"] += 1
                    b = wb[iw % nwb]
                    src = wscr[gi * gstride:(gi + 1) * gstride].rearrange("(p c n) -> p c n", p=pp, c=kc)
                    P.dma("act" if iw % 2 else "sp", b[:, :, 0:gsz], src[:, :, 0:gsz])
                else:
                    b = load_f32(gi, ji, g0, gsz)
                for m0 in range(0, gsz, 128):
                    msz = min(128, gsz - m0)
                    for n0 in range(0, TT, 512):
                        nsz = min(512, TT - n0)
                        ps = pss.next()
                        for c, (k0, ksz) in enumerate(kcs):
                            P.mm(ps[0:msz, 0:nsz], b[0:ksz, c, m0:m0 + msz], a[0:ksz, c, n0:n0 + nsz],
                                 start=(c == 0), stop=(c == kc - 1))
                        epi(P, ps[0:msz, 0:nsz], g0 + m0, msz, t0 + n0, nsz)
        P.barrier()


STORE_Q = _os.environ.get("STORE_Q", "pool")


class EpiStore:
    def __init__(self, P, es, outT, dt, tag, func=None, nbuf=3):
        self.outT = outT
        self.ring = Ring([P.sb(es, tag + "e%d" % i, [128, 512], dt) for i in range(nbuf)])
        self.func = func
        self.i = 0

    def __call__(self, P, ps, c0, csz, t0, tsz):
        o = self.ring.next()[0:csz, 0:tsz]
        if self.i % 2 == 0:
            P.act(o, ps, self.func or AF.Copy)
        else:
            if self.func is None:
                P.copy("dve", o, ps)
            else:
                P.act(o, ps, self.func)
        self.i += 1
        P.dma(STORE_Q, self.outT[c0:c0 + csz, t0:t0 + tsz], o)


class EpiResid:
    def __init__(self, P, es, xT, tag, nbuf=3):
        self.xT = xT
        self.ring = Ring([P.sb(es, tag + "r%d" % i, [128, 512], F32) for i in range(nbuf)])

    def __call__(self, P, ps, c0, csz, t0, tsz):
        o = self.ring.next()[0:csz, 0:tsz]
        P.dma("act", o, self.xT[c0:c0 + csz, t0:t0 + tsz])
        P.tt("dve", o, ps, o, ALU.add)
        P.dma(STORE_Q, self.xT[c0:c0 + csz, t0:t0 + tsz], o)


def make_consts(P, es, cd):
    c = {}
    for name, (shape, dt) in CONST_SPECS.items():
        t = P.sb(es, "k_" + name, shape, dt)
        P.dma("sp", t, cd[name])
        c[name] = t
    return c


CONST_SPECS = {
    "ones_f32": ([128, 128], F32),
    "ident_f32": ([128, 128], F32),
    "eps_1e-05": ([128, 1], F32),
    "eps_0.00064": ([128, 1], F32),
    "ones_bf": ([128, 128], BF16),
    "ident_bf": ([128, 128], BF16),
    "halfpi": ([128, 1], F32),
    "blockones_f32": ([128, 128], F32),
    "mask_su": ([64, 1, 64], F32),
    "mask_iu": ([64, 1, 64], F32),
    "mask_sl": ([64, 1, 64], F32),
    "ident64": ([64, 1, 64], F32),
    "rotA32": ([32, 32], F32),
    "rotI128": ([128, 128], F32),
    "freqA": ([128, 1], F32),
    "freqI": ([128, 1], F32),
    "pow2row": ([128, 24], F32),
}


def host_consts():
    import ml_dtypes
    d = {}
    d["ones_f32"] = np.ones((128, 128), np.float32)
    d["ident_f32"] = np.eye(128, dtype=np.float32)
    d["eps_1e-05"] = np.full((128, 1), 1e-5, np.float32)
    d["eps_0.00064"] = np.full((128, 1), 64e-5, np.float32)
    d["ones_bf"] = np.ones((128, 128), ml_dtypes.bfloat16)
    d["ident_bf"] = np.eye(128).astype(ml_dtypes.bfloat16)
    d["halfpi"] = np.full((128, 1), np.pi / 2, np.float32)
    bo = np.zeros((128, 128), np.float32)
    bo[:64, :64] = 1.0
    bo[64:, 64:] = 1.0
    d["blockones_f32"] = bo
    iu = np.arange(64)
    d["mask_su"] = (iu[:, None] < iu[None, :]).astype(np.float32).reshape(64, 1, 64)
    d["mask_iu"] = (iu[:, None] <= iu[None, :]).astype(np.float32).reshape(64, 1, 64)
    d["mask_sl"] = (iu[:, None] > iu[None, :]).astype(np.float32).reshape(64, 1, 64)
    d["ident64"] = np.eye(64, dtype=np.float32).reshape(64, 1, 64)
    ra = np.zeros((32, 32), np.float32)
    for m_ in range(16):
        ra[m_ + 16, m_] = -1.0
        ra[m_, m_ + 16] = 1.0
    d["rotA32"] = ra
    ri = np.zeros((128, 128), np.float32)
    for o_ in (0, 64):
        for m_ in range(8):
            ri[o_ + m_ + 8, o_ + m_] = -1.0
            ri[o_ + m_, o_ + m_ + 8] = 1.0
    d["rotI128"] = ri
    theta = np.float32(500000.0)
    fa = (theta ** (-np.arange(0, 32, 2, dtype=np.float32) / np.float32(32))).astype(np.float32)
    fi = (theta ** (-np.arange(0, 16, 2, dtype=np.float32) / np.float32(16))).astype(np.float32)
    fA = np.zeros((128, 1), np.float32)
    fA[0:16, 0] = fa
    fA[16:32, 0] = fa
    fI = np.zeros((128, 1), np.float32)
    for o_ in (0, 64):
        fI[o_:o_ + 8, 0] = fi
        fI[o_ + 8:o_ + 16, 0] = fi
    d["freqA"], d["freqI"] = fA, fI
    d["pow2row"] = np.tile((0.5 ** np.arange(1, 25, dtype=np.float64)).astype(np.float32)[None, :], (128, 1))
    return d


class EpiRelu2:
    def __init__(self, P, es, outT, tag, nbuf=3):
        self.outT = outT
        self.sq = Ring([P.sb(es, tag + "q%d" % i, [128, 512], F32) for i in range(nbuf)])
        self.ring = Ring([P.sb(es, tag + "e%d" % i, [128, 512], BF16) for i in range(nbuf)])

    def __call__(self, P, ps, c0, csz, t0, tsz):
        sq = self.sq.next()[0:csz, 0:tsz]
        o = self.ring.next()[0:csz, 0:tsz]
        P.act(sq, ps, AF.Square)
        P.stt("dve", o, ps, 0.0, sq, ALU.is_gt, ALU.mult)
        P.dma(STORE_Q, self.outT[c0:c0 + csz, t0:t0 + tsz], o)


class EpiGlu:
    def __init__(self, P, es, zT, bcol, outT, tag, nbuf=3):
        self.zT, self.outT, self.bcol = zT, outT, bcol
        self.z = Ring([P.sb(es, tag + "z%d" % i, [128, 512], F32) for i in range(nbuf)])
        self.sg = Ring([P.sb(es, tag + "s%d" % i, [128, 512], F32) for i in range(nbuf)])
        self.ring = Ring([P.sb(es, tag + "e%d" % i, [128, 512], BF16) for i in range(nbuf)])

    def __call__(self, P, ps, c0, csz, t0, tsz):
        z = self.z.next()[0:csz, 0:tsz]
        sg = self.sg.next()[0:csz, 0:tsz]
        o = self.ring.next()[0:csz, 0:tsz]
        P.dma("act", z, self.zT[c0:c0 + csz, t0:t0 + tsz])
        P.act(sg, ps, AF.Sigmoid, bias=self.bcol[0:csz, c0 // 128:c0 // 128 + 1])
        P.tt("dve", o, z, sg, ALU.mult)
        P.dma(STORE_Q, self.outT[c0:c0 + csz, t0:t0 + tsz], o)


def st_xattn_core(P, qT, kT, vtok, oT, S, consts, tag):
    H, M = 4, 256
    TT = 512
    sc = 128.0 ** -0.5
    with ExitStack() as es:
        k = P.sb(es, tag + "k", [128, H, M], BF16)
        v = P.sb(es, tag + "v", [128, 2, 512], BF16)
        P.dma("sp", k, kT.rearrange("(h d) m -> d h m", d=128))
        P.dma("sp", v, vtok.rearrange("(c m) n -> m c n", m=128))
        qs = [P.sb(es, tag + "q%d" % i, [128, H, TT], BF16) for i in range(2)]
        os_ = [P.sb(es, tag + "o%d" % i, [128, H, TT], BF16) for i in range(2)]
        pT = Ring([P.sb(es, tag + "p%d" % i, [128, 2, TT], BF16) for i in range(2)])
        rd = Ring([P.sb(es, tag + "rd%d" % i, [128, TT], F32) for i in range(2)])
        ps_s = Ring([P.ps(es, tag + "pss%d" % i, [128, TT], F32) for i in range(4)])
        ps_d = Ring([P.ps(es, tag + "psd%d" % i, [128, TT], F32) for i in range(2)])
        ps_o = Ring([P.ps(es, tag + "pso%d" % i, [128, TT], F32) for i in range(2)])
        ones = consts["ones_bf"]
        qv = qT.rearrange("(h d) s -> d h s", d=128)
        ov = oT.rearrange("(h d) s -> d h s", d=128)
        for it, t0 in enumerate(range(0, S, TT)):
            q = qs[it % 2]
            o = os_[it % 2]
            P.dma("sp", q, qv[:, :, t0:t0 + TT])
            for h in range(H):
                p = pT.next()
                for mc in range(2):
                    ps = ps_s.next()
                    P.mm(ps, k[:, h, mc * 128:(mc + 1) * 128], q[:, h, :])
                    P.act(p[:, mc, :], ps, AF.Exp, scale=sc)
                pd = ps_d.next()
                po = ps_o.next()
                for mc in range(2):
                    P.mm(pd, ones, p[:, mc, :], start=(mc == 0), stop=(mc == 1))
                for mc in range(2):
                    P.mm(po, v[:, mc, h * 128:(h + 1) * 128], p[:, mc, :], start=(mc == 0), stop=(mc == 1))
                r = rd.next()
                P.recip(r, pd)
                P.tt("dve", o[:, h, :], po, r, ALU.mult)
            P.dma("sp", ov[:, :, t0:t0 + TT], o)
        P.barrier()


def layer_tail(P, xT, memT, w, l, sc, S, consts, tag):
    D = 2048
    t = tag + "T"
    st_norm(P, xT, w["norm_xattn"][l], sc["xnT"], D, S, 1e-5, consts, t + "n2")
    st_norm(P, memT, w["norm_mem"][l], sc["memnT"], D, 256, 1e-5, consts, t + "nm")
    with ExitStack() as es:
        st_mm(P, sc["xnT"], D, S, [(w["xattn_wq"][l], 512, EpiStore(P, es, sc["qT"], BF16, t + "eq"))], consts, t + "mq", wscr=sc["wscr"])
    with ExitStack() as es:
        st_mm(P, sc["memnT"], D, 256, [(w["xattn_wkv"][l][:, 0:512], 512, EpiStore(P, es, sc["kT"], BF16, t + "ek"))],
              consts, t + "mk")
    with ExitStack() as es:
        st_mm(P, w["xattn_wkv"][l][:, 512:1024], D, 512, [(sc["memnT"], 256, EpiStore(P, es, sc["vtok"], BF16, t + "ev"))],
              consts, t + "mv", a_dt=F32, w_dt=BF16)
    st_xattn_core(P, sc["qT"], sc["kT"], sc["vtok"], sc["oT"], S, consts, t + "xc")
    with ExitStack() as es:
        st_mm(P, sc["oT"], 512, S, [(w["xattn_wo"][l], D, EpiResid(P, es, xT, t + "ro"))], consts, t + "mo", wscr=sc["wscr"])
    st_norm(P, xT, w["norm_ffn"][l], sc["xnT"], D, S, 1e-5, consts, t + "n3")
    with ExitStack() as es:
        st_mm(P, sc["xnT"], D, S, [(w["ffn_up"][l], 8192, EpiRelu2(P, es, sc["hT"], t + "eu"))], consts, t + "mu", wscr=sc["wscr"])
    with ExitStack() as es:
        st_mm(P, sc["hT"], 8192, S, [(w["ffn_down"][l], D, EpiResid(P, es, xT, t + "rd"))], consts, t + "md",
              TT=1024, CG=128, wscr=sc["wscr"])


TWO_PI = 2.0 * np.pi
CW1 = 6.28125
CW2 = TWO_PI - 6.28125


def sincos(P, ang, kq, r, sinv, cosv, consts, clamp_eng="dve"):
    npart = ang.shape[0]
    P.ts("dve", kq, ang, 1.0 / TWO_PI, ALU.mult)
    P.stt("dve", r, kq, -CW1, ang, ALU.mult, ALU.add)
    P.stt("dve", r, kq, -CW2, r, ALU.mult, ALU.add)
    P.ts(clamp_eng, r, r, float(np.pi), ALU.min, -float(np.pi), ALU.max)
    P.act(sinv, r, AF.Sin)
    P.act(r, r, AF.Abs)
    hp = consts["halfpi"][0:npart, :]
    P.act(cosv, r, AF.Sin, bias=hp, scale=-1.0)


def st_s5_core(P, uT, zT, w, i, S, consts, tag):
    TB = min(1024, S)
    NB = TB // 512
    with ExitStack() as es:
        sb = lambda n, sh, dt=F32: P.sb(es, tag + n, sh, dt)
        lamre, lamim, logdt = sb("lamre", [128, 64]), sb("lamim", [128, 64]), sb("logdt", [128, 64])
        P.dma("sp", lamre, w["s5_lamre_pc"][i])
        P.dma("sp", lamim, w["s5_lamim_pc"][i])
        P.dma("sp", logdt, w["s5_logdt_pc"][i])
        dcol, = [sb("dcol", [128, 16])]
        P.dma("sp", dcol, w["s5_d_pc"][i])
        lre, dt, rho, th, th2 = sb("lre", [128, 64]), sb("dt", [128, 64]), sb("rho", [128, 64]), sb("th", [128, 64]), sb("th2", [128, 64])
        P.ts("dve", lre, lamre, -1e-4, ALU.min)
        P.act(dt, logdt, AF.Exp)
        P.tt("dve", th2, lre, dt, ALU.mult)
        P.act(rho, th2, AF.Exp)
        P.tt("dve", th, lamim, dt, ALU.mult)
        kq0, r0, sth, cth = sb("kq0", [128, 64], I32), sb("r0", [128, 64]), sb("sth", [128, 64]), sb("cth", [128, 64])
        sincos(P, th, kq0, r0, sth, cth, consts)
        cr, ci, den, t1, t2 = sb("cr", [128, 64]), sb("ci", [128, 64]), sb("den", [128, 64]), sb("t1", [128, 64]), sb("t2", [128, 64])
        qr, qi, nqi = sb("qr", [128, 64]), sb("qi", [128, 64]), sb("nqi", [128, 64])
        P.tt("dve", cr, rho, cth, ALU.mult)
        P.ts("dve", cr, cr, -1.0, ALU.add)
        P.tt("dve", ci, rho, sth, ALU.mult)
        P.tt("dve", den, lre, lre, ALU.mult)
        P.tt("dve", t1, lamim, lamim, ALU.mult)
        P.tt("dve", den, den, t1, ALU.add)
        P.recip(den, den)
        P.tt("dve", t1, cr, lre, ALU.mult)
        P.tt("dve", t2, ci, lamim, ALU.mult)
        P.tt("dve", t1, t1, t2, ALU.add)
        P.tt("dve", qr, t1, den, ALU.mult)
        P.tt("dve", t1, ci, lre, ALU.mult)
        P.tt("dve", t2, cr, lamim, ALU.mult)
        P.tt("dve", t1, t1, t2, ALU.subtract)
        P.tt("dve", qi, t1, den, ALU.mult)
        P.ts("dve", nqi, qi, -1.0, ALU.mult)
        tfull = sb("tfull", [128, S])
        with ExitStack() as es2:
            tfi = P.sb(es2, tag + "tfi", [128, S], I32)
            P.I("pool", "iota", out=tfi, pattern=[[1, S]], base=0, channel_multiplier=0)
            P.copy("dve", tfull, tfi)
            P.barrier()
        f4 = lambda n: sb(n, [128, TB])
        bpad = [[sb("bp%d%d" % (a_, b_), [128, 128]) for b_ in range(2)] for a_ in range(2)]
        cpad = [[sb("cp%d%d" % (a_, b_), [128, 128]) for b_ in range(2)] for a_ in range(2)]
        bbf = [[[sb("bb%d_%d_%d" % (q, g, b_), [128, 128], BF16) for b_ in range(2)] for g in range(4)] for q in range(2)]
        cbf = [[[sb("cb%d_%d_%d" % (q, g, b_), [128, 128], BF16) for b_ in range(2)] for g in range(4)] for q in range(2)]
        state = [[[sb("st%d_%d_%d" % (q, g, b_), [128, 1]) for b_ in range(2)] for g in range(4)] for q in range(2)]
        ctmp = sb("ctmp", [128, 128])
        ubf = [sb("ubf%d" % k, [128, TB], BF16) for k in range(2)]
        uf = [sb("uf%d" % k, [128, TB]) for k in range(2)]
        ang2, kq2 = [f4("angq%d" % q) for q in range(2)], [sb("kqq%d" % q, [128, TB], I32) for q in range(2)]
        tabs3, tabc3 = [f4("tabs%d" % q) for q in range(3)], [f4("tabc%d" % q) for q in range(3)]
        raw2 = [[f4("raw%d_%d" % (q, b_)) for b_ in range(2)] for q in range(2)]
        mW, mX = [f4("mW%d" % k) for k in range(4)], [f4("mX%d" % k) for k in range(4)]
        wr = [f4("w%d" % b_) for b_ in range(2)]
        xh2 = [[f4("xh%d_%d" % (q, b_)) for b_ in range(2)] for q in range(2)]
        X = [[sb("X%d_%d" % (k, b_), [128, TB], BF16) for b_ in range(2)] for k in range(2)]
        yv, x2, zz = f4("yv"), f4("x2"), [f4("zz%d" % k) for k in range(2)]
        ps_raw = [[P.ps(es, tag + "pr%d_%d" % (b_, n), [128, 512], F32) for n in range(NB)] for b_ in range(2)]
        ps_y = [P.ps(es, tag + "py%d" % n, [128, 512], F32) for n in range(NB)]
        NTB = S // TB
        iters = [(ct, tbi, g) for ct in range(16) for tbi in range(NTB) for g in range(4)]

        def prep_ct(ct):
            q = ct % 2
            for g in range(4):
                gp = ct * 4 + g
                bp, cp = bpad[g % 2], cpad[g % 2]
                P.dma("sp", bp[0], w["s5_bre_pad"][i, gp])
                P.dma("act", bp[1], w["s5_bim_pad"][i, gp])
                P.dma("sp", cp[0], w["s5_cre_pad"][i, gp])
                P.dma("act", cp[1], w["s5_cim_pad"][i, gp])
                P.copy("pool", bbf[q][g][0], bp[0])
                P.copy("pool", bbf[q][g][1], bp[1])
                P.ts("dve", ctmp, cp[1], qi[:, gp:gp + 1], ALU.mult)
                P.stt("dve", cbf[q][g][0], cp[0], qr[:, gp:gp + 1], ctmp, ALU.mult, ALU.subtract)
                P.ts("dve", ctmp, cp[1], qr[:, gp:gp + 1], ALU.mult)
                P.stt("dve", cbf[q][g][1], cp[0], nqi[:, gp:gp + 1], ctmp, ALU.mult, ALU.subtract)
                P.memset("dve", state[q][g][0], 0.0)
                P.memset("dve", state[q][g][1], 0.0)

        def stA(k):
            ct, tbi, g = iters[k]
            t0 = tbi * TB
            gp = ct * 4 + g
            ui = (ct * NTB + tbi) % 2
            if tbi == 0 and g == 0:
                prep_ct(ct)
            if g == 0:
                P.dma("sp", uf[ui], uT[ct * 128:(ct + 1) * 128, t0:t0 + TB])
                P.copy("act", ubf[ui], uf[ui])
            u_b = ubf[ui]
            for b_ in range(2):
                for n in range(NB):
                    P.mm(ps_raw[b_][n], bbf[ct % 2][g][b_], u_b[:, n * 512:(n + 1) * 512])
            ang, kq = ang2[k % 2], kq2[k % 2]
            P.act(ang, tfull[:, t0:t0 + TB], AF.Copy, scale=th[:, gp:gp + 1])
            sincos(P, ang, kq, ang, tabs3[k % 3], tabc3[k % 3], consts)
            raw = raw2[k % 2]
            for b_ in range(2):
                for n in range(NB):
                    P.copy("act", raw[b_][:, n * 512:(n + 1) * 512], ps_raw[b_][n])

        def stB(k):
            ct, tbi, g = iters[k]
            gp = ct * 4 + g
            tabs, tabc, raw, xh = tabs3[k % 3], tabc3[k % 3], raw2[k % 2], xh2[k % 2]
            P.tt("pool", mW[0], tabc, raw[0], ALU.mult)
            P.tt("pool", mW[1], tabs, raw[1], ALU.mult)
            P.tt("pool", mW[2], tabc, raw[1], ALU.mult)
            P.tt("dve", mW[3], tabs, raw[0], ALU.mult)
            P.tt("dve", wr[0], mW[0], mW[1], ALU.add)
            P.tt("dve", wr[1], mW[2], mW[3], ALU.subtract)
            rb = rho[:, gp:gp + 1].broadcast_to([128, TB])
            st_ = state[ct % 2][g]
            for b_ in range(2):
                P.scan(xh[b_], rb, wr[b_], st_[b_], ALU.mult, ALU.add)
                P.copy("dve", st_[b_], xh[b_][:, TB - 1:TB])

        def stC(k):
            ct, tbi, g = iters[k]
            t0 = tbi * TB
            ui = (ct * NTB + tbi) % 2
            tabs, tabc, xh = tabs3[k % 3], tabc3[k % 3], xh2[k % 2]
            P.tt("pool", mX[0], tabc, xh[0], ALU.mult)
            P.tt("pool", mX[1], tabs, xh[1], ALU.mult)
            P.tt("pool", mX[2], tabs, xh[0], ALU.mult)
            P.tt("dve", mX[3], tabc, xh[1], ALU.mult)
            Xg = X[k % 2]
            P.tt("dve", Xg[0], mX[0], mX[1], ALU.subtract)
            P.tt("dve", Xg[1], mX[2], mX[3], ALU.add)
            cb = cbf[ct % 2][g]
            for n in range(NB):
                P.mm(ps_y[n], cb[0], Xg[0][:, n * 512:(n + 1) * 512], start=(g == 0), stop=False)
                P.mm(ps_y[n], cb[1], Xg[1][:, n * 512:(n + 1) * 512], start=False, stop=(g == 3))
            if g == 3:
                u_f = uf[ui]
                for n in range(NB):
                    sl = slice(n * 512, (n + 1) * 512)
                    P.stt("dve", yv[:, sl], u_f[:, sl], dcol[:, ct:ct + 1], ps_y[n], ALU.mult, ALU.add)
                z = zz[(ct * NTB + tbi) % 2]
                P.act(x2, yv, AF.Square)
                P.ts("dve", x2, x2, 0.044715, ALU.mult, 1.0, ALU.add)
                P.tt("dve", x2, x2, yv, ALU.mult)
                P.act(x2, x2, AF.Sigmoid, scale=2.0 * float(np.sqrt(2.0 / np.pi)))
                P.tt("dve", z, yv, x2, ALU.mult)
                P.dma("sp", zT[ct * 128:(ct + 1) * 128, t0:t0 + TB], z)

        NI = len(iters)
        stA(0)
        if NI > 1:
            stA(1)
        stB(0)
        for k in range(NI):
            if k + 2 < NI:
                stA(k + 2)
            if k + 1 < NI:
                stB(k + 1)
            stC(k)
        P.barrier()


def odd_layer(P, xT, memT, w, l, sc, S, consts, tag):
    i = l // 2
    D = 2048
    st_norm(P, xT, w["norm_mix"][l], sc["xnT"], D, S, 1e-5, consts, tag + "n1")
    with ExitStack() as es:
        st_mm(P, sc["xnT"], D, S, [(w["odd_w_in"][i], D, EpiStore(P, es, sc["uT"], F32, tag + "eu1"))], consts, tag + "mi", wscr=sc["wscr"])
    st_s5_core(P, sc["uT"], sc["zT"], w, i, S, consts, tag + "s5")
    with ExitStack() as es:
        bcol = P.sb(es, tag + "bglu", [128, 16], F32)
        P.dma("sp", bcol, w["s5_bglu_pc"][i])
        st_mm(P, sc["zT"], D, S, [(w["s5_w_glu"][i], D, EpiGlu(P, es, sc["zT"], bcol, sc["xnT"], tag + "eg"))], consts,
              tag + "mg", a_dt=F32, TT=512, wscr=sc["wscr"])
    with ExitStack() as es:
        st_mm(P, sc["xnT"], D, S, [(w["odd_w_out"][i], D, EpiResid(P, es, xT, tag + "ro1"))], consts, tag + "mo1", wscr=sc["wscr"])
    layer_tail(P, xT, memT, w, l, sc, S, consts, tag)


def host_layout_s5(inp):
    o = {}
    n = inp["s5_lam_re"].shape[0]

    def pc(a):
        return np.ascontiguousarray(a.reshape(n, 64, 2, 64).transpose(0, 2, 3, 1).reshape(n, 128, 64))

    o["s5_lamre_pc"] = pc(inp["s5_lam_re"])
    o["s5_lamim_pc"] = pc(inp["s5_lam_im"])
    o["s5_logdt_pc"] = pc(np.repeat(inp["s5_log_dt"][:, :, None], 64, axis=2))
    bpad = np.zeros((2, n, 64, 128, 128), np.float32)
    cpad = np.zeros((2, n, 64, 128, 128), np.float32)
    for k, (bn, cn) in enumerate((("s5_b_re", "s5_c_re"), ("s5_b_im", "s5_c_im"))):
        b = inp[bn].reshape(n, 64, 2, 64, 16)
        c = inp[cn].reshape(n, 64, 2, 16, 64)
        for gp in range(64):
            gq = gp % 4
            for gl in range(2):
                r0 = gq * 32 + gl * 16
                bpad[k, :, gp, r0:r0 + 16, gl * 64:(gl + 1) * 64] = b[:, gp, gl].transpose(0, 2, 1)
                cpad[k, :, gp, gl * 64:(gl + 1) * 64, r0:r0 + 16] = c[:, gp, gl].transpose(0, 2, 1)
    o["s5_bre_pad"], o["s5_bim_pad"] = bpad[0], bpad[1]
    o["s5_cre_pad"], o["s5_cim_pad"] = cpad[0], cpad[1]
    o["s5_d_pc"] = colvec(inp["s5_d"])
    o["s5_bglu_pc"] = colvec(inp["s5_b_glu"])
    return o


def colvec(a, pp=128):
    sh = a.shape
    return np.ascontiguousarray(np.swapaxes(a.reshape(sh[:-1] + (sh[-1] // pp, pp)), -1, -2))


KAPPA = float(np.exp(-0.5))
RW_COLS = ["rwkv_w0", "rwkv_a0", "rwkv_k_k", "rwkv_k_a", "rwkv_r_k", "rwkv_ln_w", "rwkv_ln_b"]


def st_rwkv_prep(P, hBT, w, i, sc, S, consts, tag):
    TB = 256
    NJ = 8
    with ExitStack() as es:
        sb = lambda n, sh, dt=F32: P.sb(es, tag + n, sh, dt)
        cols = sb("cols", [128, 7, NJ, 1])
        P.dma("sp", cols, w["rwkv_cols"][i])
        w0c, a0c, kkc, kac, rkc = [cols[:, k] for k in range(5)]
        omka = sb("omka", [128, NJ, 1])
        P.ts("dve", omka, kac, -1.0, ALU.mult, 1.0, ALU.add)
        mu3 = sb("mu3", [128, 3, NJ, 1])
        P.dma("sp", mu3, w["rwkv_mu_rkv"][i])
        muw, mua, mug1, mug2 = sb("muw", [64, 1]), sb("mua", [64, 1]), sb("mug1", [128, 1]), sb("mug2", [32, 1])
        P.dma("sp", muw, w["rwkv_mu_wl"][i])
        P.dma("sp", mua, w["rwkv_mu_al"][i])
        P.dma("sp", mug1, w["rwkv_mu_g1"][i])
        P.dma("sp", mug2, w["rwkv_mu_g2"][i])
        w2f, a2f, g2f1, g2f2 = sb("w2f", [64, 1024]), sb("a2f", [64, 1024]), sb("g2f1", [128, 1024]), sb("g2f2", [32, 1024])
        w2b, a2b, g2b1, g2b2 = sb("w2b", [64, 1024], BF16), sb("a2b", [64, 1024], BF16), sb("g2b1", [128, 1024], BF16), sb("g2b2", [32, 1024], BF16)
        P.dma("sp", w2f, w["rwkv_w2"][i])
        P.dma("sp", a2f, w["rwkv_a2"][i])
        P.dma("sp", g2f1, w["rwkv_g2"][i][0:128, :])
        P.dma("sp", g2f2, w["rwkv_g2"][i][128:160, :])
        for f, b in ((w2f, w2b), (a2f, a2b), (g2f1, g2b1), (g2f2, g2b2)):
            P.copy("act", b, f)
        cmask = sb("cmask", [128, NJ, TB])
        P.memset("dve", cmask, 1.0)
        P.memset("dve", cmask.rearrange("p j (c t) -> p j c t", t=64)[:, :, :, 0:1], 0.0)
        bones = consts["blockones_f32"]
        A3, B3 = sb("A3", [128, NJ, TB]), sb("B3", [128, NJ, TB])
        hmr, hmk, hmv = sb("hmr", [128, NJ, TB]), sb("hmk", [128, NJ, TB]), sb("hmv", [128, NJ, TB])
        As, Bs = sb("As", [128, TB]), sb("Bs", [128, TB])
        twl, alb, sg1, sg2 = sb("twl", [64, TB], BF16), sb("alb", [64, TB], BF16), sb("sg1", [128, TB], BF16), sb("sg2", [32, TB], BF16)
        sig, av, gv = sb("sig", [128, NJ, TB]), sb("av", [128, NJ, TB]), sb("gv", [128, NJ, TB])
        cs, e1, e2, e3 = sb("cs", [128, NJ, TB]), sb("e1", [128, NJ, TB]), sb("e2", [128, NJ, TB]), sb("e3", [128, NJ, TB])
        kraw, kk, kp, tmp = sb("kraw", [128, NJ, TB]), sb("kk", [128, NJ, TB]), sb("kp", [128, NJ, TB]), sb("tmp", [128, NJ, TB])
        oA, oB, oK, oR, oV = [sb("o" + n, [128, NJ, TB], BF16) for n in "ABKRV"]
        pcs = sb("pcs", [128, NJ, TB // 64])
        ps = Ring([P.ps(es, tag + "ps%d" % k, [128, TB], F32) for k in range(6)])
        hv = lambda r0: hBT[r0:r0 + 1024, :].rearrange("(j p) s -> p j s", p=128)
        out3 = lambda T_: T_.rearrange("(j p) s -> p j s", p=128)

        def shift3(view, dst, mu_b, t0):
            P.dma("sp", A3, view[:, :, t0:t0 + TB])
            if t0 == 0:
                P.memset("dve", B3[:, :, 0:1], 0.0)
                P.dma("act", B3[:, :, 1:TB], view[:, :, 0:TB - 1])
            else:
                P.dma("act", B3, view[:, :, t0 - 1:t0 + TB - 1])
            P.tt("dve", B3, B3, A3, ALU.subtract)
            P.tt("pool", B3, B3, mu_b.broadcast_to([128, NJ, TB]), ALU.mult)
            P.tt("dve", dst, B3, A3, ALU.add)

        def shift2(r0, nr, mu_c, t0, func, dst):
            a, b = As[0:nr, :], Bs[0:nr, :]
            P.dma("sp", a, hBT[r0:r0 + nr, t0:t0 + TB])
            if t0 == 0:
                P.memset("dve", b[:, 0:1], 0.0)
                P.dma("act", b[:, 1:TB], hBT[r0:r0 + nr, 0:TB - 1])
            else:
                P.dma("act", b, hBT[r0:r0 + nr, t0 - 1:t0 + TB - 1])
            P.tt("dve", b, b, a, ALU.subtract)
            P.stt("dve", b, b, mu_c, a, ALU.mult, ALU.add)
            P.act(dst, b, func)

        for t0 in range(0, S, TB):
            shift3(hv(0), hmr, mu3[:, 0], t0)
            shift3(hv(1024), hmk, mu3[:, 1], t0)
            shift3(hv(2048), hmv, mu3[:, 2], t0)
            shift2(3072, 64, muw, t0, AF.Tanh, twl)
            shift2(3136, 64, mua, t0, AF.Copy, alb)
            shift2(3200, 128, mug1, t0, AF.Sigmoid, sg1)
            shift2(3328, 32, mug2, t0, AF.Sigmoid, sg2)
            for j in range(NJ):
                cj = slice(j * 128, (j + 1) * 128)
                p1 = ps.next()
                P.mm(p1, w2b[:, cj], twl)
                P.act(sig[:, j, :], p1, AF.Sigmoid, bias=w0c[:, j, :])
                p2 = ps.next()
                P.mm(p2, a2b[:, cj], alb)
                P.act(av[:, j, :], p2, AF.Sigmoid, bias=a0c[:, j, :])
                p3 = ps.next()
                P.mm(p3, g2b1[:, cj], sg1, start=True, stop=False)
                P.mm(p3, g2b2[:, cj], sg2, start=False, stop=True)
                P.copy("dve", gv[:, j, :], p3)
            P.dma("sp", out3(sc["gT"])[:, :, t0:t0 + TB], gv)
            P.scan(cs.rearrange("p j t -> p (j t)"), cmask.rearrange("p j t -> p (j t)"), sig.rearrange("p j t -> p (j t)"),
                   0.0, ALU.mult, ALU.add)
            P.tt("dve", tmp, cs, sig, ALU.subtract)
            P.act(e1, tmp, AF.Exp, scale=-KAPPA)
            P.act(e2, cs, AF.Exp, scale=KAPPA)
            P.act(e3, cs, AF.Exp, scale=-KAPPA)
            P.copy("dve", pcs, e3.rearrange("p j (c t) -> p j c t", t=64)[:, :, :, 63])
            P.dma("sp", out3(sc["PCT"])[:, :, t0 // 64:(t0 + TB) // 64], pcs)
            P.tt("pool", kraw, hmk, kkc.broadcast_to([128, NJ, TB]), ALU.mult)
            P.act(tmp, kraw, AF.Square)
            for j in range(NJ):
                p1 = ps.next()
                P.mm(p1, bones, tmp[:, j, :])
                P.ts("dve", kk[:, j, :], p1, 1e-24, ALU.max)
            P.act(kk, kk, AF.Sqrt)
            P.recip(kk, kk)
            P.tt("dve", kk, kk, kraw, ALU.mult)
            P.tt("pool", tmp, av, kac.broadcast_to([128, NJ, TB]), ALU.mult)
            P.tt("pool", tmp, tmp, omka.broadcast_to([128, NJ, TB]), ALU.add)
            P.tt("dve", kp, hmk, tmp, ALU.mult)
            P.stt("dve", oA, kk, -1.0, e1, ALU.mult, ALU.mult)
            P.tt("pool", tmp, kk, av, ALU.mult)
            P.tt("dve", oB, tmp, e2, ALU.mult)
            P.tt("dve", oK, kp, e2, ALU.mult)
            P.tt("dve", oR, hmr, e3, ALU.mult)
            P.copy("act", oV, hmv)
            for nm, o in (("AhT", oA), ("BhT", oB), ("KhT", oK), ("RhT", oR), ("vT", oV)):
                P.dma("sp", out3(sc[nm])[:, :, t0:t0 + TB], o)
            P.tt("pool", tmp, hmr, kp, ALU.mult)
            P.tt("pool", tmp, tmp, rkc.broadcast_to([128, NJ, TB]), ALU.mult)
            for j in range(NJ):
                p1 = ps.next()
                P.mm(p1, bones, tmp[:, j, :])
                P.tt("dve", e1[:, j, :], p1, hmv[:, j, :], ALU.mult)
            P.dma("sp", out3(sc["bonusT"])[:, :, t0:t0 + TB], e1)
        P.barrier()


def st_rwkv_A(P, sc, S, consts, tag):
    CB = 4
    NCH = S // 64
    with ExitStack() as es:
        sb = lambda n, sh, dt=F32: P.sb(es, tag + n, sh, dt)
        names = ["AhT", "BhT", "KhT", "RhT", "vT"]
        blk = [[sb("bl%d_%d" % (k, q), [64, 16, CB * 64], BF16) for q in range(5)] for k in range(2)]
        msu = consts["mask_su"].broadcast_to([64, 16, 64])
        miu = consts["mask_iu"].broadcast_to([64, 16, 64])
        msl = consts["mask_sl"].broadcast_to([64, 16, 64])
        idb = consts["ident64"].broadcast_to([64, 16, 64])
        identb = consts["ident_bf"]
        N32s = [sb("N32_%d" % q, [64, 16, 64]) for q in range(2)]
        T32s = [sb("T32_%d" % q, [64, 16, 64]) for q in range(2)]
        Nbs = [[sb("Nb%d_%d" % (q, k), [64, 16, 64], BF16) for k in range(2)] for q in range(2)]
        NTbs = [[sb("NTb%d_%d" % (q, k), [64, 16, 64], BF16) for k in range(2)] for q in range(2)]
        Tbs = [sb("Tb%d" % q, [64, 16, 64], BF16) for q in range(2)]
        mats = [sb("mats%d" % k, [64, 4, 16, 64], BF16) for k in range(2)]
        tok = [sb("tok%d" % k, [64, 3, 16, 64], BF16) for k in range(2)]
        pr = Ring([P.ps(es, tag + "pr%d" % k, [64, 16, 64], F32) for k in range(3)])
        pt = Ring([P.ps(es, tag + "pt%d" % k, [64, 16, 64], BF16) for k in range(2)])
        view = lambda nm: sc[nm].rearrange("(h c) s -> c h s", c=64)

        def mmh(out, L, R):
            for h in range(16):
                P.mm(out[:, h, :], L[:, h, :], R[:, h, :])

        def chunk_gen(c, bl):
            q = c % 2
            N32, T32, Nb, NTb, Tb = N32s[q], T32s[q], Nbs[q], NTbs[q], Tbs[q]
            cc = slice((c % CB) * 64, (c % CB + 1) * 64)
            Ah, Bh, Kh, Rh, Vh = [bl[k][:, :, cc] for k in range(5)]
            mt = mats[q]
            tk = tok[q]
            p = pr.next()
            mmh(p, Ah, Bh)
            P.tt("dve", NTb[0], p, msl, ALU.mult)
            yield
            p = pr.next()
            mmh(p, Bh, Ah)
            P.tt("dve", N32, p, msu, ALU.mult)
            P.copy("act", Nb[0], N32)
            P.tt("pool", T32, N32, idb, ALU.add)
            P.copy("act", Tb, T32)
            yield
            p = pr.next()
            mmh(p, Kh, Ah)
            P.tt("dve", mt[:, 1], p, msu, ALU.mult)
            yield
            p = pr.next()
            mmh(p, Bh, Rh)
            P.tt("dve", mt[:, 2], p, miu, ALU.mult)
            yield
            p = pr.next()
            mmh(p, Kh, Rh)
            P.tt("dve", mt[:, 3], p, miu, ALU.mult)
            yield
            cur = 0
            srcs = (Bh, Kh, Vh)
            for j in range(1, 6):
                nxt = 1 - cur
                p = pr.next()
                mmh(p, Nb[cur], NTb[cur])
                P.copy("dve", NTb[nxt], p)
                yield
                if j < 5:
                    p = pr.next()
                    mmh(p, NTb[cur], Nb[cur])
                    P.copy("act", Nb[nxt], p)
                    yield
                if j <= 3:
                    tp = pt.next()
                    for h in range(16):
                        P.tr(tp[:, h, :], srcs[j - 1][:, h, :], identb[0:64, 0:64])
                    P.copy("act", tk[:, j - 1], tp)
                    yield
                p = pr.next()
                mmh(p, NTb[nxt], Tb)
                P.tt("dve", T32, T32, p, ALU.add)
                if j < 5:
                    P.copy("act", Tb, T32)
                yield
                cur = nxt
            P.copy("act", mt[:, 0], T32)
            P.dma("sp", sc["mats"][c], mt)
            P.dma("act", sc["tok3"][c], tk)
            yield

        for c0 in range(0, NCH, 2):
            if c0 % CB == 0:
                bl = blk[(c0 // CB) % 2]
                for k, nm in enumerate(names):
                    P.dma("sp" if k % 2 == 0 else "act", bl[k], view(nm)[:, :, c0 * 64:(c0 + CB) * 64])
            gens = [chunk_gen(c0, bl)]
            if c0 + 1 < NCH:
                gens.append(chunk_gen(c0 + 1, bl))
            alive = True
            while alive:
                alive = False
                for g in gens:
                    try:
                        next(g)
                        alive = True
                    except StopIteration:
                        pass
        P.barrier()


def st_rwkv_B(P, sc, S, consts, tag):
    CB = 4
    NCH = S // 64
    with ExitStack() as es:
        sb = lambda n, sh, dt=F32: P.sb(es, tag + n, sh, dt)
        blk = [[sb("bl%d_%d" % (k, q), [64, 16, CB * 64], BF16) for q in range(2)] for k in range(2)]
        pc = sb("pc", [64, 16, NCH])
        P.dma("sp", pc, sc["PCT"].rearrange("(h c) n -> c h n", c=64))
        mats = [sb("mats%d" % k, [64, 4, 16, 64], BF16) for k in range(3)]
        tok = [sb("tok%d" % k, [64, 3, 16, 64], BF16) for k in range(3)]
        S32, Sb = sb("S32", [64, 16, 64]), sb("Sb", [64, 16, 64], BF16)
        tmp = sb("tmp", [64, 16, 64])
        WTb = sb("WTb", [64, 16, 64], BF16)
        UTb = sb("UTb", [64, 16, 64], BF16)
        O = [sb("O%d" % k, [64, 16, 64]) for k in range(2)]
        P.memset("dve", S32, 0.0)
        P.memset("dve", Sb, 0.0)
        pW, pU, pO, pS = [P.ps(es, tag + n, [64, 16, 64], F32) for n in ("pW", "pU", "pO", "pS")]
        view = lambda nm: sc[nm].rearrange("(h c) s -> c h s", c=64)
        otok = sc["otok"].rearrange("(n t) (h v) -> n t h v", t=64, v=64)
        for c in range(NCH):
            if c % CB == 0:
                bl = blk[(c // CB) % 2]
                P.dma("sp", bl[0], view("AhT")[:, :, c * 64:(c + CB) * 64])
                P.dma("sp", bl[1], view("RhT")[:, :, c * 64:(c + CB) * 64])
            cc = slice((c % CB) * 64, (c % CB + 1) * 64)
            Ah, Rh = bl[0][:, :, cc], bl[1][:, :, cc]
            mt, tk = mats[c % 3], tok[c % 3]
            P.dma("sp", mt, sc["mats"][c])
            P.dma("act", tk, sc["tok3"][c])
            Tm, Nka, Mbr, Mkr = [mt[:, q] for q in range(4)]
            bt, kt, vt = [tk[:, q] for q in range(3)]
            for h in range(16):
                P.mm(pW[:, h, :], Ah[:, h, :], Sb[:, h, :], start=True, stop=False)
                P.mm(pW[:, h, :], Nka[:, h, :], vt[:, h, :], start=False, stop=True)
            P.copy("act", WTb, pW)
            for h in range(16):
                P.mm(pU[:, h, :], Tm[:, h, :], WTb[:, h, :])
            P.copy("act", UTb, pU)
            for h in range(16):
                P.mm(pO[:, h, :], Rh[:, h, :], Sb[:, h, :], start=True, stop=False)
                P.mm(pO[:, h, :], Mbr[:, h, :], UTb[:, h, :], start=False, stop=False)
                P.mm(pO[:, h, :], Mkr[:, h, :], vt[:, h, :], start=False, stop=True)
            o = O[c % 2]
            P.copy("act", o, pO)
            P.dma("sp", otok[c], o)
            for h in range(16):
                P.mm(pS[:, h, :], bt[:, h, :], UTb[:, h, :], start=True, stop=False)
                P.mm(pS[:, h, :], kt[:, h, :], vt[:, h, :], start=False, stop=True)
            P.tt("dve", tmp, pS, S32, ALU.add)
            P.tt("dve", S32, tmp, pc[:, :, c:c + 1].broadcast_to([64, 16, 64]), ALU.mult)
            P.copy("act", Sb, S32)
        P.barrier()


def st_rwkv_post(P, w, i, sc, S, consts, tag):
    TB = min(512, S)
    with ExitStack() as es:
        sb = lambda n, sh, dt=F32: P.sb(es, tag + n, sh, dt)
        cols = sb("cols", [128, 7, 8, 1])
        P.dma("sp", cols, w["rwkv_cols"][i])
        lnw, lnb = cols[:, 5], cols[:, 6]
        bon = [sb("bon%d" % k, [128, 8, TB]) for k in range(2)]
        gt = [sb("g%d" % k, [128, 8, TB]) for k in range(2)]
        yo = [sb("yo%d" % k, [128, 8, TB], BF16) for k in range(2)]
        ot = [sb("ot%d" % k, [128, 16, 64]) for k in range(2)]
        xc, sq = sb("xc", [128, 16, 64]), sb("sq", [128, 16, 64])
        sm, vr = sb("sm", [128, 16, 1]), sb("vr", [128, 16, 1])
        y1 = sb("y1", [128, 128])
        pT = Ring([P.ps(es, tag + "pT%d" % k, [128, 128], F32) for k in range(4)])
        identf = consts["ident_f32"]
        eps = consts["eps_0.00064"]
        f3 = lambda T_: T_.rearrange("(j p) s -> p j s", p=128)
        yv = sc["yT"][1024:2048, :].rearrange("(j p) s -> p j s", p=128)
        otv = sc["otok"].rearrange("(n t) (h v) -> n t h v", t=128, v=64)
        for ib, t0 in enumerate(range(0, S, TB)):
            b_, g_, y_ = bon[ib % 2], gt[ib % 2], yo[ib % 2]
            P.dma("sp", b_, f3(sc["bonusT"])[:, :, t0:t0 + TB])
            P.dma("act", g_, f3(sc["gT"])[:, :, t0:t0 + TB])
            for k in range(TB // 128):
                n = (t0 // 128) + k
                o = ot[n % 2]
                P.dma("sp", o, otv[n])
                P.reduce(sm.rearrange("p h o -> p (h o)"), o, ALU.add)
                P.ts("dve", sm, sm, 1.0 / 64, ALU.mult)
                P.tt("dve", xc, o, sm.broadcast_to([128, 16, 64]), ALU.subtract)
                P.act(sq, xc, AF.Square)
                P.reduce(vr.rearrange("p h o -> p (h o)"), sq, ALU.add)
                P.act(vr, vr, AF.Sqrt, bias=eps, scale=1.0 / 64)
                P.recip(vr, vr)
                P.tt("dve", xc, xc, vr.broadcast_to([128, 16, 64]), ALU.mult)
                for j in range(8):
                    p = pT.next()
                    P.tr(p, xc[:, 2 * j:2 * j + 2, :].rearrange("p a v -> p (a v)"), identf)
                    ts_ = slice(k * 128, (k + 1) * 128)
                    P.ts("dve", y1, p, lnw[:, j, :], ALU.mult, lnb[:, j, :], ALU.add)
                    P.tt("pool", y1, y1, b_[:, j, ts_], ALU.add)
                    P.tt("dve", y_[:, j, ts_], y1, g_[:, j, ts_], ALU.mult)
            P.dma("sp", yv[:, :, t0:t0 + TB], y_)
        P.barrier()


def host_layout_rwkv(inp):
    o = {}
    n = inp["rwkv_mu"].shape[0]
    mu = inp["rwkv_mu"]
    o["rwkv_mu_rkv"] = np.ascontiguousarray(np.stack([colvec(mu[:, k * 1024:(k + 1) * 1024]) for k in range(3)], 2)[..., None])
    o["rwkv_mu_wl"] = np.ascontiguousarray(mu[:, 3072:3136, None])
    o["rwkv_mu_al"] = np.ascontiguousarray(mu[:, 3136:3200, None])
    o["rwkv_mu_g1"] = np.ascontiguousarray(mu[:, 3200:3328, None])
    o["rwkv_mu_g2"] = np.ascontiguousarray(mu[:, 3328:3360, None])
    o["rwkv_cols"] = np.ascontiguousarray(np.stack([colvec(inp[k].reshape(n, 1024)) for k in RW_COLS], 2)[..., None])
    for k in ("rwkv_w2", "rwkv_a2", "rwkv_g2"):
        o[k] = inp[k]
    return o


def rwkv_scratch(nc, S, pre="", kind="Internal"):
    NCH = S // 64
    sc = {}
    _dram = dram
    dram_ = lambda nc_, n_, sh_, dt_: _dram(nc_, n_, sh_, dt_, kind)
    for nm in ("AhT", "BhT", "KhT", "RhT", "vT"):
        sc[nm] = dram_(nc, pre + nm, [1024, S], BF16)
    sc["PCT"] = dram_(nc, pre + "PCT", [1024, NCH], F32)
    sc["gT"] = dram_(nc, pre + "gT", [1024, S], F32)
    sc["bonusT"] = dram_(nc, pre + "bonusT", [1024, S], F32)
    sc["mats"] = dram_(nc, pre + "mats", [NCH, 64, 4, 16, 64], BF16)
    sc["tok3"] = dram_(nc, pre + "tok3", [NCH, 64, 3, 16, 64], BF16)
    sc["otok"] = dram_(nc, pre + "otok", [S, 1024], F32)
    return sc


def st_rope_tables(P, start_col, sc, S, consts, tag):
    TB = min(2048, S)
    with ExitStack() as es:
        sb = lambda n, sh, dt=F32: P.sb(es, tag + n, sh, dt)
        st_i, st_f = sb("sti", [128, 1], I32), sb("stf", [128, 1])
        P.dma("sp", st_i, start_col)
        P.copy("dve", st_f, st_i)
        ti, pos = sb("ti", [128, TB], I32), sb("pos", [128, TB])
        ang, kq, sn, cs = sb("ang", [128, TB]), sb("kq", [128, TB], I32), sb("sn", [128, TB]), sb("cs", [128, TB])
        for t0 in range(0, S, TB):
            P.I("pool", "iota", out=ti, pattern=[[1, TB]], base=t0, channel_multiplier=0)
            P.copy("dve", pos, ti)
            P.ts("dve", pos, pos, st_f, ALU.add)
            for nm, fq in (("A", consts["freqA"]), ("I", consts["freqI"])):
                P.ts("dve", ang, pos, fq, ALU.mult)
                sincos(P, ang, kq, ang, sn, cs, consts)
                P.dma("sp", sc["cos" + nm][:, t0:t0 + TB], cs)
                P.dma("sp", sc["sin" + nm][:, t0:t0 + TB], sn)
        P.barrier()


def st_dsa_kprep(P, sc, S, consts, tag):
    TB = 512
    with ExitStack() as es:
        sb = lambda n, sh, dt=F32: P.sb(es, tag + n, sh, dt)
        ring = lambda n, sh, dt=F32, k=2: Ring([P.sb(es, tag + n + str(q), sh, dt) for q in range(k)])
        kr, ki = ring("kr", [32, TB]), ring("ki", [64, TB])
        cA, sA, cI, sI = ring("cA", [32, TB]), ring("sA", [32, TB]), ring("cI", [64, TB]), ring("sI", [64, TB])
        t1, t2 = ring("t1", [64, TB]), ring("t2", [64, TB])
        okr, oki = ring("okr", [32, TB], BF16), ring("oki", [64, TB], BF16)
        ckv = ring("ckv", [128, 2, TB], BF16)
        ctok = ring("ctok", [128, TB // 128, 256], BF16)
        pr = Ring([P.ps(es, tag + "pr%d" % q, [64, TB], F32) for q in range(2)])
        pt = Ring([P.ps(es, tag + "pt%d" % q, [128, 2, 128], BF16) for q in range(2)])
        rotA, rotI = consts["rotA32"], consts["rotI128"]
        identb = consts["ident_bf"]
        for t0 in range(0, S, TB):
            ts_ = slice(t0, t0 + TB)
            for (src, x, c, s, cn, sn_, rot, npart, o, dst) in (
                    (sc["kropeT"], kr.next(), cA.next(), sA.next(), "cosA", "sinA", rotA, 32, okr.next(), sc["kvcatT"][256:288, :]),
                    (sc["kidxnT"], ki.next(), cI.next(), sI.next(), "cosI", "sinI", rotI, 64, oki.next(), sc["kidxT"])):
                P.dma("sp", x, src[:, ts_])
                P.dma("act", c, sc[cn][0:npart, ts_])
                P.dma("act", s, sc[sn_][0:npart, ts_])
                p = pr.next()
                P.mm(p[0:npart, :], rot[0:npart, 0:npart], x)
                a, b = t1.next()[0:npart, :], t2.next()[0:npart, :]
                P.tt("dve", a, p[0:npart, :], s, ALU.mult)
                P.tt("pool", b, x, c, ALU.mult)
                P.tt("dve", o, a, b, ALU.add)
                P.dma("sp", dst[:, ts_], o)
            ck = ckv.next()
            P.dma("sp", ck, sc["ckvnT"].rearrange("(c p) s -> p c s", p=128)[:, :, ts_])
            P.dma("act", sc["kvcatT"][0:256, :].rearrange("(c p) s -> p c s", p=128)[:, :, ts_], ck)
            ct = ctok.next()
            for k in range(TB // 128):
                tp = pt.next()
                for c2 in range(2):
                    P.tr(tp[:, c2, :], ck[:, c2, k * 128:(k + 1) * 128], identb)
                P.copy("act", ct[:, k, :].rearrange("p (c r) -> p c r", c=2), tp)
            P.dma("sp", sc["ckvtok"].rearrange("(n p) r -> p n r", p=128)[:, t0 // 128:(t0 + TB) // 128, :], ct)
        P.barrier()


def st_dsa_qprep(P, w, i, sc, S, consts, tag):
    TB = 512
    with ExitStack() as es:
        sb = lambda n, sh, dt=F32: P.sb(es, tag + n, sh, dt)
        ring = lambda n, sh, dt=F32, k=2: Ring([P.sb(es, tag + n + str(q), sh, dt) for q in range(k)])
        wf = sb("wf", [128, 4, 1024])
        wuq, wqi = sb("wuq", [128, 4, 1024], BF16), sb("wqi", [128, 4, 1024], BF16)
        P.dma("sp", wf, w["dsa_w_uq"][i].rearrange("(c p) h d -> p c (h d)", p=128))
        P.copy("act", wuq, wf)
        P.dma("sp", wf, w["dsa_w_qidx"][i].rearrange("(c p) h d -> p c (h d)", p=128))
        P.copy("act", wqi, wf)
        wkf = sb("wkf", [128, 8, 256])
        wuk = sb("wuk", [128, 8, 256], BF16)
        P.dma("sp", wkf, w["dsa_wukT_pad"][i].rearrange("h d r -> d h r"))
        P.copy("act", wuk, wkf)
        cq = ring("cq", [128, 4, TB], BF16)
        cA, sA, cI, sI = ring("cA", [32, TB]), ring("sA", [32, TB]), ring("cI", [128, TB]), ring("sI", [128, TB])
        qh = ring("qh", [128, TB], BF16)
        x32 = ring("x32", [32, TB])
        xi = ring("xi", [128, TB])
        t1, t2 = ring("t1", [128, TB]), ring("t2", [128, TB])
        ol = ring("ol", [128, 2, TB], BF16, 3)
        orp = ring("orp", [32, TB], BF16, 3)
        oi = ring("oi", [128, TB], BF16, 3)
        pq = Ring([P.ps(es, tag + "pq%d" % q, [128, TB], F32) for q in range(3)])
        prr = Ring([P.ps(es, tag + "prr%d" % q, [128, TB], F32) for q in range(2)])
        pl = Ring([P.ps(es, tag + "pl%d" % q, [128, TB], F32) for q in range(3)])
        rotA, rotI = consts["rotA32"], consts["rotI128"]
        for t0 in range(0, S, TB):
            ts_ = slice(t0, t0 + TB)
            c = cq.next()
            P.dma("sp", c, sc["cqnT"].rearrange("(c p) s -> p c s", p=128)[:, :, ts_])
            ca, sa, ci, si = cA.next(), sA.next(), cI.next(), sI.next()
            P.dma("act", ca, sc["cosA"][0:32, ts_])
            P.dma("act", sa, sc["sinA"][0:32, ts_])
            P.dma("act", ci, sc["cosI"][:, ts_])
            P.dma("act", si, sc["sinI"][:, ts_])
            import os
            dbg = int(os.environ.get("QDBG", "511"))
            for h in range(8 if dbg & 1 else 0):
                p = pq.next()
                for k in range(4):
                    P.mm(p, wuq[:, k, h * 128:(h + 1) * 128], c[:, k, :], start=(k == 0), stop=(k == 3))
                q_ = qh.next()
                P.copy("act", q_, p)
                if dbg & 4:
                    x = x32.next()
                    P.copy("dve", x, p[0:32, :])
                    pr_ = prr.next()
                    if dbg & 32:
                        P.mm(pr_[0:32, :], rotA, x)
                    a, b = t1.next()[0:32, :], t2.next()[0:32, :]
                    if dbg & 64:
                        P.tt("dve", a, pr_[0:32, :], sa, ALU.mult)
                    if dbg & 128:
                        P.tt("pool", b, x, ca, ALU.mult)
                    o_r = orp.next()
                    if dbg & 256:
                        P.tt("dve", o_r, a, b, ALU.add)
                    if dbg & 16:
                        P.dma("sp", sc["qcatT"][h, 256:288, ts_], o_r)
                o_l = ol.next()
                for c2 in range(2 if dbg & 8 else 0):
                    p2 = pl.next()
                    P.mm(p2, wuk[:, h, c2 * 128:(c2 + 1) * 128], q_)
                    P.copy("act" if c2 == 0 else "dve", o_l[:, c2, :], p2)
                if dbg & 8:
                    P.dma("sp", sc["qcatT"][h, 0:256, ts_].rearrange("(c p) s -> p c s", p=128), o_l)
            for j in range(8 if dbg & 2 else 0):
                p = pq.next()
                for k in range(4):
                    P.mm(p, wqi[:, k, j * 128:(j + 1) * 128], c[:, k, :], start=(k == 0), stop=(k == 3))
                x = xi.next()
                P.copy("act", x, p)
                pr_ = prr.next()
                P.mm(pr_, rotI, x)
                a, b = t1.next(), t2.next()
                P.tt("dve", a, pr_, si, ALU.mult)
                P.tt("pool", b, x, ci, ALU.mult)
                o_i = oi.next()
                P.tt("dve", o_i, a, b, ALU.add)
                P.dma("sp", sc["qidxT"][j * 128:(j + 1) * 128, ts_], o_i)
        P.barrier()


NEG_MASK = -30000.0


def st_dsa_attn(P, w, i, sc, S, consts, tag):
    QT = 128
    NQ = S // QT
    TOPK = min(256, S // 4)
    NS = 20
    scale = 128.0 ** -0.5
    with ExitStack() as es:
        sb = lambda n, sh, dt=F32: P.sb(es, tag + n, sh, dt)
        ring = lambda n, sh, dt=F32, k=2: Ring([P.sb(es, tag + n + str(q), sh, dt) for q in range(k)])
        kidx = sb("kidx", [64, S], BF16)
        kvc = sb("kvc", [128, 3, S], BF16)
        ctok = sb("ctok", [128, S // 128, 256], BF16)
        P.dma("sp", kidx, sc["kidxT"])
        P.dma("sp", kvc[:, 0:2, :], sc["kvcatT"][0:256, :].rearrange("(c p) s -> p c s", p=128))
        P.dma("sp", kvc[0:32, 2, :], sc["kvcatT"][256:288, :])
        P.dma("sp", ctok, sc["ckvtok"].rearrange("(n p) r -> p n r", p=128))
        wuv = sb("wuv", [128, 2, 1024], BF16)
        with ExitStack() as es2:
            wvf = P.sb(es2, tag + "wvf", [128, 2, 1024], F32)
            P.dma("sp", wvf, w["dsa_w_uv"][i].rearrange("(c p) h d -> p c (h d)", p=128))
            P.copy("act", wuv, wvf)
            P.barrier()
        qidx = [sb("qidx%d" % q, [64, 16, QT], BF16) for q in range(2)]
        qcat = [sb("qcat%d" % q, [128, 8, 3, QT], BF16) for q in range(2)]
        wT = [sb("wT%d" % q, [16, QT]) for q in range(2)]
        wtok = sb("wtok", [128, 16])
        Dm = sb("Dm", [128, 16, 128], BF16)
        rl = ring("rl", [128, 512], BF16, 4)
        scb = [sb("scb%d" % q, [128, S]) for q in range(2)]
        madd = [sb("madd%d" % q, [128, S], BF16) for q in range(2)]
        junk = sb("junk", [128, S], BF16)
        sm = [sb("sm%d" % q, [128, S]) for q in range(1)] * 2
        pb = [sb("pb%d" % q, [128, S], BF16) for q in range(2)]
        pTs = [sb("pTs%d" % q, [128, S // 128, 128], BF16) for q in range(2)]
        lo = [sb("lo%d" % q, [128, 1]) for q in range(2)]
        whalf = [sb("whalf%d" % q, [128, NS + 1]) for q in range(2)]
        mxs, mns, w0s = sb("mxs", [128, 1]), sb("mns", [128, 1]), sb("w0s", [128, 1])
        mid, cnt, inc = sb("mid", [128, 1]), sb("cnt", [128, 1]), sb("inc", [128, 1])
        mx, rs = ring("mx", [128, 1]), ring("rs", [128, 1])
        olat = ring("olat", [128, 256], BF16)
        oT = ring("oT", [128, 2, 128], BF16)
        ya = ring("ya", [128, 8, QT], BF16)
        p_l = Ring([P.ps(es, tag + "pl%d" % q, [128, 512], F32) for q in range(2)])
        p_sc = P.ps(es, tag + "psc", [128, 512], F32)
        p_qk = Ring([P.ps(es, tag + "pqk%d" % q, [128, 512], F32) for q in range(2)])
        p_ts = Ring([P.ps(es, tag + "ptr%d" % q, [128, 4, 128], BF16) for q in range(2)])
        p_m = P.ps(es, tag + "pm", [128, 512], F32)
        p_mb = p_m.bitcast(BF16)
        p_o = p_m[:, 256:512]
        identb, identf = consts["ident_bf"], consts["ident_f32"]
        pow2 = consts["pow2row"]

        def idx_phase(qt):
            s_ = qt % 2
            t0 = qt * QT
            L = t0 + QT
            qi_, qc_, wT_ = qidx[s_], qcat[s_], wT[s_]
            P.dma("sp", qi_, sc["qidxT"].rearrange("(h d) s -> d h s", d=64)[:, :, t0:t0 + QT])
            for c2 in range(2):
                P.dma("act", qc_[:, :, c2, :], sc["qcatT"][:, c2 * 128:(c2 + 1) * 128, t0:t0 + QT].rearrange("h p s -> p h s"))
            P.dma("act", qc_[0:32, :, 2, :], sc["qcatT"][:, 256:288, t0:t0 + QT].rearrange("h p s -> p h s"))
            md = madd[s_]
            if t0 < TOPK:
                def gen0():
                    P.memset("dve", md[:, 0:L], 0.0)
                    P.memset("dve", md[0:64, t0 + 64:t0 + 128], NEG_MASK)
                    yield
                return gen0()
            P.dma("sp", wT_, sc["widxT"][:, t0:t0 + QT])
            P.tr(p_m[:, 0:16], wT_, identf[0:16, 0:16])
            P.ts("dve", wtok, p_m[:, 0:16], 1.0 / 32.0, ALU.mult)
            for h in range(16):
                P.ts("dve" if h % 2 else "pool", Dm[:, h, :], identb, wtok[:, h:h + 1], ALU.mult)
            sc_ = scb[s_]
            for kb in range(0, L, 512):
                kw = min(512, L - kb)
                rprev = None
                for h in range(17):
                    if h < 16:
                        p = p_l.next()
                        P.mm(p[:, 0:kw], qi_[:, h, :], kidx[:, kb:kb + kw])
                        r = rl.next()
                        P.act(r[:, 0:kw], p[:, 0:kw], AF.Relu)
                    if rprev is not None:
                        P.mm(p_sc[:, 0:kw], Dm[:, h - 1, :], rprev[:, 0:kw], start=(h == 1), stop=(h == 16))
                    rprev = r
                P.copy("act", sc_[:, kb:kb + kw], p_sc[:, 0:kw])

            def gen():
                lo_, wh = lo[s_], whalf[s_]
                P.reduce(mxs, sc_[:, 0:L], ALU.max)
                yield
                P.reduce(mns, sc_[:, 0:L], ALU.min)
                P.memset("dve", sc_[0:64, t0 + 64:t0 + 128], -1e30)
                P.ts("dve", lo_, mns, -1.0, ALU.add)
                P.tt("dve", w0s, mxs, lo_, ALU.subtract)
                P.ts("dve", wh, pow2[:, 0:NS + 1], w0s, ALU.mult)
                P.tt("dve", mid, lo_, wh[:, 0:1], ALU.add)
                yield
                for k in range(NS):
                    P.ts("dve", junk[:, 0:L], sc_[:, 0:L], mid, ALU.is_gt, 0.0, ALU.add, accum_out=cnt)
                    P.stt("dve", inc, cnt, TOPK - 0.5, wh[:, k:k + 1], ALU.is_gt, ALU.mult)
                    P.stt("dve", mid, inc, mid, wh[:, k + 1:k + 2], ALU.add, ALU.subtract)
                    yield
                P.tt("dve", lo_, mid, wh[:, NS:NS + 1], ALU.subtract)
                P.ts("dve", md[:, 0:L], sc_[:, 0:L], lo_, ALU.is_le, NEG_MASK, ALU.mult)
                yield
            return gen()

        ycur = {}
        olats = {}

        def stage1(qt, h):
            s_ = qt % 2
            t0 = qt * QT
            L = t0 + QT
            qc_, md = qcat[s_], madd[s_]
            hb = h % 2
            sm_, pb_ = sm[hb], pb[hb]
            for kb in range(0, L, 512):
                kw = min(512, L - kb)
                p = p_qk.next()
                P.mm(p[:, 0:kw], qc_[:, h, 0, :], kvc[:, 0, kb:kb + kw], start=True, stop=False)
                P.mm(p[:, 0:kw], qc_[:, h, 1, :], kvc[:, 1, kb:kb + kw], start=False, stop=False)
                P.mm(p[:, 0:kw], qc_[0:32, h, 2, :], kvc[0:32, 2, kb:kb + kw], start=False, stop=True)
                P.stt("dve", sm_[:, kb:kb + kw], p[:, 0:kw], scale, md[:, kb:kb + kw], ALU.mult, ALU.add)
            mx_, rs_ = mxr[hb], rsr[hb]
            P.reduce(mx_, sm_[:, 0:L], ALU.max)
            P.ts("dve", mx_, mx_, -1.0, ALU.mult)
            P.act(pb_[:, 0:L], sm_[:, 0:L], AF.Exp, bias=mx_, accum_out=rs_)
            P.recip(rs_, rs_)

        def stage2(qt, h):
            t0 = qt * QT
            L = t0 + QT
            hb = h % 2
            pb_, pT_, rs_ = pb[hb], pTs[hb], rsr[hb]
            if h == 0:
                ycur[qt] = ya.next()
            y_ = ycur[qt]
            nb = L // 128
            for b0 in range(0, nb, 4):
                nn = min(4, nb - b0)
                p_t = p_ts.next()
                for b in range(nn):
                    P.tr(p_t[:, b, :], pb_[:, (b0 + b) * 128:(b0 + b + 1) * 128], identb)
                P.copy("act", pT_[:, b0:b0 + nn, :], p_t[:, 0:nn, :])
            for b in range(nb):
                P.mm(p_o, pT_[:, b, :], ctok[:, b, :], start=(b == 0), stop=(b == nb - 1))
            ol_ = olat.next()
            P.ts("dve", ol_, p_o, rs_, ALU.mult)
            olats[(qt, h)] = ol_

        def stage3(qt, h):
            t0 = qt * QT
            y_ = ycur[qt]
            ol_ = olats.pop((qt, h))
            oT_ = oT.next()
            for c2 in range(2):
                P.tr(p_mb[:, c2 * 128:(c2 + 1) * 128], ol_[:, c2 * 128:(c2 + 1) * 128], identb)
            P.copy("act", oT_.rearrange("p c q -> p (c q)"), p_mb[:, 0:256])
            for c2 in range(2):
                P.mm(p_m[:, 128:256], wuv[:, c2, h * 128:(h + 1) * 128], oT_[:, c2, :], start=(c2 == 0), stop=(c2 == 1))
            P.copy("act", y_[:, h, :], p_m[:, 128:256])
            if h == 7:
                P.dma("sp", sc["yT"][0:1024, t0:t0 + QT].rearrange("(h d) s -> d h s", d=128), y_)
                del ycur[qt]

        mxr = [sb("mxr%d" % q, [128, 1]) for q in range(2)]
        rsr = [sb("rsr%d" % q, [128, 1]) for q in range(2)]
        items = [(qt, h) for qt in range(NQ) for h in range(8)]
        g = idx_phase(0)
        for _ in g:
            pass
        g = None
        stage1(0, 0)
        for k, (qt, h) in enumerate(items):
            stage2(qt, h)
            if k >= 1:
                stage3(*items[k - 1])
            if k + 1 < len(items):
                nqt, nh = items[k + 1]
                if nh == 0 and g is not None:
                    for _ in g:
                        pass
                    g = None
                stage1(nqt, nh)
            if h == 0 and qt + 1 < NQ:
                g = idx_phase(qt + 1)
            if g is not None:
                for _ in range(4):
                    next(g, None)
        stage3(*items[-1])
        P.barrier()


def host_layout_dsa(inp):
    o = {}
    n = inp["dsa_w_uk"].shape[0]
    wuk = inp["dsa_w_uk"]
    pad = np.zeros((n, 8, 128, 256), np.float32)
    pad[:, :, 32:, :] = wuk.transpose(0, 2, 3, 1)
    o["dsa_wukT_pad"] = pad
    o["dsa_cq_norm_pc"] = colvec(inp["dsa_cq_norm"])
    o["dsa_ckv_norm_pc"] = colvec(inp["dsa_ckv_norm"])
    o["dsa_kidx_norm_pc"] = colvec(inp["dsa_kidx_norm"], 64)
    for k in ("dsa_w_uq", "dsa_w_uv", "dsa_w_qidx"):
        o[k] = inp[k]
    return o


def dsa_scratch(nc, S, pre="", kind="Internal"):
    d = lambda n, sh, dt: dram(nc, pre + n, sh, dt, kind)
    sc = {}
    for nm in ("cosA", "sinA", "cosI", "sinI"):
        sc[nm] = d(nm, [128, S], F32)
    sc["cqT"] = d("cqT", [512, S], F32)
    sc["ckvT"] = d("ckvT", [256, S], F32)
    sc["kropeT"] = d("kropeT", [32, S], F32)
    sc["kidxrT"] = d("kidxrT", [64, S], F32)
    sc["widxT"] = d("widxT", [16, S], F32)
    sc["cqnT"] = d("cqnT", [512, S], BF16)
    sc["ckvnT"] = d("ckvnT", [256, S], BF16)
    sc["kidxnT"] = d("kidxnT", [64, S], F32)
    sc["kvcatT"] = d("kvcatT", [288, S], BF16)
    sc["kidxT"] = d("kidxT", [64, S], BF16)
    sc["ckvtok"] = d("ckvtok", [S, 256], BF16)
    sc["qcatT"] = d("qcatT", [8, 288, S], BF16)
    sc["qidxT"] = d("qidxT", [1024, S], BF16)
    return sc


def dsa_mixer_stages(P, w, i, sc, S, consts, tag):
    st_norm(P, sc["cqT"], w["dsa_cq_norm_pc"][i], sc["cqnT"], 512, S, 1e-5, consts, tag + "nq")
    st_norm(P, sc["ckvT"], w["dsa_ckv_norm_pc"][i], sc["ckvnT"], 256, S, 1e-5, consts, tag + "nk")
    st_norm(P, sc["kidxrT"], w["dsa_kidx_norm_pc"][i], sc["kidxnT"], 64, S, 1e-5, consts, tag + "ni", out_dt=F32)
    st_dsa_kprep(P, sc, S, consts, tag + "kp")
    st_dsa_qprep(P, w, i, sc, S, consts, tag + "qp")
    st_dsa_attn(P, w, i, sc, S, consts, tag + "at")


A_SPLITS = [("cqT", 0, 512), ("ckvT", 512, 256), ("kropeT", 768, 32), ("kidxrT", 800, 64), ("widxT", 864, 16)]


def even_layer(P, xT, memT, w, l, sc, S, consts, tag):
    i = l // 2
    D = 2048
    st_norm(P, xT, w["norm_mix"][l], sc["xnT"], D, S, 1e-5, consts, tag + "n1")
    with ExitStack() as es:
        jobs = []
        Win = w["even_w_in"][i]
        for nm, c0, n in A_SPLITS:
            jobs.append((Win[:, c0:c0 + n], n, EpiStore(P, es, sc[nm], F32, tag + "e" + nm, nbuf=2)))
        jobs.append((Win[:, 880:4240], 3360, EpiStore(P, es, sc["hBT"], F32, tag + "ehB")))
        st_mm(P, sc["xnT"], D, S, jobs, consts, tag + "mi", wscr=sc["wscr"])
    dsa_mixer_stages(P, w, i, sc, S, consts, tag + "d")
    st_rwkv_prep(P, sc["hBT"], w, i, sc, S, consts, tag + "rp")
    st_rwkv_A(P, sc, S, consts, tag + "ra")
    st_rwkv_B(P, sc, S, consts, tag + "rb")
    st_rwkv_post(P, w, i, sc, S, consts, tag + "rq")
    with ExitStack() as es:
        st_mm(P, sc["yT"], D, S, [(w["even_w_out"][i], D, EpiResid(P, es, xT, tag + "rmo"))], consts, tag + "mo", wscr=sc["wscr"])
    layer_tail(P, xT, memT, w, l, sc, S, consts, tag)


PLAIN_W = ["xattn_wq", "xattn_wkv", "xattn_wo", "ffn_up", "ffn_down", "even_w_in", "even_w_out",
           "odd_w_in", "odd_w_out", "s5_w_glu"]


def host_layout_all(inp):
    o = {}
    for k in PLAIN_W:
        o[k] = np.ascontiguousarray(inp[k], dtype=np.float32)
    for k in ("norm_mix", "norm_xattn", "norm_mem", "norm_ffn"):
        o[k] = colvec(np.asarray(inp[k], np.float32))
    o["final_norm"] = colvec(np.asarray(inp["final_norm"], np.float32))
    f = {k: np.asarray(v, np.float32) for k, v in inp.items() if k.startswith(("s5_", "rwkv_", "dsa_"))}
    o.update(host_layout_s5(f))
    o.update(host_layout_rwkv(f))
    o.update(host_layout_dsa(f))
    return o


def build_program(S, wshapes, depth=4, dbg_out=None):
    nc = bass.Bass("TRN2", target_bir_lowering=False)
    D = 2048
    xin = dram(nc, "xT_in", [D, S], F32, "ExternalInput")
    memT = dram(nc, "memT", [D, 256], F32, "ExternalInput")
    start = dram(nc, "start_col", [128, 1], I32, "ExternalInput")
    outT = dram(nc, "outT", [D, S], F32, "ExternalOutput")
    w = {k: dram(nc, k, list(sh), F32, "ExternalInput") for k, sh in wshapes.items()}
    hc = host_consts()
    cd = {k: dram(nc, "c_" + k, list(v.shape), CONST_SPECS[k][1], "ExternalInput") for k, v in hc.items()}
    sc = {}
    sc.update(dsa_scratch(nc, S))
    sc.update(rwkv_scratch(nc, S))
    sc["xT"] = dram(nc, "xres", [D, S], F32)
    sc["xnT"] = dram(nc, "xnT", [D, S], BF16)
    sc["hBT"] = dram(nc, "hBT", [3360, S], F32)
    sc["yT"] = dram(nc, "yT", [D, S], BF16)
    sc["uT"] = dram(nc, "uT", [D, S], F32)
    sc["zT"] = dram(nc, "zT", [D, S], F32)
    sc["memnT"] = dram(nc, "memnT", [D, 256], BF16)
    sc["qT"] = dram(nc, "qT", [512, S], BF16)
    sc["kT"] = dram(nc, "kT", [512, 256], BF16)
    sc["vtok"] = dram(nc, "vtok", [256, 512], BF16)
    sc["oT"] = dram(nc, "oT", [512, S], BF16)
    sc["hT"] = dram(nc, "hT", [8192, S], BF16)
    sc["wscr"] = dram(nc, "wscr", [20 * 1024 * 1024], BF16)
    with ExitStack() as es:
        P = Prog(nc, es)
        consts = make_consts(P, es, cd)
        xT = sc["xT"]
        for c in range(16):
            P.dma("sp" if c % 2 == 0 else "act", xT[c * 128:(c + 1) * 128, :], xin[c * 128:(c + 1) * 128, :])
        P.barrier()
        st_rope_tables(P, start, sc, S, consts, "rt")
        for l in range(depth):
            if l % 2 == 0:
                even_layer(P, xT, memT, w, l, sc, S, consts, "L%d" % l)
            else:
                odd_layer(P, xT, memT, w, l, sc, S, consts, "L%d" % l)
        st_norm(P, xT, w["final_norm"], outT, D, S, 1e-5, consts, "fn", out_dt=F32)
        P.barrier()
        nc._ninst = P.ninst
    return nc


_CACHE = {}


def kernel(**inputs):
    x = np.asarray(inputs["x"], np.float32)
    mem = np.asarray(inputs["mem"], np.float32)
    start = np.asarray(inputs["start_frame"]).astype(np.int32)
    B, S, D = x.shape
    hl = host_layout_all(inputs)
    hc = host_consts()
    wshapes = {k: v.shape[1:] if False else v.shape for k, v in hl.items()}
    nc = build_program(S, wshapes)
    in_maps = []
    for b in range(B):
        m = {"xT_in": np.ascontiguousarray(x[b].T), "memT": np.ascontiguousarray(mem[b].T),
             "start_col": np.full((128, 1), start[b], np.int32)}
        m.update(hl)
        for k, v in hc.items():
            m["c_" + k] = v
        in_maps.append(m)
    res = run_bass_kernel_spmd(nc, in_maps, core_ids=list(range(B)))
    out = np.stack([np.ascontiguousarray(np.asarray(res.results[b]["outT"]).T) for b in range(B)], 0)
    return out.astype(np.float32)
```
